# Optimizing a Trainium2 kernel written in Bass

```python
import jax
import jax.numpy as jnp
from jax import lax
import numpy as np

D_MODEL = 2048
BATCH = 1
SEQ = 8192
DEPTH = 2

M_HEADS = 4
M_QK = 256
M_V = 512
M_CHUNK = 128
A_HEADS = 8
A_QK = 128
A_V = 2 * A_QK
Q_BLOCK = 128
ROPE_THETA = 500000.0
ROPE_DIM = A_QK // 4
N_EXPERTS = 32
TOP_K = 4
D_FF = D_MODEL
SWIGLU_LIMIT = 7.0
SWIGLU_ALPHA = 1.702
E_BLOCK = 256
EPS = 1e-6

M_QW = M_HEADS * M_QK
M_VW = M_HEADS * M_V
M_NG = 4 * M_HEADS
A_QW = A_HEADS * 2 * A_QK
A_VW = A_HEADS * A_V
IN_SIZES = (M_QW, M_QW, M_VW, M_VW, M_NG, A_QW, A_QW, A_VW, D_MODEL, D_MODEL)
IN_COLS = 2 * M_QW + 2 * M_VW + M_NG + 2 * A_QW + A_VW + 2 * D_MODEL

kernel_name = "hybrid_mlstm_diffattn_moe_encoder"


def _rmsnorm(x, w):
    x32 = x.astype(jnp.float32)
    y = x32 * lax.rsqrt(jnp.mean(x32 * x32, axis=-1, keepdims=True) + EPS)
    return (y * w.astype(jnp.float32)).astype(x.dtype)


def _modulate(h, shift, scale):
    return h * (1 + scale[:, None, :]) + shift[:, None, :]


def _rope_tables(S):
    inv = ROPE_THETA ** (-jnp.arange(0, ROPE_DIM, 2, dtype=jnp.float32) / ROPE_DIM)
    ang = jnp.arange(S, dtype=jnp.float32)[:, None] * inv[None, :]
    return jnp.cos(ang), jnp.sin(ang)


def _rope_partial(t, cos, sin):
    half = ROPE_DIM // 2
    c = cos[None, :, None, None, :]
    s = sin[None, :, None, None, :]
    t32 = t.astype(jnp.float32)
    x1 = t32[..., :half]
    x2 = t32[..., half:ROPE_DIM]
    out = jnp.concatenate([x1 * c - x2 * s, x1 * s + x2 * c, t32[..., ROPE_DIM:]], axis=-1)
    return out.astype(t.dtype)


def _mlstm_scan(q, k, v, i_pre, log_f):
    B, H, S, DK = q.shape
    DV = v.shape[-1]
    nc = S // M_CHUNK

    def to_chunks(t):
        return jnp.moveaxis(t.reshape(B, H, nc, M_CHUNK, *t.shape[3:]), 2, 0)

    xs = (to_chunks(q), to_chunks(k), to_chunks(v), to_chunks(i_pre), to_chunks(log_f))
    tri = jnp.tril(jnp.ones((M_CHUNK, M_CHUNK), dtype=bool))

    def step(carry, inp):
        C, n, m = carry
        qb, kb, vb, ib, fb = inp
        b = jnp.cumsum(fb, axis=-1)
        dmat = jnp.where(tri, b[..., :, None] - b[..., None, :] + ib[..., None, :], -jnp.inf)
        inter = b + m[..., None]
        m_t = jnp.maximum(inter, jnp.max(dmat, axis=-1))
        w_intra = jnp.exp(dmat - m_t[..., None])
        w_inter = jnp.exp(inter - m_t)
        s = jnp.einsum("bhtd,bhsd->bhts", qb, kb) * w_intra
        num = jnp.einsum("bhts,bhsv->bhtv", s, vb) + w_inter[..., None] * jnp.einsum("bhtd,bhdv->bhtv", qb, C)
        den = jnp.sum(s, axis=-1) + w_inter * jnp.einsum("bhtd,bhd->bht", qb, n)
        h = num / jnp.maximum(jnp.abs(den), jnp.exp(-m_t))[..., None]
        b_last = b[..., -1]
        src = b_last[..., None] - b + ib
        m_new = jnp.maximum(b_last + m, jnp.max(src, axis=-1))
        w_src = jnp.exp(src - m_new[..., None])
        decay = jnp.exp(b_last + m - m_new)
        C_new = decay[..., None, None] * C + jnp.einsum("bhsd,bhsv->bhdv", kb * w_src[..., None], vb)
        n_new = decay[..., None] * n + jnp.einsum("bhs,bhsd->bhd", w_src, kb)
        return (C_new, n_new, m_new), h

    init = (jnp.zeros((B, H, DK, DV), jnp.float32),
            jnp.zeros((B, H, DK), jnp.float32),
            jnp.zeros((B, H), jnp.float32))
    _, h = lax.scan(step, init, xs)
    return jnp.moveaxis(h, 0, 2).reshape(B, H, S, DV)


def _flip_seq(t):
    return jnp.flip(t, axis=2)


def _mlstm_branch(q, k, v, o, g, b_gates, w_norm):
    B, S, _ = q.shape

    def heads(t, d):
        return t.reshape(B, S, M_HEADS, d).transpose(0, 2, 1, 3).astype(jnp.float32)

    qh = heads(q, M_QK) * (M_QK ** -0.5)
    kh = heads(k, M_QK)
    vh = heads(v, M_V)
    gp = (g.astype(jnp.float32) + b_gates.astype(jnp.float32)).reshape(B, S, 4, M_HEADS).transpose(2, 0, 3, 1)
    i_fw, f_fw, i_bw, f_bw = gp[0], gp[1], gp[2], gp[3]
    h_fw = _mlstm_scan(qh, kh, vh, i_fw, jax.nn.log_sigmoid(f_fw))
    h_bw = _flip_seq(_mlstm_scan(_flip_seq(qh), _flip_seq(kh), _flip_seq(vh),
                                 _flip_seq(i_bw), _flip_seq(jax.nn.log_sigmoid(f_bw))))
    h = _rmsnorm(h_fw + h_bw, w_norm[:, None, :])
    h = h.transpose(0, 2, 1, 3).reshape(B, S, M_VW)
    return (h * jax.nn.sigmoid(o.astype(jnp.float32))).astype(q.dtype)


def _diff_attention(q, k, v, lam_q1, lam_k1, lam_q2, lam_k2, w_subln, lam_init, cos, sin):
    B, S, _ = q.shape
    qh = _rope_partial(q.reshape(B, S, A_HEADS, 2, A_QK), cos, sin) * (A_QK ** -0.5)
    kh = _rope_partial(k.reshape(B, S, A_HEADS, 2, A_QK), cos, sin)
    vh = v.reshape(B, S, A_HEADS, A_V)
    lam = (jnp.exp(jnp.sum(lam_q1.astype(jnp.float32) * lam_k1.astype(jnp.float32)))
           - jnp.exp(jnp.sum(lam_q2.astype(jnp.float32) * lam_k2.astype(jnp.float32))) + lam_init)
    nb = S // Q_BLOCK
    qb = jnp.moveaxis(qh.reshape(B, nb, Q_BLOCK, A_HEADS, 2, A_QK), 1, 0)

    def block(qblk):
        s = jnp.einsum("bqhcd,bkhcd->bhcqk", qblk, kh, preferred_element_type=jnp.float32)
        p = jax.nn.softmax(s, axis=-1)
        a = p[:, :, 0] - lam * p[:, :, 1]
        return jnp.einsum("bhqk,bkhv->bqhv", a.astype(vh.dtype), vh)

    o = lax.map(block, qb)
    o = jnp.moveaxis(o, 0, 1).reshape(B, S, A_HEADS, A_V)
    o = _rmsnorm(o, w_subln) * (1.0 - lam_init)
    return o.reshape(B, S, A_VW)


def _mixer(h, w_in, b_mgates, m_norm, lam_q1, lam_k1, lam_q2, lam_k2, a_norm,
           w_br_m, w_br_a, w_out, lam_init, cos, sin):
    proj = h @ w_in
    bounds = []
    acc = 0
    for size in IN_SIZES[:-1]:
        acc += size
        bounds.append(acc)
    mq, mk, mv, mo, mg, aq, ak, av, gm, ga = jnp.split(proj, bounds, axis=-1)
    y_m = _mlstm_branch(mq, mk, mv, mo, mg, b_mgates, m_norm)
    y_a = _diff_attention(aq, ak, av, lam_q1, lam_k1, lam_q2, lam_k2, a_norm, lam_init, cos, sin)
    merged = jax.nn.sigmoid(gm) * (y_m @ w_br_m) + jax.nn.sigmoid(ga) * (y_a @ w_br_a)
    return merged @ w_out


def _moe(h, w_router, b_router, w_gu, b_gu, w_down, b_down):
    B, S, D = h.shape
    N = B * S
    NK = N * TOP_K
    P = -(-NK // E_BLOCK) * E_BLOCK + N_EXPERTS * E_BLOCK
    nblk = P // E_BLOCK
    xt = h.reshape(N, D)
    logits = (xt @ w_router + b_router).astype(jnp.float32)
    top_val, top_idx = lax.top_k(logits, TOP_K)
    gates = jax.nn.softmax(top_val, axis=-1)
    eid = top_idx.reshape(NK)
    tok = jnp.repeat(jnp.arange(N, dtype=jnp.int32), TOP_K)
    order = jnp.argsort(eid)
    e_sorted = eid[order]
    counts = jnp.bincount(eid, length=N_EXPERTS)
    starts = jnp.cumsum(counts) - counts
    padded = (counts + E_BLOCK - 1) // E_BLOCK * E_BLOCK
    pends = jnp.cumsum(padded)
    pstarts = pends - padded
    dest = pstarts[e_sorted] + jnp.arange(NK, dtype=counts.dtype) - starts[e_sorted]
    slot_tok = jnp.zeros((P,), jnp.int32).at[dest].set(tok[order])
    slot_gate = jnp.zeros((P,), jnp.float32).at[dest].set(gates.reshape(NK)[order])
    blk_e = jnp.minimum(jnp.searchsorted(pends, jnp.arange(nblk, dtype=pends.dtype) * E_BLOCK, side="right"),
                        N_EXPERTS - 1)
    xb = xt[slot_tok].reshape(nblk, E_BLOCK, D)

    def expert_block(args):
        xe, e = args
        gu = xe @ w_gu[e] + b_gu[e]
        g = jnp.minimum(gu[:, :D_FF], SWIGLU_LIMIT)
        u = jnp.clip(gu[:, D_FF:], -SWIGLU_LIMIT, SWIGLU_LIMIT)
        act = (u + 1) * (g * jax.nn.sigmoid(SWIGLU_ALPHA * g))
        return act @ w_down[e] + b_down[e]

    ys = lax.map(expert_block, (xb, blk_e)).reshape(P, D)
    out = jnp.zeros((N, D), ys.dtype).at[slot_tok].add(ys * slot_gate[:, None].astype(ys.dtype))
    return out.reshape(B, S, D)


def setup_inputs(seed: int = 0) -> dict:
    key = jax.random.key(seed)
    ks = jax.random.split(key, 26)
    L, D, E, F = DEPTH, D_MODEL, N_EXPERTS, D_FF

    def nrm(k, shape, scale):
        return jax.random.normal(k, shape, jnp.float32) * scale

    gate_offset = jnp.array([0.0, 3.0, 0.0, 3.0], jnp.float32)[None, :, None]
    return {
        "x": nrm(ks[0], (BATCH, SEQ, D), 1.0),
        "c": nrm(ks[1], (BATCH, D), 1.0),
        "norm_mix": 1.0 + nrm(ks[2], (L, D), 0.02),
        "norm_ffn": 1.0 + nrm(ks[3], (L, D), 0.02),
        "w_ada": nrm(ks[4], (L, D, 6 * D), 0.5 * D ** -0.5),
        "b_ada": nrm(ks[5], (L, 6 * D), 0.02),
        "w_in": nrm(ks[6], (L, D, IN_COLS), D ** -0.5),
        "b_mgates": (gate_offset + nrm(ks[7], (L, 4, M_HEADS), 0.5)).reshape(L, M_NG),
        "m_norm": 1.0 + nrm(ks[8], (L, M_HEADS, M_V), 0.02),
        "lam_q1": nrm(ks[9], (L, A_QK), 0.1),
        "lam_k1": nrm(ks[10], (L, A_QK), 0.1),
        "lam_q2": nrm(ks[11], (L, A_QK), 0.1),
        "lam_k2": nrm(ks[12], (L, A_QK), 0.1),
        "a_norm": 1.0 + nrm(ks[13], (L, A_V), 0.02),
        "w_br_m": nrm(ks[14], (L, M_VW, D), M_VW ** -0.5),
        "w_br_a": nrm(ks[15], (L, A_VW, D), A_VW ** -0.5),
        "w_out": nrm(ks[16], (L, D, D), D ** -0.5),
        "w_router": nrm(ks[17], (L, D, E), D ** -0.5),
        "b_router": nrm(ks[18], (L, E), 0.01),
        "w_gu": nrm(ks[19], (L, E, D, 2 * F), D ** -0.5),
        "b_gu": nrm(ks[20], (L, E, 2 * F), 0.02),
        "w_down": nrm(ks[21], (L, E, F, D), F ** -0.5),
        "b_down": nrm(ks[22], (L, E, D), 0.02),
        "norm_final": 1.0 + nrm(ks[23], (D,), 0.02),
    }


def reference(x, c, norm_mix, norm_ffn, w_ada, b_ada, w_in, b_mgates, m_norm,
              lam_q1, lam_k1, lam_q2, lam_k2, a_norm, w_br_m, w_br_a, w_out,
              w_router, b_router, w_gu, b_gu, w_down, b_down, norm_final):
    S = x.shape[1]
    cos, sin = _rope_tables(S)
    c_act = jax.nn.silu(c)
    for l in range(DEPTH):
        lam_init = 0.8 - 0.6 * float(np.exp(-0.3 * l))
        mod = c_act @ w_ada[l] + b_ada[l]
        sh_m, sc_m, g_m, sh_f, sc_f, g_f = jnp.split(mod, 6, axis=-1)
        h = _modulate(_rmsnorm(x, norm_mix[l]), sh_m, sc_m)
        y = _mixer(h, w_in[l], b_mgates[l], m_norm[l], lam_q1[l], lam_k1[l], lam_q2[l], lam_k2[l],
                   a_norm[l], w_br_m[l], w_br_a[l], w_out[l], lam_init, cos, sin)
        x = x + g_m[:, None, :] * y
        h = _modulate(_rmsnorm(x, norm_ffn[l]), sh_f, sc_f)
        y = _moe(h, w_router[l], b_router[l], w_gu[l], b_gu[l], w_down[l], b_down[l])
        x = x + g_f[:, None, :] * y
    return _rmsnorm(x, norm_final)
```

```python
import numpy as np
import ml_dtypes
import concourse.bass as bass
import concourse.mybir as mybir
from concourse.bass_utils import run_bass_kernel_spmd
from contextlib import ExitStack

F32 = mybir.dt.float32
BF16 = mybir.dt.bfloat16
I32 = mybir.dt.int32
U32 = mybir.dt.uint32
AF = mybir.ActivationFunctionType
ALU = mybir.AluOpType
AX = mybir.AxisListType
NPBF = ml_dtypes.bfloat16

NCORES = 8
D = 2048
S = 8192
TL = S // NCORES
DEPTH = 2
M_HEADS, M_QK, M_V = 4, 256, 512
A_HEADS, A_QK, A_V = 8, 128, 256
NE, TOPK, DFF, EBLK = 32, 4, 2048, 256
NSLOT = S * TOPK + NE * EBLK
NBLK = NSLOT // EBLK
BPC = NBLK // NCORES
EPS = 1e-6
IN_SIZES = (1024, 1024, 2048, 2048, 16, 2048, 2048, 2048, 2048, 2048)
IN_COLS = sum(IN_SIZES)
O_MQ, O_MK, O_MV, O_MO, O_MG, O_AQ, O_AK, O_AV, O_GM, O_GA = np.cumsum((0,) + IN_SIZES[:-1]).tolist()

ENGS = ["tensor", "vector", "scalar", "gpsimd", "sync"]


class Buf:
    __slots__ = ("t", "lw", "rd", "name", "root")

    def __init__(self, t, name="", root=None):
        self.t = t
        self.lw = None
        self.rd = []
        self.name = name
        self.root = root if root is not None else self

    def __getitem__(self, k):
        return self.t[k]


class Prog:
    def __init__(self, nc, es, ndma=12):
        self.nc = nc
        self.es = es
        self.q = {e: [] for e in ENGS}
        self.cnt = {}
        self.sem = {}
        self.seen = {e: {} for e in ENGS}
        for e in ENGS:
            self.sem[e] = es.enter_context(nc.semaphore("s_" + e))
            self.cnt[e] = 0
        self.dch = {}
        self.dnext = {}
        for q in ("sync", "gpsimd", "scalar"):
            self.dch[q] = []
            for i in range(ndma):
                nm = "d%s%d" % (q[0:2], i)
                self.sem[nm] = es.enter_context(nc.semaphore("s_" + nm))
                self.cnt[nm] = 0
                self.dch[q].append(nm)
            self.dnext[q] = 0
        self.nbuf = 0

    def sb(self, name, shape, dt):
        self.nbuf += 1
        return Buf(self.es.enter_context(self.nc.sbuf_tensor("%s_%d" % (name, self.nbuf), shape, dt)), name)

    def ps(self, name, shape, dt):
        self.nbuf += 1
        return Buf(self.es.enter_context(self.nc.psum_tensor("%s_%d" % (name, self.nbuf), shape, dt)), name)

    def _wait(self, eng, s, i):
        if self.seen[eng].get(s, 0) < i:
            self.seen[eng][s] = i
            sem = self.sem[s]
            self.q[eng].append(lambda e, sem=sem, i=i: e.wait_ge(sem, i))

    def _deps(self, eng, reads, writes, skip_same=False):
        deps = {}
        reads = [b.root for b in reads]
        writes = [b.root for b in writes]
        for b in reads:
            if b.lw is not None:
                s, i = b.lw
                deps[s] = max(deps.get(s, 0), i)
        for b in writes:
            if b.lw is not None:
                s, i = b.lw
                deps[s] = max(deps.get(s, 0), i)
            for s, i in b.rd:
                deps[s] = max(deps.get(s, 0), i)
        for s, i in deps.items():
            if skip_same and s == eng:
                continue
            self._wait(eng, s, i)

    def _mark(self, key, idx, reads, writes):
        reads = [b.root for b in reads]
        writes = [b.root for b in writes]
        for b in reads:
            b.rd = [(s, i) for (s, i) in b.rd if s != key] + [(key, idx)]
        for b in writes:
            b.lw = (key, idx)
            b.rd = []

    def op(self, eng, fn, reads=(), writes=()):
        self._deps(eng, reads, writes, skip_same=(eng == "tensor"))
        self.cnt[eng] += 1
        idx = self.cnt[eng]
        sem = self.sem[eng]
        self.q[eng].append(lambda e, fn=fn, sem=sem: fn(e).then_inc(sem, 1))
        self._mark(eng, idx, reads, writes)

    def dma(self, fn, reads=(), writes=(), eng="sync"):
        chs = self.dch[eng]
        ch = chs[self.dnext[eng]]
        self.dnext[eng] = (self.dnext[eng] + 1) % len(chs)
        if self.cnt[ch] > 0:
            self._wait(eng, ch, self.cnt[ch])
        self._deps(eng, reads, writes)
        self.cnt[ch] += 16
        idx = self.cnt[ch]
        sem = self.sem[ch]
        self.q[eng].append(lambda e, fn=fn, sem=sem: fn(e).then_inc(sem, 16))
        self._mark(ch, idx, reads, writes)

    def finish(self, eng="sync"):
        for s in list(self.sem.keys()):
            if self.cnt[s] > 0 and s != eng:
                self._wait(eng, s, self.cnt[s])

    def emit(self):
        with self.nc.Block() as block:
            for e in ENGS:
                if not self.q[e]:
                    continue
                lst = self.q[e]

                def body(engine, lst=lst):
                    for f in lst:
                        f(engine)
                getattr(block, e)(body)


def _new_nc():
    return bass.Bass("TRN2", target_bir_lowering=False)


def _din(nc, name, shape, dt=F32):
    return nc.dram_tensor(name, list(shape), dt, kind="ExternalInput").ap()


def _dout(nc, name, shape, dt=F32):
    return nc.dram_tensor(name, list(shape), dt, kind="ExternalOutput").ap()


def _run(nc, in_maps):
    res = run_bass_kernel_spmd(nc, in_maps, core_ids=list(range(NCORES)))
    return res.results


class RR:
    def __init__(self, items):
        self.items = list(items)
        self.i = 0

    def __call__(self):
        x = self.items[self.i]
        self.i = (self.i + 1) % len(self.items)
        return x


A_NC = 6 * D // NCORES


def build_A():
    nc = _new_nc()
    cT = _din(nc, "cT", [128, 16])
    wa = _din(nc, "wa", [DEPTH, D, A_NC])
    ba = _din(nc, "ba", [1, DEPTH * A_NC])
    mod = _dout(nc, "mod", [1, DEPTH * A_NC])
    with ExitStack() as es:
        P = Prog(nc, es)
        ct = P.sb("ct", [128, 16], F32)
        ca = P.sb("ca", [128, 16], F32)
        bat = P.sb("bat", [1, DEPTH * A_NC], F32)
        ot = P.sb("ot", [1, DEPTH * A_NC], F32)
        wt = [P.sb("wt", [128, 16, 512], F32) for _ in range(2)]
        pss = [P.ps("ps", [1, 512], F32) for _ in range(2)]
        P.dma(lambda e: e.dma_start(out=ct[:], in_=cT[:, :]), writes=[ct])
        P.dma(lambda e: e.dma_start(out=bat[:], in_=ba[:, :]), writes=[bat])
        P.op("scalar", lambda e: e.activation(out=ca[:], in_=ct[:], func=AF.Silu), reads=[ct], writes=[ca])
        it = 0
        for l in range(DEPTH):
            for n in range(A_NC // 512):
                w = wt[it % 2]
                ps = pss[it % 2]
                src = wa[l, :, n * 512:(n + 1) * 512].rearrange("(k p) n -> p k n", p=128)
                P.dma(lambda e, w=w, src=src: e.dma_start(out=w[:], in_=src), writes=[w])
                for k in range(16):
                    P.op("tensor", lambda e, w=w, ps=ps, k=k: e.matmul(ps[:], lhsT=ca[:, k:k + 1], rhs=w[:, k, :],
                                                                     start=(k == 0), stop=(k == 15)),
                         reads=[ca, w], writes=[ps])
                o0 = l * A_NC + n * 512
                P.op("vector", lambda e, ps=ps, o0=o0: e.tensor_tensor(out=ot[:, o0:o0 + 512], in0=ps[:],
                                                                      in1=bat[:, o0:o0 + 512], op=ALU.add),
                     reads=[ps, bat], writes=[ot])
                it += 1
        P.dma(lambda e: e.dma_start(out=mod[:, :], in_=ot[:]), reads=[ot])
        P.finish()
        P.emit()
    return nc


def run_A(c, w_ada, b_ada):
    nc = build_A()
    cT = np.ascontiguousarray(c.reshape(16, 128).T)
    in_maps = []
    for i in range(NCORES):
        sl = slice(i * A_NC, (i + 1) * A_NC)
        in_maps.append({"cT": cT,
                        "wa": np.ascontiguousarray(w_ada[:, :, sl]),
                        "ba": np.ascontiguousarray(b_ada[:, sl]).reshape(1, DEPTH * A_NC)})
    res = _run(nc, in_maps)
    mod = np.concatenate([r["mod"].reshape(DEPTH, A_NC) for r in res], axis=1)
    return mod


def rmsnorm_mod_tile(P, xt, wmod, shb, hb, scr, ss, rstd):
    P.op("scalar", lambda e: e.activation(out=scr[:], in_=xt[:], func=AF.Square, accum_out=ss[:]),
         reads=[xt], writes=[scr, ss])
    P.op("vector", lambda e: e.tensor_scalar(out=rstd[:], in0=ss[:], scalar1=1.0 / D, scalar2=EPS,
                                             op0=ALU.mult, op1=ALU.add), reads=[ss], writes=[rstd])
    P.op("scalar", lambda e: e.activation(out=rstd[:], in_=rstd[:], func=AF.Sqrt), reads=[rstd], writes=[rstd])
    P.op("vector", lambda e: e.reciprocal(out=rstd[:], in_=rstd[:]), reads=[rstd], writes=[rstd])
    P.op("vector", lambda e: e.scalar_tensor_tensor(out=scr[:], in0=xt[:], scalar=rstd[:, 0:1], in1=wmod[:],
                                                    op0=ALU.mult, op1=ALU.mult),
         reads=[xt, rstd, wmod], writes=[scr])
    P.op("gpsimd", lambda e: e.tensor_tensor(out=hb[:], in0=scr[:], in1=shb[:], op=ALU.add),
         reads=[scr, shb], writes=[hb])


def transpose_tile(P, src, dst, ident, ptr, evac, nchunk=16, src_sl=None, dst_sl=None):
    for g in range(0, nchunk, 8):
        pt = ptr()
        n = min(8, nchunk - g)
        for j in range(n):
            k = g + j
            sl = src_sl(k) if src_sl else slice(k * 128, (k + 1) * 128)
            P.op("tensor", lambda e, pt=pt, j=j, sl=sl: e.transpose(out=pt[:, j * 128:(j + 1) * 128], in_=src[:, sl],
                                                                   identity=ident[:]),
                 reads=[src, ident], writes=[pt])
        eng = "vector"
        dsl = dst_sl(g, n) if dst_sl else (lambda d, g=g, n=n: d[:, g:g + n, :])
        if eng == "scalar":
            P.op("scalar", lambda e, pt=pt, n=n, dsl=dsl: e.activation(out=dsl(dst), in_=pt[:, 0:n * 128].rearrange(
                "p (k t) -> p k t", t=128), func=AF.Copy), reads=[pt], writes=[dst])
        else:
            P.op(eng, lambda e, pt=pt, n=n, dsl=dsl: e.tensor_copy(out=dsl(dst), in_=pt[:, 0:n * 128].rearrange(
                "p (k t) -> p k t", t=128)), reads=[pt], writes=[dst])


def build_B():
    nc = _new_nc()
    x = _din(nc, "x", [TL, D])
    nw = _din(nc, "nw", [1, D])
    sc = _din(nc, "sc", [1, D])
    sh = _din(nc, "sh", [1, D])
    win = _din(nc, "win", [D, IN_COLS])
    bg = _din(nc, "bg", [1, 16])
    cs4 = _din(nc, "cs4", [TL, 128])
    idn = _din(nc, "idn", [128, 128], BF16)
    pb = _dout(nc, "pb", [TL, IN_COLS - 16], BF16)
    pg = _dout(nc, "pg", [TL, 16])
    NT = TL // 128
    with ExitStack() as es:
        P = Prog(nc, es)
        ident = P.sb("ident", [128, 128], BF16)
        nwb = P.sb("nwb", [128, D], F32)
        wmod = P.sb("wmod", [128, D], F32)
        shb = P.sb("shb", [128, D], F32)
        bgb = P.sb("bgb", [128, 16], F32)
        cst = P.sb("cst", [128, NT, 128], F32)
        xts = [P.sb("xt", [128, D], F32) for _ in range(2)]
        scr = P.sb("scr", [128, D], F32)
        hbs = [P.sb("hb", [128, D], BF16) for _ in range(2)]
        ss = [P.sb("ss", [128, 1], F32) for _ in range(2)]
        rstd = [P.sb("rstd", [128, 1], F32) for _ in range(2)]
        hT = [P.sb("hT", [128, 16, 128], BF16) for _ in range(NT)]
        wts = [P.sb("wt", [128, 16, 512], BF16) for _ in range(3)]
        wg = P.sb("wg", [128, 16, 16], BF16)
        ptr_l = [P.ps("ptr", [128, 1024], BF16) for _ in range(2)]
        acc_l = [P.ps("acc", [128, 512], F32) for _ in range(4)]
        stg = [P.sb("stg", [128, 512], BF16) for _ in range(3)]
        r32 = [P.sb("r32", [128, 512], F32) for _ in range(2)]
        rt = [P.sb("rt", [128, 4, 4, 16], F32) for _ in range(2)]
        sg = P.sb("sg", [128, 16], F32)
        ptr = RR(ptr_l)
        acc = RR(acc_l)
        stgr = RR(stg)
        r32r = RR(r32)
        rtr = RR(rt)
        evac = RR(["scalar", "vector"])

        P.dma(lambda e: e.dma_start(out=ident[:], in_=idn[:, :]), writes=[ident])
        P.dma(lambda e: e.dma_start(out=nwb[:], in_=nw.to_broadcast([128, D])), writes=[nwb])
        P.dma(lambda e: e.dma_start(out=wmod[:], in_=sc.to_broadcast([128, D])), writes=[wmod])
        P.dma(lambda e: e.dma_start(out=shb[:], in_=sh.to_broadcast([128, D])), writes=[shb])
        P.dma(lambda e: e.dma_start(out=bgb[:], in_=bg.to_broadcast([128, 16])), writes=[bgb])
        P.dma(lambda e: e.dma_start(out=cst[:], in_=cs4.rearrange("(t p) c -> p t c", p=128)), writes=[cst])
        P.op("vector", lambda e: e.scalar_tensor_tensor(out=wmod[:], in0=wmod[:], scalar=1.0, in1=nwb[:],
                                                        op0=ALU.add, op1=ALU.mult), reads=[wmod, nwb], writes=[wmod])
        for tt in range(NT):
            xt = xts[tt % 2]
            hb = hbs[tt % 2]
            P.dma(lambda e, xt=xt, tt=tt: e.dma_start(out=xt[:], in_=x[tt * 128:(tt + 1) * 128, :]), writes=[xt])
            rmsnorm_mod_tile(P, xt, wmod, shb, hb, scr, ss[tt % 2], rstd[tt % 2])
            transpose_tile(P, hb, hT[tt], ident, ptr, evac)

        chunks = []
        for (o, n) in zip((O_MQ, O_MK, O_MV, O_MO, O_MG, O_AQ, O_AK, O_AV, O_GM, O_GA), IN_SIZES):
            if n == 16:
                chunks.append((o, 16, "mg"))
                continue
            kind = {O_MQ: "mq", O_AQ: "aq", O_AK: "ak"}.get(o, "plain")
            for c0 in range(o, o + n, 512):
                chunks.append((c0, 512, kind))
        wi = 0
        for (c0, ncol, kind) in chunks:
            if kind == "mg":
                w = wg
            else:
                w = wts[wi % 3]
                wi += 1
            src = win[:, c0:c0 + ncol].rearrange("(k p) n -> p k n", p=128)
            P.dma(lambda e, w=w, src=src: e.dma_start(out=w[:], in_=src), writes=[w], eng="gpsimd")
            oc = c0 if c0 < O_MG else c0 - 16
            for tt in range(NT):
                a = acc()
                for k in range(16):
                    P.op("tensor", lambda e, a=a, w=w, tt=tt, k=k, ncol=ncol: e.matmul(
                        a[:, 0:ncol], lhsT=hT[tt][:, k, :], rhs=w[:, k, :], start=(k == 0), stop=(k == 15)),
                        reads=[hT[tt], w], writes=[a])
                rows = slice(tt * 128, (tt + 1) * 128)
                if kind == "mg":
                    P.op("vector", lambda e, a=a: e.tensor_tensor(out=sg[:], in0=a[:, 0:16], in1=bgb[:], op=ALU.add),
                         reads=[a, bgb], writes=[sg])
                    P.dma(lambda e, rows=rows: e.dma_start(out=pg[rows, :], in_=sg[:]), reads=[sg])
                    continue
                st = stgr()
                if kind == "plain" or kind == "mq":
                    scl = 1.0 if kind == "plain" else float(M_QK) ** -0.5
                    eng = evac()
                    if eng == "scalar":
                        P.op("scalar", lambda e, a=a, st=st, scl=scl: e.activation(out=st[:], in_=a[:], func=AF.Copy,
                                                                                 scale=scl), reads=[a], writes=[st])
                    else:
                        P.op("vector", lambda e, a=a, st=st, scl=scl: e.tensor_scalar(
                            out=st[:], in0=a[:], scalar1=scl, scalar2=None, op0=ALU.mult), reads=[a], writes=[st])
                else:
                    scl = float(A_QK) ** -0.5 if kind == "aq" else 1.0
                    r = r32r()
                    t4 = rtr()
                    P.op("scalar", lambda e, a=a, r=r, scl=scl: e.activation(out=r[:], in_=a[:], func=AF.Copy, scale=scl),
                         reads=[a], writes=[r])
                    rv = lambda r: r[:].rearrange("p (g d) -> p g d", d=128)
                    cosv = lambda tt: cst[:, tt, 0:64].rearrange("p (g j) -> p g j", j=16)
                    sinv = lambda tt: cst[:, tt, 64:128].rearrange("p (g j) -> p g j", j=16)
                    P.op("vector", lambda e, r=r, t4=t4, tt=tt: e.tensor_tensor(out=t4[:, 0], in0=rv(r)[:, :, 0:16], in1=cosv(tt), op=ALU.mult),
                         reads=[r, cst], writes=[t4])
                    P.op("vector", lambda e, r=r, t4=t4, tt=tt: e.tensor_tensor(out=t4[:, 1], in0=rv(r)[:, :, 16:32], in1=sinv(tt), op=ALU.mult),
                         reads=[r, cst], writes=[t4])
                    P.op("gpsimd", lambda e, r=r, t4=t4, tt=tt: e.tensor_tensor(out=t4[:, 2], in0=rv(r)[:, :, 0:16], in1=sinv(tt), op=ALU.mult),
                         reads=[r, cst], writes=[t4])
                    P.op("gpsimd", lambda e, r=r, t4=t4, tt=tt: e.tensor_tensor(out=t4[:, 3], in0=rv(r)[:, :, 16:32], in1=cosv(tt), op=ALU.mult),
                         reads=[r, cst], writes=[t4])
                    P.op("vector", lambda e, r=r, t4=t4: e.tensor_tensor(out=rv(r)[:, :, 0:16], in0=t4[:, 0], in1=t4[:, 1], op=ALU.subtract),
                         reads=[t4, r], writes=[r])
                    P.op("vector", lambda e, r=r, t4=t4: e.tensor_tensor(out=rv(r)[:, :, 16:32], in0=t4[:, 2], in1=t4[:, 3], op=ALU.add),
                         reads=[t4, r], writes=[r])
                    P.op("gpsimd", lambda e, r=r, st=st: e.tensor_copy(out=st[:], in_=r[:]), reads=[r], writes=[st])
                P.dma(lambda e, rows=rows, oc=oc, st=st: e.dma_start(out=pb[rows, oc:oc + 512], in_=st[:]), reads=[st])
        P.finish()
        P.emit()
    return nc


def rope_tables():
    half = A_QK // 4 // 2
    inv = (500000.0 ** (-np.arange(0, 2 * half, 2, dtype=np.float32) / np.float32(2 * half))).astype(np.float32)
    ang = np.arange(S, dtype=np.float32)[:, None] * inv[None, :]
    cos = np.cos(ang).astype(np.float32)
    sin = np.sin(ang).astype(np.float32)
    return np.concatenate([np.tile(cos, (1, 4)), np.tile(sin, (1, 4))], axis=1)


def run_B(ncB, x2d, nw, sc, sh, w_in_l, bg):
    cs4 = rope_tables()
    idn = np.eye(128, dtype=np.float32).astype(NPBF)
    in_maps = []
    for i in range(NCORES):
        rows = slice(i * TL, (i + 1) * TL)
        in_maps.append({"x": np.ascontiguousarray(x2d[rows]), "nw": nw.reshape(1, D), "sc": sc.reshape(1, D),
                        "sh": sh.reshape(1, D), "win": w_in_l, "bg": bg.reshape(1, 16),
                        "cs4": np.ascontiguousarray(cs4[rows]), "idn": idn})
    res = _run(ncB, in_maps)
    pb = np.concatenate([r["pb"] for r in res], axis=0)
    pg = np.concatenate([r["pg"] for r in res], axis=0)
    return pb, pg


def build_C1():
    nc = _new_nc()
    qT = _din(nc, "qT", [2, 128, S], BF16)
    kT = _din(nc, "kT", [2, 128, S], BF16)
    v = _din(nc, "v", [S, A_V], BF16)
    lam4 = _din(nc, "lam4", [4, A_QK])
    an = _din(nc, "an", [1, A_V])
    li = _din(nc, "li", [1, 1])
    ya = _dout(nc, "ya", [S, A_V], BF16)
    NKB = S // 128
    QT = 256
    with ExitStack() as es:
        P = Prog(nc, es)
        qs = P.sb("qs", [128, 2, S], BF16)
        ks = P.sb("ks", [128, 2, S], BF16)
        vs = P.sb("vs", [128, NKB, A_V + 1], BF16)
        lb = P.sb("lb", [128, 4, A_QK], F32)
        lt = P.sb("lt", [128, 2, A_QK], F32)
        ls = P.sb("ls", [128, 2], F32)
        lib = P.sb("lib", [128, 1], F32)
        lam = P.sb("lam", [128, 1], F32)
        nlam = P.sb("nlam", [128, 1], F32)
        anb = P.sb("anb", [128, A_V], F32)
        sps_l = [P.ps("sps", [128, 512], F32) for _ in range(4)]
        acc = [[P.ps("acc", [128, 512], F32) for _ in range(2)] for _ in range(2)]
        pts_l = [P.sb("pt", [128, 512], BF16) for _ in range(4)]
        sps = RR(sps_l)
        pts = RR(pts_l)
        rz = [P.sb("rz", [128, 2], F32) for _ in range(2)]
        o0 = [P.sb("o0", [128, A_V], F32) for _ in range(2)]
        oo = [P.sb("oo", [128, A_V], F32) for _ in range(2)]
        sq = P.sb("sq", [128, A_V], F32)
        ss = [P.sb("ss", [128, 1], F32) for _ in range(2)]
        yb = [P.sb("yb", [128, A_V], BF16) for _ in range(2)]

        for c in range(2):
            P.dma(lambda e, c=c: e.dma_start(out=qs[:, c, :], in_=qT[c]), writes=[qs])
            P.dma(lambda e, c=c: e.dma_start(out=ks[:, c, :], in_=kT[c]), writes=[ks])
        P.dma(lambda e: e.dma_start(out=vs[:, :, 0:A_V], in_=v.rearrange("(kb p) c -> p kb c", p=128)), writes=[vs])
        P.op("gpsimd", lambda e: e.memset(vs[:, :, A_V:A_V + 1], 1.0), writes=[vs])
        for i in range(4):
            P.dma(lambda e, i=i: e.dma_start(out=lb[:, i, :], in_=lam4[i:i + 1, :].to_broadcast([128, A_QK])), writes=[lb])
        P.dma(lambda e: e.dma_start(out=anb[:], in_=an.to_broadcast([128, A_V])), writes=[anb])
        P.dma(lambda e: e.dma_start(out=lib[:], in_=li.to_broadcast([128, 1])), writes=[lib])
        for i in range(2):
            P.op("vector", lambda e, i=i: e.tensor_tensor(out=lt[:, i, :], in0=lb[:, 2 * i, :], in1=lb[:, 2 * i + 1, :],
                                                          op=ALU.mult), reads=[lb], writes=[lt])
            P.op("vector", lambda e, i=i: e.reduce_sum(out=ls[:, i:i + 1], in_=lt[:, i, :], axis=AX.X), reads=[lt], writes=[ls])
        P.op("scalar", lambda e: e.activation(out=ls[:], in_=ls[:], func=AF.Exp), reads=[ls], writes=[ls])
        P.op("vector", lambda e: e.tensor_tensor(out=lam[:], in0=ls[:, 0:1], in1=ls[:, 1:2], op=ALU.subtract),
             reads=[ls], writes=[lam])
        P.op("vector", lambda e: e.tensor_tensor(out=lam[:], in0=lam[:], in1=lib[:], op=ALU.add), reads=[lam, lib], writes=[lam])
        P.op("vector", lambda e: e.tensor_scalar(out=nlam[:], in0=lam[:], scalar1=-1.0, scalar2=None, op0=ALU.mult),
             reads=[lam], writes=[nlam])
        P.op("vector", lambda e: e.tensor_scalar(out=lib[:], in0=lib[:], scalar1=-1.0, scalar2=1.0, op0=ALU.mult, op1=ALU.add),
             reads=[lib], writes=[lib])
        P.op("vector", lambda e: e.tensor_scalar(out=anb[:], in0=anb[:], scalar1=lib[:, 0:1], scalar2=None, op0=ALU.mult),
             reads=[anb, lib], writes=[anb])

        for qt in range(S // QT):
            q0 = qt * QT
            for kb in range(NKB):
                sp = sps()
                for c in range(2):
                    P.op("tensor", lambda e, sp=sp, c=c, kb=kb, q0=q0: e.matmul(
                        sp[:, c * QT:(c + 1) * QT], lhsT=ks[:, c, kb * 128:(kb + 1) * 128], rhs=qs[:, c, q0:q0 + QT],
                        start=True, stop=True), reads=[ks, qs], writes=[sp])
                pt = pts()
                P.op("scalar", lambda e, sp=sp, pt=pt: e.activation(out=pt[:], in_=sp[:], func=AF.Exp), reads=[sp], writes=[pt])
                for c in range(2):
                    for j in range(2):
                        a = acc[c][j]
                        P.op("tensor", lambda e, a=a, pt=pt, c=c, j=j, kb=kb: e.matmul(
                            a[:, 0:A_V + 1], lhsT=pt[:, c * QT + j * 128:c * QT + (j + 1) * 128], rhs=vs[:, kb, :],
                            start=(kb == 0), stop=(kb == NKB - 1)), reads=[pt, vs], writes=[a])
            for j in range(2):
                a0, a1 = acc[0][j], acc[1][j]
                r, o_0, o_, s_, y_ = rz[j], o0[j], oo[j], ss[j], yb[j]
                P.op("vector", lambda e, a0=a0, r=r: e.reciprocal(out=r[:, 0:1], in_=a0[:, A_V:A_V + 1]), reads=[a0], writes=[r])
                P.op("vector", lambda e, a1=a1, r=r: e.reciprocal(out=r[:, 1:2], in_=a1[:, A_V:A_V + 1]), reads=[a1], writes=[r])
                P.op("vector", lambda e, a0=a0, r=r, o_0=o_0: e.tensor_scalar(out=o_0[:], in0=a0[:, 0:A_V], scalar1=r[:, 0:1],
                                                                              scalar2=None, op0=ALU.mult), reads=[a0, r], writes=[o_0])
                P.op("vector", lambda e, r=r: e.tensor_tensor(out=r[:, 1:2], in0=r[:, 1:2], in1=nlam[:], op=ALU.mult),
                     reads=[r, nlam], writes=[r])
                P.op("vector", lambda e, a1=a1, r=r, o_0=o_0, o_=o_: e.scalar_tensor_tensor(
                    out=o_[:], in0=a1[:, 0:A_V], scalar=r[:, 1:2], in1=o_0[:], op0=ALU.mult, op1=ALU.add),
                    reads=[a1, r, o_0], writes=[o_])
                P.op("scalar", lambda e, o_=o_, s_=s_: e.activation(out=sq[:], in_=o_[:], func=AF.Square, accum_out=s_[:]),
                     reads=[o_], writes=[sq, s_])
                P.op("vector", lambda e, s_=s_: e.tensor_scalar(out=s_[:], in0=s_[:], scalar1=1.0 / A_V, scalar2=EPS,
                                                               op0=ALU.mult, op1=ALU.add), reads=[s_], writes=[s_])
                P.op("scalar", lambda e, s_=s_: e.activation(out=s_[:], in_=s_[:], func=AF.Sqrt), reads=[s_], writes=[s_])
                P.op("vector", lambda e, s_=s_: e.reciprocal(out=s_[:], in_=s_[:]), reads=[s_], writes=[s_])
                P.op("vector", lambda e, o_=o_, s_=s_, y_=y_: e.scalar_tensor_tensor(
                    out=y_[:], in0=o_[:], scalar=s_[:, 0:1], in1=anb[:], op0=ALU.mult, op1=ALU.mult),
                    reads=[o_, s_, anb], writes=[y_])
                r0 = q0 + j * 128
                P.dma(lambda e, y_=y_, r0=r0: e.dma_start(out=ya[r0:r0 + 128, :], in_=y_[:]), reads=[y_])
        P.finish()
        P.emit()
    return nc


def run_C1(ncC1, pb, lam4, a_norm_l, lam_init):
    aq = pb[:, O_AQ - 16:O_AQ - 16 + 2048].reshape(S, A_HEADS, 2, A_QK)
    ak = pb[:, O_AK - 16:O_AK - 16 + 2048].reshape(S, A_HEADS, 2, A_QK)
    av = pb[:, O_AV - 16:O_AV - 16 + 2048].reshape(S, A_HEADS, A_V)
    in_maps = []
    for h in range(NCORES):
        in_maps.append({"qT": np.ascontiguousarray(aq[:, h].transpose(1, 2, 0)),
                        "kT": np.ascontiguousarray(ak[:, h].transpose(1, 2, 0)),
                        "v": np.ascontiguousarray(av[:, h]),
                        "lam4": lam4, "an": a_norm_l.reshape(1, A_V),
                        "li": np.full((1, 1), lam_init, np.float32)})
    res = _run(ncC1, in_maps)
    return np.concatenate([r["ya"] for r in res], axis=1)


def View(ap, name="", root=None):
    return Buf(ap, name, root)


def build_C2(nch=None, dbg=False):
    nc = _new_nc()
    qT = _din(nc, "qT", [M_QK, S], BF16)
    kT = _din(nc, "kT", [M_QK, S], BF16)
    kk = _din(nc, "kk", [S, M_QK], BF16)
    vv = _din(nc, "vv", [S, M_V], BF16)
    gi = _din(nc, "gi", [64, 128])
    gf = _din(nc, "gf", [64, 128])
    tri = _din(nc, "tri", [128, 128])
    cst = _din(nc, "cst", [64, 128])
    hd = _dout(nc, "hd", [S, M_V])
    gscr = _dout(nc, "gscr", [64, 128])
    NCH = S // 128
    dbgo = _dout(nc, "dbgo", [128, 4 * 64]) if dbg else None
    with ExitStack() as es:
        P = Prog(nc, es)
        g_i = P.sb("g_i", [64, 128], F32)
        g_f = P.sb("g_f", [64, 128], F32)
        Bw = P.sb("Bw", [64, 128], F32)
        Aw = P.sb("Aw", [64, 128], F32)
        o64 = P.sb("o64", [64, 128], F32)
        cs = P.sb("cs", [64, 128], F32)
        c1 = P.sb("c1", [64, 4], F32)
        rowM = P.sb("rowM", [1, 64], F32)
        r_gl = P.sb("r_gl", [1, NCH], F32)
        r_gp = P.sb("r_gp", [1, NCH], F32)
        r_G = P.sb("r_G", [1, S], F32)
        r_1 = P.sb("r_1", [1, 128], F32)
        one11 = P.sb("one11", [1, 1], F32)
        trim = P.sb("trim", [128, 128], F32)
        onesb = P.sb("onesb", [128, 1], BF16)
        A_col = P.sb("A_col", [128, NCH], F32)
        E_col = P.sb("E_col", [128, NCH], F32)
        GLB = P.sb("GLB", [128, NCH], F32)
        GPB = P.sb("GPB", [128, NCH], F32)
        WS = P.sb("WS", [128, NCH], F32)
        DEC = P.sb("DEC", [128, NCH], F32)
        Cst = [P.sb("C", [128, M_V], F32) for _ in range(2)]
        Cb = [P.sb("Cb", [128, M_V], BF16) for _ in range(2)]
        nst = P.sb("n", [128, 2], F32)
        nb = P.sb("nb", [128, 2], BF16)
        bk = [P.ps("bk", [128, 512], F32) for _ in range(4)]
        sT_l = [View(bk[i][:, 0:128], root=bk[i]) for i in range(2)]
        GmB_l = [View(bk[i][:, 128:256], root=bk[i]) for i in range(2)]
        Dn_l = [View(bk[2 + i][:, 0:1], root=bk[2 + i]) for i in range(2)]
        nS_l = [View(bk[2 + i][:, 2:4], root=bk[2 + i]) for i in range(2)]
        pc1 = View(bk[2][0:64, 4:5], root=bk[2])
        pc2 = View(bk[2][0:64, 5:6], root=bk[2])
        prow = View(bk[2][0:1, 64:128], root=bk[2])
        pcol = [View(bk[i][:, 256:320], root=bk[i]) for i in range(4)]
        N_l = [P.ps("N", [128, M_V], F32) for _ in range(2)]
        KV = [P.ps("KV", [128, M_V], F32) for _ in range(2)]
        kTc_l = [P.sb("kTc", [128, 2, 128], BF16) for _ in range(3)]
        qTc_l = [P.sb("qTc", [128, 2, 128], BF16) for _ in range(3)]
        kc_l = [P.sb("kc", [128, M_QK], BF16) for _ in range(3)]
        vc_l = [P.sb("vc", [128, M_V], BF16) for _ in range(3)]
        Dt_l = [P.sb("Dt", [128, 128], F32) for _ in range(2)]
        W_l = [P.sb("W", [128, 128], F32) for _ in range(2)]
        WI_l = [P.sb("WI", [128, 128], F32) for _ in range(2)]
        sw_l = [P.sb("sw", [128, 128], BF16) for _ in range(2)]
        qtl_l = [P.sb("qtl", [128, 2, 128], BF16) for _ in range(2)]
        ktl_l = [P.sb("ktl", [128, M_QK], BF16) for _ in range(2)]
        den_l = [P.sb("den", [128, 1], F32) for _ in range(2)]
        ho_l = [P.sb("ho", [128, M_V], F32) for _ in range(3)]

        P.dma(lambda e: e.dma_start(out=g_i[:], in_=gi[:, :]), writes=[g_i])
        P.dma(lambda e: e.dma_start(out=g_f[:], in_=gf[:, :]), writes=[g_f])
        P.dma(lambda e: e.dma_start(out=trim[:], in_=tri[:, :]), writes=[trim])
        P.dma(lambda e: e.dma_start(out=cs[:], in_=cst[:, :]), writes=[cs])
        P.op("gpsimd", lambda e: e.memset(r_1[:], 1.0), writes=[r_1])
        P.op("gpsimd", lambda e: e.memset(o64[:], 1.0), writes=[o64])
        P.op("gpsimd", lambda e: e.memset(one11[:], 1.0), writes=[one11])
        P.op("gpsimd", lambda e: e.memset(onesb[:], 1.0), writes=[onesb])
        for j in range(2):
            P.op("gpsimd", lambda e, j=j: e.memset(Cst[j][:], 0.0), writes=[Cst[j]])
            P.op("gpsimd", lambda e, j=j: e.memset(Cb[j][:], 0.0), writes=[Cb[j]])
        P.op("gpsimd", lambda e: e.memset(nst[:], 0.0), writes=[nst])
        P.op("gpsimd", lambda e: e.memset(nb[:], 0.0), writes=[nb])
        P.op("scalar", lambda e: e.activation(out=g_f[:], in_=g_f[:], func=AF.Exp, scale=-1.0), reads=[g_f], writes=[g_f])
        P.op("vector", lambda e: e.tensor_scalar(out=g_f[:], in0=g_f[:], scalar1=1.0, scalar2=None, op0=ALU.add),
             reads=[g_f], writes=[g_f])
        P.op("scalar", lambda e: e.activation(out=g_f[:], in_=g_f[:], func=AF.Ln), reads=[g_f], writes=[g_f])
        P.op("vector", lambda e: e.tensor_scalar(out=g_f[:], in0=g_f[:], scalar1=-1.0, scalar2=None, op0=ALU.mult),
             reads=[g_f], writes=[g_f])
        P.op("vector", lambda e: e.tensor_tensor_scan(out=Bw[:], data0=o64[:], data1=g_f[:], initial=0.0,
                                                      op0=ALU.mult, op1=ALU.add), reads=[o64, g_f], writes=[Bw])
        P.op("vector", lambda e: e.tensor_copy(out=c1[:, 0:1], in_=Bw[:, 127:128]), reads=[Bw], writes=[c1])
        P.op("tensor", lambda e: e.matmul(pc1[:], lhsT=cs[:, 64:128], rhs=c1[:, 0:1], start=True, stop=True),
             reads=[cs, c1], writes=[pc1])
        P.op("vector", lambda e: e.tensor_copy(out=c1[:, 1:2], in_=pc1[:]), reads=[pc1], writes=[c1])
        P.op("vector", lambda e: e.tensor_scalar(out=Bw[:], in0=Bw[:], scalar1=c1[:, 1:2], scalar2=None, op0=ALU.add),
             reads=[Bw, c1], writes=[Bw])
        P.op("vector", lambda e: e.tensor_tensor(out=g_i[:], in0=g_i[:], in1=Bw[:], op=ALU.subtract),
             reads=[g_i, Bw], writes=[g_i])
        P.op("vector", lambda e: e.tensor_tensor_scan(out=Aw[:], data0=g_i[:], data1=g_i[:], initial=-1.0e30,
                                                      op0=ALU.max, op1=ALU.max), reads=[g_i], writes=[Aw])
        P.op("vector", lambda e: e.tensor_copy(out=c1[:, 2:3], in_=Aw[:, 127:128]), reads=[Aw], writes=[c1])
        P.op("tensor", lambda e: e.matmul(prow[:], lhsT=c1[:, 2:3], rhs=cs[:, 0:64], start=True, stop=True),
             reads=[cs, c1], writes=[prow])
        P.op("vector", lambda e: e.tensor_copy(out=rowM[:], in_=prow[:]), reads=[prow], writes=[rowM])
        P.op("vector", lambda e: e.tensor_tensor_scan(out=r_gl[:], data0=rowM[:], data1=rowM[:], initial=0.0,
                                                      op0=ALU.max, op1=ALU.max), reads=[rowM], writes=[r_gl])
        P.op("gpsimd", lambda e: e.memset(r_gp[:, 0:1], 0.0), writes=[r_gp])
        P.op("vector", lambda e: e.tensor_copy(out=r_gp[:, 1:NCH], in_=r_gl[:, 0:NCH - 1]), reads=[r_gl, r_gp], writes=[r_gp])
        P.op("tensor", lambda e: e.matmul(pc2[:], lhsT=r_gp[0:1, :], rhs=one11[0:1, 0:1], start=True, stop=True),
             reads=[r_gp, one11], writes=[pc2])
        P.op("vector", lambda e: e.tensor_copy(out=c1[:, 3:4], in_=pc2[:]), reads=[pc2], writes=[c1])
        P.op("vector", lambda e: e.tensor_scalar(out=Aw[:], in0=Aw[:], scalar1=c1[:, 3:4], scalar2=None, op0=ALU.max),
             reads=[Aw, c1], writes=[Aw])
        P.op("vector", lambda e: e.tensor_tensor(out=Bw[:], in0=Bw[:], in1=Aw[:], op=ALU.add), reads=[Bw, Aw], writes=[Bw])
        P.op("scalar", lambda e: e.activation(out=Bw[:], in_=Bw[:], func=AF.Exp, scale=-1.0), reads=[Bw], writes=[Bw])
        gs = View(gscr)
        P.dma(lambda e: e.dma_start(out=gscr[:, :], in_=Aw[:]), reads=[Aw], writes=[gs])
        P.dma(lambda e: e.dma_start(out=r_G[:], in_=gscr.rearrange("(o c) t -> o (c t)", o=1)), reads=[gs], writes=[r_G])
        for (src, col, pc) in ((g_i, A_col, pcol[0]), (Bw, E_col, pcol[1])):
            P.op("tensor", lambda e, src=src, pc=pc: e.transpose(out=pc[:], in_=src[:], identity=cs[:, 0:64]),
                 reads=[src, cs], writes=[pc])
            P.op("vector", lambda e, col=col, pc=pc: e.tensor_copy(out=col[:], in_=pc[:]), reads=[pc], writes=[col])
        for (row, col, pc) in ((r_gl, GLB, pcol[2]), (r_gp, GPB, pcol[3])):
            P.op("tensor", lambda e, row=row, pc=pc: e.matmul(pc[:], lhsT=r_1[0:1, 0:128], rhs=row[0:1, :], start=True, stop=True),
                 reads=[row, r_1], writes=[pc])
            P.op("vector", lambda e, col=col, pc=pc: e.tensor_copy(out=col[:], in_=pc[:]), reads=[pc], writes=[col])
        P.op("vector", lambda e: e.tensor_tensor(out=WS[:], in0=A_col[:], in1=GLB[:], op=ALU.subtract), reads=[A_col, GLB], writes=[WS])
        P.op("scalar", lambda e: e.activation(out=WS[:], in_=WS[:], func=AF.Exp), reads=[WS], writes=[WS])
        P.op("vector", lambda e: e.tensor_tensor(out=DEC[:], in0=GPB[:], in1=GLB[:], op=ALU.subtract), reads=[GPB, GLB], writes=[DEC])
        P.op("scalar", lambda e: e.activation(out=DEC[:], in_=DEC[:], func=AF.Exp), reads=[DEC], writes=[DEC])

        if dbg:
            for i, col in enumerate((A_col, E_col, GLB, GPB)):
                P.dma(lambda e, i=i, col=col: e.dma_start(out=dbgo[:, i * 64:(i + 1) * 64], in_=col[:]), reads=[col])
        for c in range(NCH if nch is None else nch):
            t0 = c * 128
            kTc, qTc, kc, vc = kTc_l[c % 3], qTc_l[c % 3], kc_l[c % 3], vc_l[c % 3]
            P.dma(lambda e, kTc=kTc, t0=t0: e.dma_start(out=kTc[:], in_=kT[:, t0:t0 + 128].rearrange("(j p) t -> p j t", p=128)), writes=[kTc])
            P.dma(lambda e, qTc=qTc, t0=t0: e.dma_start(out=qTc[:], in_=qT[:, t0:t0 + 128].rearrange("(j p) t -> p j t", p=128)), writes=[qTc])
            P.dma(lambda e, kc=kc, t0=t0: e.dma_start(out=kc[:], in_=kk[t0:t0 + 128, :]), writes=[kc])
            P.dma(lambda e, vc=vc, t0=t0: e.dma_start(out=vc[:], in_=vv[t0:t0 + 128, :]), writes=[vc])
            sT, GmB, Dn, Np, nS = sT_l[c % 2], GmB_l[c % 2], Dn_l[c % 2], N_l[c % 2], nS_l[c % 2]
            Dt, W, WI, sw, qtl, ktl, den, ho = Dt_l[c % 2], W_l[c % 2], WI_l[c % 2], sw_l[c % 2], qtl_l[c % 2], ktl_l[c % 2], den_l[c % 2], ho_l[c % 3]
            for j in range(2):
                P.op("tensor", lambda e, sT=sT, kTc=kTc, qTc=qTc, j=j: e.matmul(sT[:], lhsT=kTc[:, j, :], rhs=qTc[:, j, :],
                                                                               start=(j == 0), stop=(j == 1)),
                     reads=[kTc, qTc], writes=[sT])
            P.op("tensor", lambda e, GmB=GmB, t0=t0: e.matmul(GmB[:], lhsT=r_1[0:1, 0:128], rhs=r_G[0:1, t0:t0 + 128],
                                                              start=True, stop=True), reads=[r_1, r_G], writes=[GmB])
            P.op("vector", lambda e, Dt=Dt, GmB=GmB, c=c: e.tensor_scalar(out=Dt[:], in0=GmB[:], scalar1=A_col[:, c:c + 1], scalar2=0.0,
                                                                          op0=ALU.subtract, op1=ALU.max), reads=[GmB, A_col], writes=[Dt])
            P.op("scalar", lambda e, Dt=Dt, W=W: e.activation(out=W[:], in_=Dt[:], func=AF.Exp, scale=-1.0), reads=[Dt], writes=[W])
            P.op("gpsimd", lambda e, W=W: e.tensor_tensor(out=W[:], in0=W[:], in1=trim[:], op=ALU.mult), reads=[W, trim], writes=[W])
            P.op("vector", lambda e, sw=sw, sT=sT, W=W: e.tensor_tensor(out=sw[:], in0=sT[:], in1=W[:], op=ALU.mult),
                 reads=[sT, W], writes=[sw])
            P.op("scalar", lambda e, WI=WI, GmB=GmB, c=c: e.activation(out=WI[:], in_=GmB[:], func=AF.Exp, scale=-1.0,
                                                                      bias=GPB[:, c:c + 1]), reads=[GmB, GPB], writes=[WI])
            for j in range(2):
                P.op("gpsimd" if j == 0 else "vector", lambda e, qtl=qtl, qTc=qTc, WI=WI, j=j: e.tensor_tensor(
                    out=qtl[:, j, :], in0=qTc[:, j, :], in1=WI[:], op=ALU.mult), reads=[qTc, WI], writes=[qtl])
            P.op("tensor", lambda e, Np=Np, sw=sw, vc=vc: e.matmul(Np[:], lhsT=sw[:], rhs=vc[:], start=True, stop=False),
                 reads=[sw, vc], writes=[Np])
            for j in range(2):
                P.op("tensor", lambda e, Np=Np, qtl=qtl, j=j: e.matmul(Np[:], lhsT=qtl[:, j, :], rhs=Cb[j][:], start=False, stop=(j == 1)),
                     reads=[qtl, Cb[j]], writes=[Np])
            P.op("tensor", lambda e, Dn=Dn, sw=sw: e.matmul(Dn[:], lhsT=sw[:], rhs=onesb[:], start=True, stop=False),
                 reads=[sw, onesb], writes=[Dn])
            for j in range(2):
                P.op("tensor", lambda e, Dn=Dn, qtl=qtl, j=j: e.matmul(Dn[:], lhsT=qtl[:, j, :], rhs=nb[:, j:j + 1], start=False, stop=(j == 1)),
                     reads=[qtl, nb], writes=[Dn])
            P.op("scalar", lambda e, den=den, Dn=Dn: e.activation(out=den[:], in_=Dn[:], func=AF.Abs), reads=[Dn], writes=[den])
            P.op("vector", lambda e, den=den, c=c: e.tensor_scalar(out=den[:], in0=den[:], scalar1=E_col[:, c:c + 1], scalar2=None,
                                                                   op0=ALU.max), reads=[den, E_col], writes=[den])
            P.op("vector", lambda e, den=den: e.reciprocal(out=den[:], in_=den[:]), reads=[den], writes=[den])
            P.op("scalar", lambda e, ho=ho, Np=Np, den=den: e.activation(out=ho[:], in_=Np[:], func=AF.Copy, scale=den[:, 0:1]),
                 reads=[Np, den], writes=[ho])
            P.dma(lambda e, ho=ho, t0=t0: e.dma_start(out=hd[t0:t0 + 128, :], in_=ho[:]), reads=[ho])
            P.op("gpsimd", lambda e, ktl=ktl, kc=kc, c=c: e.tensor_scalar(out=ktl[:], in0=kc[:], scalar1=WS[:, c:c + 1], scalar2=None,
                                                                          op0=ALU.mult), reads=[kc, WS], writes=[ktl])
            for j in range(2):
                P.op("tensor", lambda e, ktl=ktl, vc=vc, j=j: e.matmul(KV[j][:], lhsT=ktl[:, j * 128:(j + 1) * 128], rhs=vc[:],
                                                                      start=True, stop=True), reads=[ktl, vc], writes=[KV[j]])
            for j in range(2):
                P.op("tensor", lambda e, ktl=ktl, j=j, nS=nS: e.matmul(nS[:, j:j + 1], lhsT=ktl[:, j * 128:(j + 1) * 128], rhs=onesb[:],
                                                               start=True, stop=True), reads=[ktl, onesb], writes=[nS])
            for j in range(2):
                P.op("vector", lambda e, j=j, c=c: e.scalar_tensor_tensor(out=Cst[j][:], in0=Cst[j][:], scalar=DEC[:, c:c + 1], in1=KV[j][:],
                                                                          op0=ALU.mult, op1=ALU.add), reads=[Cst[j], DEC, KV[j]], writes=[Cst[j]])
                P.op("scalar", lambda e, j=j: e.activation(out=Cb[j][:], in_=Cst[j][:], func=AF.Copy), reads=[Cst[j]], writes=[Cb[j]])
            P.op("vector", lambda e, c=c, nS=nS: e.scalar_tensor_tensor(out=nst[:], in0=nst[:], scalar=DEC[:, c:c + 1], in1=nS[:],
                                                                 op0=ALU.mult, op1=ALU.add), reads=[nst, DEC, nS], writes=[nst])
            P.op("vector", lambda e: e.tensor_copy(out=nb[:], in_=nst[:]), reads=[nst], writes=[nb])
        P.finish()
        P.emit()
    return nc


def run_C2(ncC2, pb, pg):
    mq = pb[:, O_MQ:O_MQ + 1024].reshape(S, M_HEADS, M_QK)
    mk = pb[:, O_MK:O_MK + 1024].reshape(S, M_HEADS, M_QK)
    mv = pb[:, O_MV:O_MV + 2048].reshape(S, M_HEADS, M_V)
    g = pg.reshape(S, 4, M_HEADS)
    tri = np.triu(np.ones((128, 128), np.float32))
    cst = np.concatenate([np.eye(64, dtype=np.float32), np.triu(np.ones((64, 64), np.float32), 1)], axis=1)
    in_maps = []
    for core in range(NCORES):
        h, d = core // 2, core % 2
        fl = (lambda a: a[::-1]) if d == 1 else (lambda a: a)
        in_maps.append({"qT": np.ascontiguousarray(fl(mq[:, h]).T), "kT": np.ascontiguousarray(fl(mk[:, h]).T),
                        "kk": np.ascontiguousarray(fl(mk[:, h])), "vv": np.ascontiguousarray(fl(mv[:, h])),
                        "gi": np.ascontiguousarray(fl(g[:, 2 * d, h])).reshape(64, 128),
                        "gf": np.ascontiguousarray(fl(g[:, 2 * d + 1, h])).reshape(64, 128), "tri": tri, "cst": cst})
    res = _run(ncC2, in_maps)
    hfw = np.concatenate([res[2 * h]["hd"] for h in range(M_HEADS)], axis=1)
    hbw = np.concatenate([res[2 * h + 1]["hd"][::-1] for h in range(M_HEADS)], axis=1)
    return hfw, np.ascontiguousarray(hbw)


def build_D():
    nc = _new_nc()
    x = _din(nc, "x", [TL, D])
    hfw = _din(nc, "hfw", [TL, D])
    hbw = _din(nc, "hbw", [TL, D])
    ya = _din(nc, "ya", [TL, D], BF16)
    mo = _din(nc, "mo", [TL, D], BF16)
    gmi = _din(nc, "gm", [TL, D], BF16)
    gai = _din(nc, "ga", [TL, D], BF16)
    vecs = _din(nc, "vecs", [5, D])
    wbm = _din(nc, "wbm", [D, D])
    wba = _din(nc, "wba", [D, D])
    wo = _din(nc, "wo", [D, D])
    wr = _din(nc, "wr", [D, NE])
    br = _din(nc, "br", [1, NE])
    idn = _din(nc, "idn", [128, 128], BF16)
    idf = _din(nc, "idf", [128, 128])
    x1o = _dout(nc, "x1", [TL, D])
    h2o = _dout(nc, "h2", [TL, D], BF16)
    Go = _dout(nc, "G", [TL, NE])
    GT = 2
    NG = TL // 128 // GT
    with ExitStack() as es:
        P = Prog(nc, es)
        ident = P.sb("ident", [128, 128], BF16)
        identf = P.sb("identf", [128, 128], F32)
        mnb = P.sb("mnb", [128, D], F32)
        gmb = P.sb("gmb", [128, D], F32)
        wmod = P.sb("wmod", [128, D], F32)
        shb = P.sb("shb", [128, D], F32)
        wrs = P.sb("wrs", [128, 16, NE], F32)
        brb = P.sb("brb", [128, NE], F32)
        tA = [P.sb("tA", [128, D], F32)] * 2
        tB = [P.sb("tB", [128, D], F32)] * 2
        tC = P.sb("tC", [128, D], F32)
        mot = [P.sb("mot", [128, D], BF16)] * 2
        yat = [P.sb("yat", [128, D], BF16)] * 2
        ymb = [P.sb("ymb", [128, D], BF16) for _ in range(2)]
        ss4 = [P.sb("ss4", [128, 4], F32) for _ in range(2)]
        ymT = [P.sb("ymT", [128, 16, 128], BF16) for _ in range(GT)]
        yaT = [P.sb("yaT", [128, 16, 128], BF16) for _ in range(GT)]
        mrg = [P.sb("mrg", [128, D], BF16) for _ in range(GT)]
        xt = [P.sb("xt", [128, D], F32) for _ in range(GT)]
        wts = [P.sb("wt", [128, 16, 512], BF16) for _ in range(3)]
        gch = [P.sb("gch", [128, 512], BF16) for _ in range(4)]
        sgc = [P.sb("sgc", [128, 512], F32) for _ in range(4)]
        t12 = [P.sb("t12", [128, 512], F32) for _ in range(4)]
        ssr = [P.sb("ssr", [128, 1], F32) for _ in range(2)]
        rstd = [P.sb("rstd", [128, 1], F32) for _ in range(2)]
        lg = [P.sb("lg", [128, NE], F32) for _ in range(2)]
        mx8 = [P.sb("mx8", [128, 8], F32) for _ in range(2)]
        msk = [P.sb("msk", [128, NE], F32) for _ in range(2)]
        ex = [P.sb("ex", [128, NE], F32) for _ in range(2)]
        zz = [P.sb("zz", [128, 2], F32) for _ in range(2)]
        ptr_l = [P.ps("ptr", [128, 1024], BF16) for _ in range(2)]
        acc_l = [P.ps("acc", [128, 512], F32) for _ in range(4)]
        ptf_l = [P.ps("ptf", [128, 512], F32) for _ in range(2)]
        ptr, acc, ptf = RR(ptr_l), RR(acc_l), RR(ptf_l)
        wtr, gchr, sgcr, t12r = RR(wts), RR(gch), RR(sgc), RR(t12)
        evac = RR(["vector"])

        P.dma(lambda e: e.dma_start(out=ident[:], in_=idn[:, :]), writes=[ident])
        P.dma(lambda e: e.dma_start(out=identf[:], in_=idf[:, :]), writes=[identf])
        P.dma(lambda e: e.dma_start(out=mnb[:], in_=vecs[0:1, :].to_broadcast([128, D])), writes=[mnb])
        P.dma(lambda e: e.dma_start(out=gmb[:], in_=vecs[1:2, :].to_broadcast([128, D])), writes=[gmb])
        P.dma(lambda e: e.dma_start(out=tC[:], in_=vecs[2:3, :].to_broadcast([128, D])), writes=[tC])
        P.dma(lambda e: e.dma_start(out=wmod[:], in_=vecs[3:4, :].to_broadcast([128, D])), writes=[wmod])
        P.dma(lambda e: e.dma_start(out=shb[:], in_=vecs[4:5, :].to_broadcast([128, D])), writes=[shb])
        P.dma(lambda e: e.dma_start(out=wrs[:], in_=wr.rearrange("(k p) n -> p k n", p=128)), writes=[wrs])
        P.dma(lambda e: e.dma_start(out=brb[:], in_=br.to_broadcast([128, NE])), writes=[brb])
        P.op("vector", lambda e: e.scalar_tensor_tensor(out=wmod[:], in0=wmod[:], scalar=1.0, in1=tC[:],
                                                        op0=ALU.add, op1=ALU.mult), reads=[wmod, tC], writes=[wmod])

        for g in range(NG):
            for i in range(GT):
                r0 = (g * GT + i) * 128
                rows = slice(r0, r0 + 128)
                a, b, m_, y_, yb_, s4 = tA[i % 2], tB[i % 2], mot[i % 2], yat[i % 2], ymb[i % 2], ss4[i % 2]
                P.dma(lambda e, a=a, rows=rows: e.dma_start(out=a[:], in_=hfw[rows, :]), writes=[a])
                P.dma(lambda e, b=b, rows=rows: e.dma_start(out=b[:], in_=hbw[rows, :]), writes=[b])
                P.dma(lambda e, m_=m_, rows=rows: e.dma_start(out=m_[:], in_=mo[rows, :]), writes=[m_])
                P.dma(lambda e, y_=y_, rows=rows: e.dma_start(out=y_[:], in_=ya[rows, :]), writes=[y_])
                P.dma(lambda e, i=i, rows=rows: e.dma_start(out=xt[i][:], in_=x[rows, :]), writes=[xt[i]])
                P.op("gpsimd", lambda e, a=a, b=b: e.tensor_tensor(out=a[:], in0=a[:], in1=b[:], op=ALU.add), reads=[a, b], writes=[a])
                for h in range(M_HEADS):
                    hs = slice(h * M_V, (h + 1) * M_V)
                    P.op("scalar", lambda e, a=a, b=b, s4=s4, h=h, hs=hs: e.activation(out=b[:, hs], in_=a[:, hs], func=AF.Square,
                                                                                     accum_out=s4[:, h:h + 1]), reads=[a], writes=[b, s4])
                P.op("vector", lambda e, s4=s4: e.tensor_scalar(out=s4[:], in0=s4[:], scalar1=1.0 / M_V, scalar2=EPS, op0=ALU.mult, op1=ALU.add),
                     reads=[s4], writes=[s4])
                P.op("scalar", lambda e, s4=s4: e.activation(out=s4[:], in_=s4[:], func=AF.Sqrt), reads=[s4], writes=[s4])
                P.op("vector", lambda e, s4=s4: e.reciprocal(out=s4[:], in_=s4[:]), reads=[s4], writes=[s4])
                for h in range(M_HEADS):
                    hs = slice(h * M_V, (h + 1) * M_V)
                    P.op("vector", lambda e, a=a, s4=s4, h=h, hs=hs: e.scalar_tensor_tensor(
                        out=a[:, hs], in0=a[:, hs], scalar=s4[:, h:h + 1], in1=mnb[:, hs], op0=ALU.mult, op1=ALU.mult),
                        reads=[a, s4, mnb], writes=[a])
                P.op("scalar", lambda e, m_=m_: e.activation(out=tC[:], in_=m_[:], func=AF.Sigmoid), reads=[m_], writes=[tC])
                P.op("gpsimd", lambda e, a=a, yb_=yb_: e.tensor_tensor(out=yb_[:], in0=a[:], in1=tC[:], op=ALU.mult), reads=[a, tC], writes=[yb_])
                transpose_tile(P, yb_, ymT[i], ident, ptr, evac)
                transpose_tile(P, y_, yaT[i], ident, ptr, evac)
            for n in range(4):
                cols = slice(n * 512, (n + 1) * 512)
                w1, w2 = wtr(), wtr()
                P.dma(lambda e, w1=w1, cols=cols: e.dma_start(out=w1[:], in_=wbm[:, cols].rearrange("(k p) n -> p k n", p=128)),
                      writes=[w1], eng="gpsimd")
                P.dma(lambda e, w2=w2, cols=cols: e.dma_start(out=w2[:], in_=wba[:, cols].rearrange("(k p) n -> p k n", p=128)),
                      writes=[w2], eng="gpsimd")
                for i in range(GT):
                    r0 = (g * GT + i) * 128
                    rows = slice(r0, r0 + 128)
                    a1, a2 = acc(), acc()
                    for k in range(16):
                        P.op("tensor", lambda e, a1=a1, w1=w1, i=i, k=k: e.matmul(a1[:], lhsT=ymT[i][:, k, :], rhs=w1[:, k, :],
                                                                                start=(k == 0), stop=(k == 15)), reads=[ymT[i], w1], writes=[a1])
                    for k in range(16):
                        P.op("tensor", lambda e, a2=a2, w2=w2, i=i, k=k: e.matmul(a2[:], lhsT=yaT[i][:, k, :], rhs=w2[:, k, :],
                                                                                start=(k == 0), stop=(k == 15)), reads=[yaT[i], w2], writes=[a2])
                    g1, g2, s1, s2, t1, t2 = gchr(), gchr(), sgcr(), sgcr(), t12r(), t12r()
                    P.dma(lambda e, g1=g1, rows=rows, cols=cols: e.dma_start(out=g1[:], in_=gmi[rows, cols]), writes=[g1])
                    P.dma(lambda e, g2=g2, rows=rows, cols=cols: e.dma_start(out=g2[:], in_=gai[rows, cols]), writes=[g2])
                    P.op("scalar", lambda e, g1=g1, s1=s1: e.activation(out=s1[:], in_=g1[:], func=AF.Sigmoid), reads=[g1], writes=[s1])
                    P.op("scalar", lambda e, g2=g2, s2=s2: e.activation(out=s2[:], in_=g2[:], func=AF.Sigmoid), reads=[g2], writes=[s2])
                    P.op("vector", lambda e, a1=a1, s1=s1, t1=t1: e.tensor_tensor(out=t1[:], in0=a1[:], in1=s1[:], op=ALU.mult), reads=[a1, s1], writes=[t1])
                    P.op("vector", lambda e, a2=a2, s2=s2, t2=t2: e.tensor_tensor(out=t2[:], in0=a2[:], in1=s2[:], op=ALU.mult), reads=[a2, s2], writes=[t2])
                    P.op("gpsimd", lambda e, t1=t1, t2=t2, i=i, cols=cols: e.tensor_tensor(out=mrg[i][:, cols], in0=t1[:], in1=t2[:], op=ALU.add),
                         reads=[t1, t2], writes=[mrg[i]])
            for i in range(GT):
                transpose_tile(P, mrg[i], ymT[i], ident, ptr, evac)
            for n in range(4):
                cols = slice(n * 512, (n + 1) * 512)
                w1 = wtr()
                P.dma(lambda e, w1=w1, cols=cols: e.dma_start(out=w1[:], in_=wo[:, cols].rearrange("(k p) n -> p k n", p=128)),
                      writes=[w1], eng="gpsimd")
                for i in range(GT):
                    a1 = acc()
                    for k in range(16):
                        P.op("tensor", lambda e, a1=a1, w1=w1, i=i, k=k: e.matmul(a1[:], lhsT=ymT[i][:, k, :], rhs=w1[:, k, :],
                                                                                start=(k == 0), stop=(k == 15)), reads=[ymT[i], w1], writes=[a1])
                    t1 = t12r()
                    P.op("vector", lambda e, a1=a1, t1=t1, cols=cols: e.tensor_tensor(out=t1[:], in0=a1[:], in1=gmb[:, cols], op=ALU.mult),
                         reads=[a1, gmb], writes=[t1])
                    P.op("gpsimd", lambda e, t1=t1, i=i, cols=cols: e.tensor_tensor(out=xt[i][:, cols], in0=xt[i][:, cols], in1=t1[:], op=ALU.add),
                         reads=[t1, xt[i]], writes=[xt[i]])
            for i in range(GT):
                r0 = (g * GT + i) * 128
                rows = slice(r0, r0 + 128)
                scr, h2f, h2b = tA[i % 2], tB[i % 2], ymb[i % 2]
                P.dma(lambda e, i=i, rows=rows: e.dma_start(out=x1o[rows, :], in_=xt[i][:]), reads=[xt[i]])
                rmsnorm_mod_tile(P, xt[i], wmod, shb, h2f, scr, ssr[i % 2], rstd[i % 2])
                P.op("scalar", lambda e, h2f=h2f, h2b=h2b: e.activation(out=h2b[:], in_=h2f[:], func=AF.Copy), reads=[h2f], writes=[h2b])
                P.dma(lambda e, h2b=h2b, rows=rows: e.dma_start(out=h2o[rows, :], in_=h2b[:]), reads=[h2b])
                for q4 in range(4):
                    pt = ptf()
                    for j in range(4):
                        k = q4 * 4 + j
                        P.op("tensor", lambda e, pt=pt, j=j, k=k, h2f=h2f: e.transpose(out=pt[:, j * 128:(j + 1) * 128],
                                                                                   in_=h2f[:, k * 128:(k + 1) * 128], identity=identf[:]),
                             reads=[h2f, identf], writes=[pt])
                    P.op("scalar" if q4 % 2 == 0 else "vector",
                         (lambda e, pt=pt, q4=q4: e.activation(out=tC[:, q4 * 512:(q4 + 1) * 512], in_=pt[:], func=AF.Copy)) if q4 % 2 == 0 else
                         (lambda e, pt=pt, q4=q4: e.tensor_copy(out=tC[:, q4 * 512:(q4 + 1) * 512], in_=pt[:])),
                         reads=[pt], writes=[tC])
                a1 = acc()
                for k in range(16):
                    P.op("tensor", lambda e, a1=a1, k=k: e.matmul(a1[:, 0:NE], lhsT=tC[:, k * 128:(k + 1) * 128], rhs=wrs[:, k, :],
                                                               start=(k == 0), stop=(k == 15)), reads=[tC, wrs], writes=[a1])
                l_, m8, mk, e_, z_ = lg[i % 2], mx8[i % 2], msk[i % 2], ex[i % 2], zz[i % 2]
                P.op("vector", lambda e, a1=a1, l_=l_: e.tensor_tensor(out=l_[:], in0=a1[:, 0:NE], in1=brb[:], op=ALU.add), reads=[a1, brb], writes=[l_])
                P.op("vector", lambda e, l_=l_, m8=m8: e.max(out=m8[:], in_=l_[:]), reads=[l_], writes=[m8])
                P.op("vector", lambda e, l_=l_, m8=m8, mk=mk: e.tensor_scalar(out=mk[:], in0=l_[:], scalar1=m8[:, 3:4], scalar2=None, op0=ALU.is_ge),
                     reads=[l_, m8], writes=[mk])
                P.op("vector", lambda e, m8=m8, z_=z_: e.tensor_scalar(out=z_[:, 0:1], in0=m8[:, 0:1], scalar1=-1.0, scalar2=None, op0=ALU.mult),
                     reads=[m8], writes=[z_])
                P.op("scalar", lambda e, l_=l_, e_=e_, z_=z_: e.activation(out=e_[:], in_=l_[:], func=AF.Exp, bias=z_[:, 0:1]), reads=[l_, z_], writes=[e_])
                P.op("vector", lambda e, e_=e_, mk=mk: e.tensor_tensor(out=e_[:], in0=e_[:], in1=mk[:], op=ALU.mult), reads=[e_, mk], writes=[e_])
                P.op("vector", lambda e, e_=e_, z_=z_: e.reduce_sum(out=z_[:, 1:2], in_=e_[:], axis=AX.X), reads=[e_], writes=[z_])
                P.op("vector", lambda e, z_=z_: e.reciprocal(out=z_[:, 1:2], in_=z_[:, 1:2]), reads=[z_], writes=[z_])
                P.op("vector", lambda e, e_=e_, z_=z_: e.tensor_scalar(out=e_[:], in0=e_[:], scalar1=z_[:, 1:2], scalar2=None, op0=ALU.mult),
                     reads=[e_, z_], writes=[e_])
                P.dma(lambda e, e_=e_, rows=rows: e.dma_start(out=Go[rows, :], in_=e_[:]), reads=[e_])
        P.finish()
        P.emit()
    return nc


def run_D(ncD, x2d, hfw, hbw, ya, pb, m_norm_l, g_m, norm_ffn_l, sc_f, sh_f, w_br_m, w_br_a, w_out, w_router, b_router):
    vecs = np.stack([m_norm_l.reshape(D), g_m, norm_ffn_l, sc_f, sh_f]).astype(np.float32)
    idn = np.eye(128, dtype=np.float32).astype(NPBF)
    idf = np.eye(128, dtype=np.float32)
    mo = pb[:, O_MO:O_MO + 2048]
    gm = pb[:, O_GM - 16:O_GM - 16 + 2048]
    ga = pb[:, O_GA - 16:O_GA - 16 + 2048]
    in_maps = []
    for i in range(NCORES):
        rows = slice(i * TL, (i + 1) * TL)
        c = np.ascontiguousarray
        in_maps.append({"x": c(x2d[rows]), "hfw": c(hfw[rows]), "hbw": c(hbw[rows]), "ya": c(ya[rows]), "mo": c(mo[rows]),
                        "gm": c(gm[rows]), "ga": c(ga[rows]), "vecs": vecs, "wbm": w_br_m, "wba": w_br_a, "wo": w_out,
                        "wr": w_router, "br": b_router.reshape(1, NE), "idn": idn, "idf": idf})
    res = _run(ncD, in_maps)
    x1 = np.concatenate([r["x1"] for r in res], axis=0)
    h2 = np.concatenate([r["h2"] for r in res], axis=0)
    G = np.concatenate([r["G"] for r in res], axis=0)
    return x1, h2, G


SPC = BPC * EBLK
SERIAL_SCATTER = False
BIGI = 1000000.0


def build_E():
    nc = _new_nc()
    GTi = _din(nc, "GT", [NE, S])
    h2 = _din(nc, "h2", [S, D], BF16)
    wgu = _din(nc, "wgu", [NE * D * 2, 2048])
    wdn = _din(nc, "wdn", [NE * DFF, D])
    bgu = _din(nc, "bgu", [NE * 2, 2048])
    bdn = _din(nc, "bdn", [NE, D])
    idn = _din(nc, "idn", [128, 128], BF16)
    idf = _din(nc, "idf", [128, 128])
    cst = _din(nc, "cst", [NE, NE + BPC + 32])
    jc = _din(nc, "jc", [128, 48])
    tokid = _din(nc, "tokid", [128, S // 128], I32)
    basei = _din(nc, "base", [128, 2])
    ys = _dout(nc, "ys", [SPC, D], BF16)
    destMo = _dout(nc, "destM", [NE, S])
    stok = _dout(nc, "stok", [SPC + 128, 1], I32)
    NTT = S // 128
    with ExitStack() as es:
        P = Prog(nc, es)
        Wr = [P.sb("W", [128, 16, 2048], BF16) for _ in range(2)]
        Wj = [[View(Wr[r][:, j, :]) for j in range(16)] for r in range(2)]
        raw = Wr[0].t[:].rearrange("p j n -> p (j n)").bitcast(F32)
        mk = View(raw[0:NE, 0:S], root=Wr[0])
        cum = View(raw[0:NE, S:2 * S], root=Wr[0])
        raw1 = Wr[1].t[:].rearrange("p j n -> p (j n)").bitcast(F32)
        dtok = View(raw1[:, 0:NTT * NE].rearrange("p (t e) -> p t e", e=NE), root=Wr[1])
        ident = P.sb("ident", [128, 128], BF16)
        identf = P.sb("identf", [128, 128], F32)
        cs = P.sb("cs", [NE, NE + BPC + 32], F32)
        jct = P.sb("jct", [128, 48], F32)
        tki = P.sb("tki", [128, NTT], I32)
        bas = P.sb("bas", [128, 2], F32)
        c32 = P.sb("c32", [NE, 8], F32)
        cmp_ = P.sb("cmp", [NE, BPC], F32)
        o32 = P.sb("o32", [NE, 128], F32)
        ebf = P.sb("ebf", [128, BPC], F32)
        wif = P.sb("wif", [128, BPC * 48], F32)
        wii = P.sb("wii", [128, BPC * 48], I32)
        bif = P.sb("bif", [128, BPC * 4], F32)
        bii = P.sb("bii", [128, BPC * 4], I32)
        i4 = P.sb("i4", [128, NTT * 4], I32)
        zt = P.sb("zt", [128, SPC // 128 + 1], I32)
        sti = P.sb("sti", [128, SPC // 128], I32)
        ones1 = P.sb("ones1", [1, 128], BF16)
        xb = P.sb("xb", [128, 2, D], BF16)
        xbs = [View(xb[:, sh, :]) for sh in range(2)]
        xbT = P.sb("xbT", [128, 16, 256], BF16)
        bg = P.sb("bg", [128, 2, 2048], BF16)
        bgs = [View(bg[:, c, :]) for c in range(2)]
        bd = P.sb("bd", [128, D], BF16)
        tg = P.sb("tg", [128, 2, 2048], BF16)
        rawt = tg.t[:].rearrange("p a n -> p (a n)").bitcast(F32)
        d8 = View(rawt[:, 0:NTT * 8].rearrange("p (t k) -> p t k", k=8), root=tg)
        d4 = View(rawt[:, 512:512 + NTT * 4], root=tg)
        v1 = View(rawt[:, 768:768 + NTT * 4], root=tg)
        v2 = View(rawt[:, 1024:1024 + NTT * 4], root=tg)
        act = [P.sb("act", [128, DFF], BF16) for _ in range(2)]
        actT = P.sb("actT", [128, 16, 256], BF16)
        tmp_l = [P.sb("tmp", [128, 512], F32) for _ in range(4)]
        ysb_l = [P.sb("ysb", [128, 512], BF16) for _ in range(2)]
        ptr_l = [P.ps("ptr", [128, 1024], BF16) for _ in range(2)]
        acc_l = [P.ps("acc", [128, 512], F32) for _ in range(4)]
        ptr, acc, tmpr, ysbr = RR(ptr_l), RR(acc_l), RR(tmp_l), RR(ysb_l)
        evac = RR(["vector"])
        stv = View(stok)
        scat = [Buf(None, "scat%d" % i) for i in range(NTT * 4)]

        P.dma(lambda e: e.dma_start(out=ident[:], in_=idn[:, :]), writes=[ident])
        P.dma(lambda e: e.dma_start(out=identf[:], in_=idf[:, :]), writes=[identf])
        P.dma(lambda e: e.dma_start(out=cs[:], in_=cst[:, :]), writes=[cs])
        P.dma(lambda e: e.dma_start(out=jct[:], in_=jc[:, :]), writes=[jct])
        tk1 = [P.sb("tk1", [128, 1], I32) for _ in range(NTT)]
        for tt in range(NTT):
            P.dma(lambda e, tt=tt: e.dma_start(out=tk1[tt][:], in_=tokid[:, tt:tt + 1], allow_slow_non_contiguous=True), writes=[tk1[tt]])
        P.dma(lambda e: e.dma_start(out=bas[:], in_=basei[:, :]), writes=[bas])
        P.dma(lambda e: e.dma_start(out=mk[:], in_=GTi[:, :]), writes=[mk])
        P.op("gpsimd", lambda e: e.memset(ones1[:], 1.0), writes=[ones1])
        P.op("gpsimd", lambda e: e.memset(zt[:], 0), writes=[zt])
        P.dma(lambda e: e.dma_start(out=stok.rearrange("(p c) o -> p (c o)", p=128), in_=zt[:]), reads=[zt], writes=[stv])
        P.op("vector", lambda e: e.tensor_scalar(out=mk[:], in0=mk[:], scalar1=0.0, scalar2=None, op0=ALU.is_gt), reads=[mk], writes=[mk])
        P.op("vector", lambda e: e.tensor_tensor_scan(out=cum[:], data0=mk[:], data1=mk[:], initial=0.0, op0=ALU.add, op1=ALU.max),
             reads=[mk], writes=[cum])
        P.op("vector", lambda e: e.tensor_scalar(out=o32[:, 0:NE], in0=cs[:, NE + BPC:NE + BPC + 32], scalar1=cum[:, S - 1:S], scalar2=None,
                                                 op0=ALU.is_lt), reads=[cs, cum], writes=[o32])
        P.op("vector", lambda e: e.reduce_sum(out=c32[:, 1:2], in_=o32[:, 0:NE], axis=AX.X), reads=[o32], writes=[c32])
        P.op("vector", lambda e: e.tensor_scalar(out=c32[:, 1:2], in0=c32[:, 1:2], scalar1=256.0, scalar2=None, op0=ALU.mult), reads=[c32], writes=[c32])
        P.op("gpsimd", lambda e: e.memset(o32[:], 1.0), reads=[o32], writes=[o32])
        a0 = acc()
        P.op("tensor", lambda e: e.matmul(a0[0:NE, 0:1], lhsT=cs[:, 0:NE], rhs=c32[:, 1:2], start=True, stop=True), reads=[cs, c32], writes=[a0])
        P.op("vector", lambda e: e.tensor_copy(out=c32[:, 3:4], in_=a0[0:NE, 0:1]), reads=[a0], writes=[c32])
        P.op("vector", lambda e: e.tensor_tensor(out=c32[:, 4:5], in0=c32[:, 3:4], in1=c32[:, 1:2], op=ALU.subtract), reads=[c32], writes=[c32])
        P.op("vector", lambda e: e.tensor_tensor(out=cum[:], in0=cum[:], in1=mk[:], op=ALU.subtract), reads=[cum, mk], writes=[cum])
        P.op("vector", lambda e: e.tensor_scalar(out=cum[:], in0=cum[:], scalar1=c32[:, 4:5], scalar2=1.0, op0=ALU.add, op1=ALU.add),
             reads=[cum, c32], writes=[cum])
        P.op("vector", lambda e: e.tensor_tensor(out=cum[:], in0=cum[:], in1=mk[:], op=ALU.mult), reads=[cum, mk], writes=[cum])
        P.dma(lambda e: e.dma_start(out=destMo[:, :], in_=cum[:]), reads=[cum])
        P.op("vector", lambda e: e.tensor_scalar(out=cmp_[:], in0=cs[:, NE:NE + BPC], scalar1=c32[:, 3:4], scalar2=None, op0=ALU.is_ge),
             reads=[cs, c32], writes=[cmp_])
        a1 = acc()
        P.op("tensor", lambda e: e.matmul(a1[:, 0:BPC], lhsT=o32[:], rhs=cmp_[:], start=True, stop=True), reads=[o32, cmp_], writes=[a1])
        P.op("vector", lambda e: e.tensor_scalar(out=ebf[:], in0=a1[:, 0:BPC], scalar1=float(NE - 1), scalar2=None, op0=ALU.min), reads=[a1], writes=[ebf])
        for b in range(BPC):
            P.op("vector", lambda e, b=b: e.tensor_scalar(out=bif[:, b * 4 + 3:b * 4 + 4], in0=ebf[:, b:b + 1], scalar1=4096.0, scalar2=None, op0=ALU.mult),
                 reads=[ebf], writes=[bif])
            P.op("vector", lambda e, b=b: e.tensor_scalar(out=wif[:, b * 48:b * 48 + 32], in0=jct[:, 0:32], scalar1=bif[:, b * 4 + 3:b * 4 + 4], scalar2=None, op0=ALU.add),
                 reads=[jct, bif], writes=[wif])
            P.op("vector", lambda e, b=b: e.tensor_scalar(out=bif[:, b * 4 + 3:b * 4 + 4], in0=ebf[:, b:b + 1], scalar1=2048.0, scalar2=None, op0=ALU.mult),
                 reads=[ebf], writes=[bif])
            P.op("vector", lambda e, b=b: e.tensor_scalar(out=wif[:, b * 48 + 32:b * 48 + 48], in0=jct[:, 32:48], scalar1=bif[:, b * 4 + 3:b * 4 + 4], scalar2=None, op0=ALU.add),
                 reads=[jct, bif], writes=[wif])
            P.op("vector", lambda e, b=b: e.tensor_scalar(out=bif[:, b * 4:b * 4 + 1], in0=ebf[:, b:b + 1], scalar1=2.0, scalar2=None, op0=ALU.mult),
                 reads=[ebf], writes=[bif])
            P.op("vector", lambda e, b=b: e.tensor_scalar(out=bif[:, b * 4 + 1:b * 4 + 2], in0=ebf[:, b:b + 1], scalar1=2.0, scalar2=1.0, op0=ALU.mult, op1=ALU.add),
                 reads=[ebf], writes=[bif])
            P.op("vector", lambda e, b=b: e.tensor_copy(out=bif[:, b * 4 + 2:b * 4 + 3], in_=ebf[:, b:b + 1]), reads=[ebf], writes=[bif])
        P.op("vector", lambda e: e.tensor_copy(out=wii[:], in_=wif[:]), reads=[wif], writes=[wii])
        P.op("vector", lambda e: e.tensor_copy(out=bii[:], in_=bif[:]), reads=[bif], writes=[bii])
        for g in range(NTT // 16):
            a = acc()
            for j in range(16):
                tt = g * 16 + j
                P.op("tensor", lambda e, a=a, j=j, tt=tt: e.transpose(out=a[:, j * NE:(j + 1) * NE], in_=cum[:, tt * 128:(tt + 1) * 128],
                                                                    identity=identf[0:NE, 0:NE]), reads=[cum, identf], writes=[a])
            P.op("vector", lambda e, a=a, g=g: e.tensor_copy(out=dtok[:, g * 16:(g + 1) * 16, :], in_=a[:].rearrange("p (t e) -> p t e", e=NE)),
                 reads=[a], writes=[dtok])
        for tt in range(NTT):
            P.op("vector", lambda e, tt=tt: e.max(out=d8[:, tt, :], in_=dtok[:, tt, :]), reads=[dtok], writes=[d8])
        P.op("vector", lambda e: e.tensor_scalar(out=d4[:].rearrange("p (t k) -> p t k", k=4), in0=d8[:, :, 0:4], scalar1=bas[:, 0:1], scalar2=-1.0, op0=ALU.subtract, op1=ALU.add),
             reads=[d8, bas], writes=[d4])
        P.op("vector", lambda e: e.tensor_scalar(out=v1[:], in0=d4[:], scalar1=0.0, scalar2=None, op0=ALU.is_ge), reads=[d4], writes=[v1])
        P.op("vector", lambda e: e.tensor_scalar(out=v2[:], in0=d4[:], scalar1=float(SPC), scalar2=None, op0=ALU.is_lt), reads=[d4], writes=[v2])
        P.op("vector", lambda e: e.tensor_tensor(out=v1[:], in0=v1[:], in1=v2[:], op=ALU.mult), reads=[v1, v2], writes=[v1])
        P.op("vector", lambda e: e.scalar_tensor_tensor(out=d4[:], in0=d4[:], scalar=bas[:, 1:2], in1=v1[:], op0=ALU.subtract, op1=ALU.mult),
             reads=[d4, v1, bas], writes=[d4])
        P.op("vector", lambda e: e.tensor_scalar(out=d4[:], in0=d4[:], scalar1=bas[:, 1:2], scalar2=None, op0=ALU.add), reads=[d4, bas], writes=[d4])
        P.op("vector", lambda e: e.tensor_copy(out=i4[:], in_=d4[:]), reads=[d4], writes=[i4])
        for tt in range(NTT):
            for k in range(4):
                P.dma(lambda e, tt=tt, k=k: e.indirect_dma_start(
                    out=stok[:, :], out_offset=bass.IndirectOffsetOnAxis(ap=i4[:, tt * 4 + k:tt * 4 + k + 1], axis=0), in_=tk1[tt][:, :],
                    in_offset=None), reads=[i4, tk1[tt]] + ([] if SERIAL_SCATTER else [stv]),
                    writes=[scat[tt * 4 + k]] + ([stv] if SERIAL_SCATTER else []), eng="gpsimd")
        P.dma(lambda e: e.dma_start(out=sti[:], in_=stok[0:SPC, :].rearrange("(c p) o -> p (c o)", p=128), allow_slow_non_contiguous=True),
              reads=scat, writes=[sti, stv])

        fz = P.sb("fz", [1, 1], F32)
        P.op("gpsimd", lambda e: e.memset(fz[:], 0.0), writes=[fz, Wr[0], Wr[1], tg] + Wj[0] + Wj[1])
        ring = 0
        for b in range(BPC):
            for sh in range(2):
                P.dma(lambda e, b=b, sh=sh: e.indirect_dma_start(
                    out=xb[:, sh, :], out_offset=None, in_=h2[:, :],
                    in_offset=bass.IndirectOffsetOnAxis(ap=sti[:, b * 2 + sh:b * 2 + sh + 1], axis=0)), reads=[sti], writes=[xbs[sh]], eng="gpsimd")
            for c in range(2):
                P.dma(lambda e, b=b, c=c: e.indirect_dma_start(
                    out=bg[:, c, :], out_offset=None, in_=bgu[:, :],
                    in_offset=bass.IndirectOffsetOnAxis(ap=bii[:, b * 4 + c:b * 4 + c + 1], axis=0)), reads=[bii], writes=[bgs[c]], eng="gpsimd")
            P.dma(lambda e, b=b: e.indirect_dma_start(
                out=bd[:], out_offset=None, in_=bdn[:, :],
                in_offset=bass.IndirectOffsetOnAxis(ap=bii[:, b * 4 + 2:b * 4 + 3], axis=0)), reads=[bii], writes=[bd], eng="gpsimd")
            for sh in range(2):
                transpose_tile(P, xbs[sh], xbT, ident, ptr, evac,
                               dst_sl=lambda g, n, sh=sh: (lambda d, g=g, n=n, sh=sh: d[:, g:g + n, sh * 128:(sh + 1) * 128]))
            for c in range(2):
                r = ring % 2
                ring += 1
                for j in range(16):
                    P.dma(lambda e, b=b, c=c, j=j, r=r: e.indirect_dma_start(
                        out=Wr[r][:, j, :], out_offset=None, in_=wgu[:, :],
                        in_offset=bass.IndirectOffsetOnAxis(ap=wii[:, b * 48 + j * 2 + c:b * 48 + j * 2 + c + 1], axis=0)),
                        reads=[wii], writes=[Wj[r][j]], eng="gpsimd")
                for n in range(4):
                    cols = slice(n * 512, (n + 1) * 512)
                    for sh in range(2):
                        a = acc()
                        for j in range(16):
                            P.op("tensor", lambda e, a=a, r=r, j=j, sh=sh, cols=cols: e.matmul(
                                a[:], lhsT=xbT[:, j, sh * 128:(sh + 1) * 128], rhs=Wr[r][:, j, cols], start=(j == 0), stop=False),
                                reads=[xbT, Wj[r][j]], writes=[a])
                        P.op("tensor", lambda e, a=a, c=c, cols=cols: e.matmul(a[:], lhsT=ones1[0:1, :], rhs=bg[0:1, c, cols], start=False, stop=True),
                             reads=[ones1, bgs[c]], writes=[a])
                        t_ = tmpr()
                        if c == 0:
                            s_ = tmpr()
                            P.op("vector", lambda e, a=a, t_=t_: e.tensor_scalar(out=t_[:], in0=a[:], scalar1=7.0, scalar2=None, op0=ALU.min),
                                 reads=[a], writes=[t_])
                            P.op("scalar", lambda e, t_=t_, s_=s_: e.activation(out=s_[:], in_=t_[:], func=AF.Sigmoid, scale=1.702),
                                 reads=[t_], writes=[s_])
                            P.op("gpsimd", lambda e, t_=t_, s_=s_, sh=sh, cols=cols: e.tensor_tensor(out=tg[:, sh, cols], in0=t_[:], in1=s_[:], op=ALU.mult),
                                 reads=[t_, s_], writes=[tg])
                        else:
                            P.op("vector", lambda e, a=a, t_=t_: e.tensor_scalar(out=t_[:], in0=a[:], scalar1=7.0, scalar2=-7.0, op0=ALU.min, op1=ALU.max),
                                 reads=[a], writes=[t_])
                            P.op("vector", lambda e, t_=t_, sh=sh, cols=cols: e.scalar_tensor_tensor(
                                out=act[sh][:, cols], in0=t_[:], scalar=1.0, in1=tg[:, sh, cols], op0=ALU.add, op1=ALU.mult),
                                reads=[t_, tg], writes=[act[sh]])
            for sh in range(2):
                transpose_tile(P, act[sh], actT, ident, ptr, evac,
                               dst_sl=lambda g, n, sh=sh: (lambda d, g=g, n=n, sh=sh: d[:, g:g + n, sh * 128:(sh + 1) * 128]))
            r = ring % 2
            ring += 1
            for j in range(16):
                P.dma(lambda e, b=b, j=j, r=r: e.indirect_dma_start(
                    out=Wr[r][:, j, :], out_offset=None, in_=wdn[:, :],
                    in_offset=bass.IndirectOffsetOnAxis(ap=wii[:, b * 48 + 32 + j:b * 48 + 33 + j], axis=0)),
                    reads=[wii], writes=[Wj[r][j]], eng="gpsimd")
            for n in range(4):
                cols = slice(n * 512, (n + 1) * 512)
                for sh in range(2):
                    a = acc()
                    for j in range(16):
                        P.op("tensor", lambda e, a=a, r=r, j=j, sh=sh, cols=cols: e.matmul(
                            a[:], lhsT=actT[:, j, sh * 128:(sh + 1) * 128], rhs=Wr[r][:, j, cols], start=(j == 0), stop=False),
                            reads=[actT, Wj[r][j]], writes=[a])
                    P.op("tensor", lambda e, a=a, cols=cols: e.matmul(a[:], lhsT=ones1[0:1, :], rhs=bd[0:1, cols], start=False, stop=True),
                         reads=[ones1, bd], writes=[a])
                    y_ = ysbr()
                    P.op("scalar", lambda e, a=a, y_=y_: e.activation(out=y_[:], in_=a[:], func=AF.Copy), reads=[a], writes=[y_])
                    r0 = b * EBLK + sh * 128
                    P.dma(lambda e, y_=y_, r0=r0, cols=cols: e.dma_start(out=ys[r0:r0 + 128, cols], in_=y_[:]), reads=[y_])
        P.finish()
        P.emit()
    return nc


def run_E(ncE, G, h2, w_gu_l, b_gu_l, w_down_l, b_down_l):
    GT = np.ascontiguousarray(G.T)
    idn = np.eye(128, dtype=np.float32).astype(NPBF)
    idf = np.eye(128, dtype=np.float32)
    U = np.triu(np.ones((NE, NE), np.float32))
    p = np.arange(128, dtype=np.float32)[:, None]
    jcol = np.arange(32)
    jc = np.concatenate([(jcol // 2 * 128)[None, :] * 2.0 + (jcol % 2)[None, :] + 2.0 * p,
                         (np.arange(16) * 128)[None, :] + p], axis=1).astype(np.float32)
    tokid = (np.arange(S // 128, dtype=np.int32)[None, :] * 128 + np.arange(128, dtype=np.int32)[:, None]).astype(np.int32)
    wgu = w_gu_l.reshape(NE * D * 2, 2048)
    wdn = w_down_l.reshape(NE * DFF, D)
    bgu = b_gu_l.reshape(NE * 2, 2048)
    in_maps = []
    for i in range(NCORES):
        thr = ((BPC * i + np.arange(BPC)) * EBLK).astype(np.float32)
        cst = np.concatenate([U, np.tile(thr[None, :], (NE, 1)), np.tile((np.arange(32) * 256.0)[None, :], (NE, 1))], axis=1).astype(np.float32)
        in_maps.append({"GT": GT, "h2": h2, "wgu": wgu, "wdn": wdn, "bgu": bgu, "bdn": b_down_l, "idn": idn, "idf": idf,
                        "cst": cst, "jc": jc, "tokid": tokid, "base": np.stack([np.full(128, SPC * i, np.float32), SPC + np.arange(128, dtype=np.float32)], axis=1)})
    res = _run(ncE, in_maps)
    ys = np.concatenate([r["ys"] for r in res], axis=0)
    destM = res[0]["destM"]
    return ys, destM


def build_F(final):
    nc = _new_nc()
    ysf = _din(nc, "ys", [NSLOT, D], BF16)
    dmi = _din(nc, "dm", [TL, NE])
    gti = _din(nc, "gt", [TL, NE])
    x1 = _din(nc, "x1", [TL, D])
    vecs = _din(nc, "vecs", [2, D])
    out = _dout(nc, "out", [TL, D])
    NT = TL // 128
    with ExitStack() as es:
        P = Prog(nc, es)
        gfb = P.sb("gfb", [128, D], F32)
        nfb = P.sb("nfb", [128, D], F32)
        zb = P.sb("zb", [128, D], F32)
        dm = [P.sb("dm", [128, NE], F32) for _ in range(2)]
        gt = [P.sb("gt", [128, NE], F32) for _ in range(2)]
        d8 = [P.sb("d8", [128, 8], F32) for _ in range(2)]
        d4f = [P.sb("d4f", [128, 4], F32) for _ in range(2)]
        d4i = [P.sb("d4i", [128, 4], I32) for _ in range(2)]
        oh = [P.sb("oh", [128, NE], F32) for _ in range(2)]
        g4 = [P.sb("g4", [128, 4], F32) for _ in range(2)]
        Y = [[P.sb("Y", [128, D], BF16) for _ in range(4)] for _ in range(2)]
        xt = [P.sb("xt", [128, D], F32) for _ in range(2)]
        ac = [P.sb("ac", [128, D], F32) for _ in range(2)]
        scr = P.sb("scr", [128, D], F32)
        ot = [P.sb("ot", [128, D], F32) for _ in range(2)]
        ss = [P.sb("ss", [128, 1], F32) for _ in range(2)]
        rstd = [P.sb("rstd", [128, 1], F32) for _ in range(2)]
        P.dma(lambda e: e.dma_start(out=gfb[:], in_=vecs[0:1, :].to_broadcast([128, D])), writes=[gfb])
        if final:
            P.dma(lambda e: e.dma_start(out=nfb[:], in_=vecs[1:2, :].to_broadcast([128, D])), writes=[nfb])
            P.op("gpsimd", lambda e: e.memset(zb[:], 0.0), writes=[zb])
        for tt in range(NT):
            i = tt % 2
            rows = slice(tt * 128, (tt + 1) * 128)
            P.dma(lambda e, i=i, rows=rows: e.dma_start(out=dm[i][:], in_=dmi[rows, :]), writes=[dm[i]])
            P.dma(lambda e, i=i, rows=rows: e.dma_start(out=gt[i][:], in_=gti[rows, :]), writes=[gt[i]])
            P.dma(lambda e, i=i, rows=rows: e.dma_start(out=xt[i][:], in_=x1[rows, :]), writes=[xt[i]])
            P.op("vector", lambda e, i=i: e.max(out=d8[i][:], in_=dm[i][:]), reads=[dm[i]], writes=[d8[i]])
            P.op("vector", lambda e, i=i: e.tensor_scalar(out=d4f[i][:], in0=d8[i][:, 0:4], scalar1=-1.0, scalar2=None, op0=ALU.add),
                 reads=[d8[i]], writes=[d4f[i]])
            P.op("vector", lambda e, i=i: e.tensor_copy(out=d4i[i][:], in_=d4f[i][:]), reads=[d4f[i]], writes=[d4i[i]])
            for k in range(4):
                P.dma(lambda e, i=i, k=k: e.indirect_dma_start(out=Y[i][k][:], out_offset=None, in_=ysf[:, :],
                                                               in_offset=bass.IndirectOffsetOnAxis(ap=d4i[i][:, k:k + 1], axis=0)),
                      reads=[d4i[i]], writes=[Y[i][k]], eng="gpsimd")
                P.op("vector", lambda e, i=i, k=k: e.tensor_scalar(out=oh[i][:], in0=dm[i][:], scalar1=d8[i][:, k:k + 1], scalar2=None,
                                                                   op0=ALU.is_equal), reads=[dm[i], d8[i]], writes=[oh[i]])
                P.op("vector", lambda e, i=i: e.tensor_tensor(out=oh[i][:], in0=oh[i][:], in1=gt[i][:], op=ALU.mult), reads=[oh[i], gt[i]], writes=[oh[i]])
                P.op("vector", lambda e, i=i, k=k: e.reduce_sum(out=g4[i][:, k:k + 1], in_=oh[i][:], axis=AX.X), reads=[oh[i]], writes=[g4[i]])
            P.op("vector", lambda e, i=i: e.tensor_scalar(out=ac[i][:], in0=Y[i][0][:], scalar1=g4[i][:, 0:1], scalar2=None, op0=ALU.mult),
                 reads=[Y[i][0], g4[i]], writes=[ac[i]])
            for k in range(1, 4):
                P.op("vector", lambda e, i=i, k=k: e.scalar_tensor_tensor(out=ac[i][:], in0=Y[i][k][:], scalar=g4[i][:, k:k + 1], in1=ac[i][:],
                                                                          op0=ALU.mult, op1=ALU.add), reads=[Y[i][k], g4[i], ac[i]], writes=[ac[i]])
            P.op("gpsimd", lambda e, i=i: e.tensor_tensor(out=ac[i][:], in0=ac[i][:], in1=gfb[:], op=ALU.mult), reads=[ac[i], gfb], writes=[ac[i]])
            P.op("gpsimd", lambda e, i=i: e.tensor_tensor(out=xt[i][:], in0=xt[i][:], in1=ac[i][:], op=ALU.add), reads=[ac[i], xt[i]], writes=[xt[i]])
            if final:
                rmsnorm_mod_tile(P, xt[i], nfb, zb, ot[i], scr, ss[i], rstd[i])
                P.dma(lambda e, i=i, rows=rows: e.dma_start(out=out[rows, :], in_=ot[i][:]), reads=[ot[i]])
            else:
                P.dma(lambda e, i=i, rows=rows: e.dma_start(out=out[rows, :], in_=xt[i][:]), reads=[xt[i]])
        P.finish()
        P.emit()
    return nc


def run_F(ncF, ys, destM, G, x1, g_f, norm_final):
    dmT = np.ascontiguousarray(destM.T)
    vecs = np.stack([g_f, norm_final]).astype(np.float32)
    in_maps = []
    for i in range(NCORES):
        rows = slice(i * TL, (i + 1) * TL)
        c = np.ascontiguousarray
        in_maps.append({"ys": ys, "dm": c(dmT[rows]), "gt": c(G[rows]), "x1": c(x1[rows]), "vecs": vecs})
    res = _run(ncF, in_maps)
    return np.concatenate([r["out"] for r in res], axis=0)


def kernel(x, c, norm_mix, norm_ffn, w_ada, b_ada, w_in, b_mgates, m_norm, lam_q1, lam_k1, lam_q2, lam_k2, a_norm,
           w_br_m, w_br_a, w_out, w_router, b_router, w_gu, b_gu, w_down, b_down, norm_final):
    f = lambda a: np.ascontiguousarray(np.asarray(a, dtype=np.float32))
    xs = f(x)[0]
    mod = run_A(f(c), f(w_ada), f(b_ada))
    ncB, ncC1, ncC2, ncD, ncE = build_B(), build_C1(), build_C2(), build_D(), build_E()
    for l in range(DEPTH):
        lam_init = 0.8 - 0.6 * float(np.exp(-0.3 * l))
        sh_m, sc_m, g_m, sh_f, sc_f, g_f = [f(v) for v in np.split(mod[l], 6)]
        pb, pg = run_B(ncB, xs, f(norm_mix[l]), sc_m, sh_m, f(w_in[l]), f(b_mgates[l]))
        lam4 = np.stack([f(lam_q1[l]), f(lam_k1[l]), f(lam_q2[l]), f(lam_k2[l])])
        ya = run_C1(ncC1, pb, lam4, f(a_norm[l]), lam_init)
        hfw, hbw = run_C2(ncC2, pb, pg)
        x1, h2, G = run_D(ncD, xs, hfw, hbw, ya, pb, f(m_norm[l]), g_m, f(norm_ffn[l]), sc_f, sh_f,
                          f(w_br_m[l]), f(w_br_a[l]), f(w_out[l]), f(w_router[l]), f(b_router[l]))
        ys, destM = run_E(ncE, G, h2, f(w_gu[l]), f(b_gu[l]), f(w_down[l]), f(b_down[l]))
        xs = run_F(build_F(l == DEPTH - 1), ys, destM, G, x1, g_f, f(norm_final))
    return xs.reshape(1, S, D).astype(np.float32)
```

```python
import numpy as np
import ml_dtypes
import concourse.bass as bass
import concourse.mybir as mybir
from concourse.bass_utils import run_bass_kernel_spmd
from contextlib import ExitStack

F32 = mybir.dt.float32
BF16 = mybir.dt.bfloat16
I32 = mybir.dt.int32
U32 = mybir.dt.uint32
AF = mybir.ActivationFunctionType
ALU = mybir.AluOpType
AX = mybir.AxisListType
NPBF = ml_dtypes.bfloat16

NCORES = 8
D = 2048
S = 8192
TL = S // NCORES
DEPTH = 2
M_HEADS, M_QK, M_V = 4, 256, 512
A_HEADS, A_QK, A_V = 8, 128, 256
NE, TOPK, DFF, EBLK = 32, 4, 2048, 256
NSLOT = S * TOPK + NE * EBLK
NBLK = NSLOT // EBLK
BPC = NBLK // NCORES
EPS = 1e-6
IN_SIZES = (1024, 1024, 2048, 2048, 16, 2048, 2048, 2048, 2048, 2048)
IN_COLS = sum(IN_SIZES)
O_MQ, O_MK, O_MV, O_MO, O_MG, O_AQ, O_AK, O_AV, O_GM, O_GA = np.cumsum((0,) + IN_SIZES[:-1]).tolist()

ENGS = ["tensor", "vector", "scalar", "gpsimd", "sync"]


class Buf:
    __slots__ = ("t", "lw", "rd", "name", "root")

    def __init__(self, t, name="", root=None):
        self.t = t
        self.lw = None
        self.rd = []
        self.name = name
        self.root = root if root is not None else self

    def __getitem__(self, k):
        return self.t[k]


class Prog:
    def __init__(self, nc, es, ndma=12):
        self.nc = nc
        self.es = es
        self.q = {e: [] for e in ENGS}
        self.cnt = {}
        self.sem = {}
        self.seen = {e: {} for e in ENGS}
        for e in ENGS:
            self.sem[e] = es.enter_context(nc.semaphore("s_" + e))
            self.cnt[e] = 0
        self.dch = {}
        self.dnext = {}
        for q in ("sync", "gpsimd", "scalar"):
            self.dch[q] = []
            for i in range(ndma):
                nm = "d%s%d" % (q[0:2], i)
                self.sem[nm] = es.enter_context(nc.semaphore("s_" + nm))
                self.cnt[nm] = 0
                self.dch[q].append(nm)
            self.dnext[q] = 0
        self.nbuf = 0

    def sb(self, name, shape, dt):
        self.nbuf += 1
        return Buf(self.es.enter_context(self.nc.sbuf_tensor("%s_%d" % (name, self.nbuf), shape, dt)), name)

    def ps(self, name, shape, dt):
        self.nbuf += 1
        return Buf(self.es.enter_context(self.nc.psum_tensor("%s_%d" % (name, self.nbuf), shape, dt)), name)

    def _wait(self, eng, s, i):
        if self.seen[eng].get(s, 0) < i:
            self.seen[eng][s] = i
            sem = self.sem[s]
            self.q[eng].append(lambda e, sem=sem, i=i: e.wait_ge(sem, i))

    def _deps(self, eng, reads, writes, skip_same=False):
        deps = {}
        reads = [b.root for b in reads]
        writes = [b.root for b in writes]
        for b in reads:
            if b.lw is not None:
                s, i = b.lw
                deps[s] = max(deps.get(s, 0), i)
        for b in writes:
            if b.lw is not None:
                s, i = b.lw
                deps[s] = max(deps.get(s, 0), i)
            for s, i in b.rd:
                deps[s] = max(deps.get(s, 0), i)
        for s, i in deps.items():
            if skip_same and s == eng:
                continue
            self._wait(eng, s, i)

    def _mark(self, key, idx, reads, writes):
        reads = [b.root for b in reads]
        writes = [b.root for b in writes]
        for b in reads:
            b.rd = [(s, i) for (s, i) in b.rd if s != key] + [(key, idx)]
        for b in writes:
            b.lw = (key, idx)
            b.rd = []

    def op(self, eng, fn, reads=(), writes=()):
        self._deps(eng, reads, writes, skip_same=(eng == "tensor"))
        self.cnt[eng] += 1
        idx = self.cnt[eng]
        sem = self.sem[eng]
        self.q[eng].append(lambda e, fn=fn, sem=sem: fn(e).then_inc(sem, 1))
        self._mark(eng, idx, reads, writes)

    def dma(self, fn, reads=(), writes=(), eng="sync"):
        chs = self.dch[eng]
        ch = chs[self.dnext[eng]]
        self.dnext[eng] = (self.dnext[eng] + 1) % len(chs)
        if self.cnt[ch] > 0:
            self._wait(eng, ch, self.cnt[ch])
        self._deps(eng, reads, writes)
        self.cnt[ch] += 16
        idx = self.cnt[ch]
        sem = self.sem[ch]
        self.q[eng].append(lambda e, fn=fn, sem=sem: fn(e).then_inc(sem, 16))
        self._mark(ch, idx, reads, writes)

    def finish(self, eng="sync"):
        for s in list(self.sem.keys()):
            if self.cnt[s] > 0 and s != eng:
                self._wait(eng, s, self.cnt[s])

    def emit(self):
        with self.nc.Block() as block:
            for e in ENGS:
                if not self.q[e]:
                    continue
                lst = self.q[e]

                def body(engine, lst=lst):
                    for f in lst:
                        f(engine)
                getattr(block, e)(body)


def _new_nc():
    return bass.Bass("TRN2", target_bir_lowering=False)


def _din(nc, name, shape, dt=F32):
    return nc.dram_tensor(name, list(shape), dt, kind="ExternalInput").ap()


def _dout(nc, name, shape, dt=F32):
    return nc.dram_tensor(name, list(shape), dt, kind="ExternalOutput").ap()


def _run(nc, in_maps):
    res = run_bass_kernel_spmd(nc, in_maps, core_ids=list(range(NCORES)))
    return res.results


class RR:
    def __init__(self, items):
        self.items = list(items)
        self.i = 0

    def __call__(self):
        x = self.items[self.i]
        self.i = (self.i + 1) % len(self.items)
        return x


A_NC = 6 * D // NCORES


def build_A():
    nc = _new_nc()
    cT = _din(nc, "cT", [128, 16])
    wa = _din(nc, "wa", [DEPTH, D, A_NC])
    ba = _din(nc, "ba", [1, DEPTH * A_NC])
    mod = _dout(nc, "mod", [1, DEPTH * A_NC])
    with ExitStack() as es:
        P = Prog(nc, es)
        ct = P.sb("ct", [128, 16], F32)
        ca = P.sb("ca", [128, 16], F32)
        bat = P.sb("bat", [1, DEPTH * A_NC], F32)
        ot = P.sb("ot", [1, DEPTH * A_NC], F32)
        wt = [P.sb("wt", [128, 16, 512], F32) for _ in range(2)]
        pss = [P.ps("ps", [1, 512], F32) for _ in range(2)]
        P.dma(lambda e: e.dma_start(out=ct[:], in_=cT[:, :]), writes=[ct])
        P.dma(lambda e: e.dma_start(out=bat[:], in_=ba[:, :]), writes=[bat])
        P.op("scalar", lambda e: e.activation(out=ca[:], in_=ct[:], func=AF.Silu), reads=[ct], writes=[ca])
        it = 0
        for l in range(DEPTH):
            for n in range(A_NC // 512):
                w = wt[it % 2]
                ps = pss[it % 2]
                src = wa[l, :, n * 512:(n + 1) * 512].rearrange("(k p) n -> p k n", p=128)
                P.dma(lambda e, w=w, src=src: e.dma_start(out=w[:], in_=src), writes=[w])
                for k in range(16):
                    P.op("tensor", lambda e, w=w, ps=ps, k=k: e.matmul(ps[:], lhsT=ca[:, k:k + 1], rhs=w[:, k, :],
                                                                     start=(k == 0), stop=(k == 15)),
                         reads=[ca, w], writes=[ps])
                o0 = l * A_NC + n * 512
                P.op("vector", lambda e, ps=ps, o0=o0: e.tensor_tensor(out=ot[:, o0:o0 + 512], in0=ps[:],
                                                                      in1=bat[:, o0:o0 + 512], op=ALU.add),
                     reads=[ps, bat], writes=[ot])
                it += 1
        P.dma(lambda e: e.dma_start(out=mod[:, :], in_=ot[:]), reads=[ot])
        P.finish()
        P.emit()
    return nc


def run_A(c, w_ada, b_ada):
    nc = build_A()
    cT = np.ascontiguousarray(c.reshape(16, 128).T)
    in_maps = []
    for i in range(NCORES):
        sl = slice(i * A_NC, (i + 1) * A_NC)
        in_maps.append({"cT": cT,
                        "wa": np.ascontiguousarray(w_ada[:, :, sl]),
                        "ba": np.ascontiguousarray(b_ada[:, sl]).reshape(1, DEPTH * A_NC)})
    res = _run(nc, in_maps)
    mod = np.concatenate([r["mod"].reshape(DEPTH, A_NC) for r in res], axis=1)
    return mod


def rmsnorm_mod_tile(P, xt, wmod, shb, hb, scr, ss, rstd):
    P.op("scalar", lambda e: e.activation(out=scr[:], in_=xt[:], func=AF.Square, accum_out=ss[:]),
         reads=[xt], writes=[scr, ss])
    P.op("vector", lambda e: e.tensor_scalar(out=rstd[:], in0=ss[:], scalar1=1.0 / D, scalar2=EPS,
                                             op0=ALU.mult, op1=ALU.add), reads=[ss], writes=[rstd])
    P.op("scalar", lambda e: e.activation(out=rstd[:], in_=rstd[:], func=AF.Sqrt), reads=[rstd], writes=[rstd])
    P.op("vector", lambda e: e.reciprocal(out=rstd[:], in_=rstd[:]), reads=[rstd], writes=[rstd])
    P.op("vector", lambda e: e.scalar_tensor_tensor(out=scr[:], in0=xt[:], scalar=rstd[:, 0:1], in1=wmod[:],
                                                    op0=ALU.mult, op1=ALU.mult),
         reads=[xt, rstd, wmod], writes=[scr])
    P.op("gpsimd", lambda e: e.tensor_tensor(out=hb[:], in0=scr[:], in1=shb[:], op=ALU.add),
         reads=[scr, shb], writes=[hb])


def transpose_tile(P, src, dst, ident, ptr, evac, nchunk=16, src_sl=None, dst_sl=None):
    for g in range(0, nchunk, 8):
        pt = ptr()
        n = min(8, nchunk - g)
        for j in range(n):
            k = g + j
            sl = src_sl(k) if src_sl else slice(k * 128, (k + 1) * 128)
            P.op("tensor", lambda e, pt=pt, j=j, sl=sl: e.transpose(out=pt[:, j * 128:(j + 1) * 128], in_=src[:, sl],
                                                                   identity=ident[:]),
                 reads=[src, ident], writes=[pt])
        eng = "vector"
        dsl = dst_sl(g, n) if dst_sl else (lambda d, g=g, n=n: d[:, g:g + n, :])
        if eng == "scalar":
            P.op("scalar", lambda e, pt=pt, n=n, dsl=dsl: e.activation(out=dsl(dst), in_=pt[:, 0:n * 128].rearrange(
                "p (k t) -> p k t", t=128), func=AF.Copy), reads=[pt], writes=[dst])
        else:
            P.op(eng, lambda e, pt=pt, n=n, dsl=dsl: e.tensor_copy(out=dsl(dst), in_=pt[:, 0:n * 128].rearrange(
                "p (k t) -> p k t", t=128)), reads=[pt], writes=[dst])


def build_B():
    nc = _new_nc()
    x = _din(nc, "x", [TL, D])
    nw = _din(nc, "nw", [1, D])
    sc = _din(nc, "sc", [1, D])
    sh = _din(nc, "sh", [1, D])
    win = _din(nc, "win", [D, IN_COLS])
    bg = _din(nc, "bg", [1, 16])
    cs4 = _din(nc, "cs4", [TL, 128])
    idn = _din(nc, "idn", [128, 128], BF16)
    pb = _dout(nc, "pb", [TL, IN_COLS - 16], BF16)
    pg = _dout(nc, "pg", [TL, 16])
    NT = TL // 128
    with ExitStack() as es:
        P = Prog(nc, es)
        ident = P.sb("ident", [128, 128], BF16)
        nwb = P.sb("nwb", [128, D], F32)
        wmod = P.sb("wmod", [128, D], F32)
        shb = P.sb("shb", [128, D], F32)
        bgb = P.sb("bgb", [128, 16], F32)
        cst = P.sb("cst", [128, NT, 128], F32)
        xts = [P.sb("xt", [128, D], F32) for _ in range(2)]
        scr = P.sb("scr", [128, D], F32)
        hbs = [P.sb("hb", [128, D], BF16) for _ in range(2)]
        ss = [P.sb("ss", [128, 1], F32) for _ in range(2)]
        rstd = [P.sb("rstd", [128, 1], F32) for _ in range(2)]
        hT = [P.sb("hT", [128, 16, 128], BF16) for _ in range(NT)]
        wts = [P.sb("wt", [128, 16, 512], BF16) for _ in range(3)]
        wg = P.sb("wg", [128, 16, 16], BF16)
        ptr_l = [P.ps("ptr", [128, 1024], BF16) for _ in range(2)]
        acc_l = [P.ps("acc", [128, 512], F32) for _ in range(4)]
        stg = [P.sb("stg", [128, 512], BF16) for _ in range(3)]
        r32 = [P.sb("r32", [128, 512], F32) for _ in range(2)]
        rt = [P.sb("rt", [128, 4, 4, 16], F32) for _ in range(2)]
        sg = P.sb("sg", [128, 16], F32)
        ptr = RR(ptr_l)
        acc = RR(acc_l)
        stgr = RR(stg)
        r32r = RR(r32)
        rtr = RR(rt)
        evac = RR(["scalar", "vector"])

        P.dma(lambda e: e.dma_start(out=ident[:], in_=idn[:, :]), writes=[ident])
        P.dma(lambda e: e.dma_start(out=nwb[:], in_=nw.to_broadcast([128, D])), writes=[nwb])
        P.dma(lambda e: e.dma_start(out=wmod[:], in_=sc.to_broadcast([128, D])), writes=[wmod])
        P.dma(lambda e: e.dma_start(out=shb[:], in_=sh.to_broadcast([128, D])), writes=[shb])
        P.dma(lambda e: e.dma_start(out=bgb[:], in_=bg.to_broadcast([128, 16])), writes=[bgb])
        P.dma(lambda e: e.dma_start(out=cst[:], in_=cs4.rearrange("(t p) c -> p t c", p=128)), writes=[cst])
        P.op("vector", lambda e: e.scalar_tensor_tensor(out=wmod[:], in0=wmod[:], scalar=1.0, in1=nwb[:],
                                                        op0=ALU.add, op1=ALU.mult), reads=[wmod, nwb], writes=[wmod])
        for tt in range(NT):
            xt = xts[tt % 2]
            hb = hbs[tt % 2]
            P.dma(lambda e, xt=xt, tt=tt: e.dma_start(out=xt[:], in_=x[tt * 128:(tt + 1) * 128, :]), writes=[xt])
            rmsnorm_mod_tile(P, xt, wmod, shb, hb, scr, ss[tt % 2], rstd[tt % 2])
            transpose_tile(P, hb, hT[tt], ident, ptr, evac)

        chunks = []
        for (o, n) in zip((O_MQ, O_MK, O_MV, O_MO, O_MG, O_AQ, O_AK, O_AV, O_GM, O_GA), IN_SIZES):
            if n == 16:
                chunks.append((o, 16, "mg"))
                continue
            kind = {O_MQ: "mq", O_AQ: "aq", O_AK: "ak"}.get(o, "plain")
            for c0 in range(o, o + n, 512):
                chunks.append((c0, 512, kind))
        wi = 0
        for (c0, ncol, kind) in chunks:
            if kind == "mg":
                w = wg
            else:
                w = wts[wi % 3]
                wi += 1
            src = win[:, c0:c0 + ncol].rearrange("(k p) n -> p k n", p=128)
            P.dma(lambda e, w=w, src=src: e.dma_start(out=w[:], in_=src), writes=[w], eng="gpsimd")
            oc = c0 if c0 < O_MG else c0 - 16
            for tt in range(NT):
                a = acc()
                for k in range(16):
                    P.op("tensor", lambda e, a=a, w=w, tt=tt, k=k, ncol=ncol: e.matmul(
                        a[:, 0:ncol], lhsT=hT[tt][:, k, :], rhs=w[:, k, :], start=(k == 0), stop=(k == 15)),
                        reads=[hT[tt], w], writes=[a])
                rows = slice(tt * 128, (tt + 1) * 128)
                if kind == "mg":
                    P.op("vector", lambda e, a=a: e.tensor_tensor(out=sg[:], in0=a[:, 0:16], in1=bgb[:], op=ALU.add),
                         reads=[a, bgb], writes=[sg])
                    P.dma(lambda e, rows=rows: e.dma_start(out=pg[rows, :], in_=sg[:]), reads=[sg])
                    continue
                st = stgr()
                if kind == "plain" or kind == "mq":
                    scl = 1.0 if kind == "plain" else float(M_QK) ** -0.5
                    eng = evac()
                    if eng == "scalar":
                        P.op("scalar", lambda e, a=a, st=st, scl=scl: e.activation(out=st[:], in_=a[:], func=AF.Copy,
                                                                                 scale=scl), reads=[a], writes=[st])
                    else:
                        P.op("vector", lambda e, a=a, st=st, scl=scl: e.tensor_scalar(
                            out=st[:], in0=a[:], scalar1=scl, scalar2=None, op0=ALU.mult), reads=[a], writes=[st])
                else:
                    scl = float(A_QK) ** -0.5 if kind == "aq" else 1.0
                    r = r32r()
                    t4 = rtr()
                    P.op("scalar", lambda e, a=a, r=r, scl=scl: e.activation(out=r[:], in_=a[:], func=AF.Copy, scale=scl),
                         reads=[a], writes=[r])
                    rv = lambda r: r[:].rearrange("p (g d) -> p g d", d=128)
                    cosv = lambda tt: cst[:, tt, 0:64].rearrange("p (g j) -> p g j", j=16)
                    sinv = lambda tt: cst[:, tt, 64:128].rearrange("p (g j) -> p g j", j=16)
                    P.op("vector", lambda e, r=r, t4=t4, tt=tt: e.tensor_tensor(out=t4[:, 0], in0=rv(r)[:, :, 0:16], in1=cosv(tt), op=ALU.mult),
                         reads=[r, cst], writes=[t4])
                    P.op("vector", lambda e, r=r, t4=t4, tt=tt: e.tensor_tensor(out=t4[:, 1], in0=rv(r)[:, :, 16:32], in1=sinv(tt), op=ALU.mult),
                         reads=[r, cst], writes=[t4])
                    P.op("gpsimd", lambda e, r=r, t4=t4, tt=tt: e.tensor_tensor(out=t4[:, 2], in0=rv(r)[:, :, 0:16], in1=sinv(tt), op=ALU.mult),
                         reads=[r, cst], writes=[t4])
                    P.op("gpsimd", lambda e, r=r, t4=t4, tt=tt: e.tensor_tensor(out=t4[:, 3], in0=rv(r)[:, :, 16:32], in1=cosv(tt), op=ALU.mult),
                         reads=[r, cst], writes=[t4])
                    P.op("vector", lambda e, r=r, t4=t4: e.tensor_tensor(out=rv(r)[:, :, 0:16], in0=t4[:, 0], in1=t4[:, 1], op=ALU.subtract),
                         reads=[t4, r], writes=[r])
                    P.op("vector", lambda e, r=r, t4=t4: e.tensor_tensor(out=rv(r)[:, :, 16:32], in0=t4[:, 2], in1=t4[:, 3], op=ALU.add),
                         reads=[t4, r], writes=[r])
                    P.op("gpsimd", lambda e, r=r, st=st: e.tensor_copy(out=st[:], in_=r[:]), reads=[r], writes=[st])
                P.dma(lambda e, rows=rows, oc=oc, st=st: e.dma_start(out=pb[rows, oc:oc + 512], in_=st[:]), reads=[st])
        P.finish()
        P.emit()
    return nc


def rope_tables():
    half = A_QK // 4 // 2
    inv = (500000.0 ** (-np.arange(0, 2 * half, 2, dtype=np.float32) / np.float32(2 * half))).astype(np.float32)
    ang = np.arange(S, dtype=np.float32)[:, None] * inv[None, :]
    cos = np.cos(ang).astype(np.float32)
    sin = np.sin(ang).astype(np.float32)
    return np.concatenate([np.tile(cos, (1, 4)), np.tile(sin, (1, 4))], axis=1)


def run_B(ncB, x2d, nw, sc, sh, w_in_l, bg):
    cs4 = rope_tables()
    idn = np.eye(128, dtype=np.float32).astype(NPBF)
    in_maps = []
    for i in range(NCORES):
        rows = slice(i * TL, (i + 1) * TL)
        in_maps.append({"x": np.ascontiguousarray(x2d[rows]), "nw": nw.reshape(1, D), "sc": sc.reshape(1, D),
                        "sh": sh.reshape(1, D), "win": w_in_l, "bg": bg.reshape(1, 16),
                        "cs4": np.ascontiguousarray(cs4[rows]), "idn": idn})
    res = _run(ncB, in_maps)
    pb = np.concatenate([r["pb"] for r in res], axis=0)
    pg = np.concatenate([r["pg"] for r in res], axis=0)
    return pb, pg


def build_C1():
    nc = _new_nc()
    qT = _din(nc, "qT", [2, 128, S], BF16)
    kT = _din(nc, "kT", [2, 128, S], BF16)
    v = _din(nc, "v", [S, A_V], BF16)
    lam4 = _din(nc, "lam4", [4, A_QK])
    an = _din(nc, "an", [1, A_V])
    li = _din(nc, "li", [1, 1])
    ya = _dout(nc, "ya", [S, A_V], BF16)
    NKB = S // 128
    QT = 256
    with ExitStack() as es:
        P = Prog(nc, es)
        qs = P.sb("qs", [128, 2, S], BF16)
        ks = P.sb("ks", [128, 2, S], BF16)
        vs = P.sb("vs", [128, NKB, A_V + 1], BF16)
        lb = P.sb("lb", [128, 4, A_QK], F32)
        lt = P.sb("lt", [128, 2, A_QK], F32)
        ls = P.sb("ls", [128, 2], F32)
        lib = P.sb("lib", [128, 1], F32)
        lam = P.sb("lam", [128, 1], F32)
        nlam = P.sb("nlam", [128, 1], F32)
        anb = P.sb("anb", [128, A_V], F32)
        sps_l = [P.ps("sps", [128, 512], F32) for _ in range(4)]
        acc = [[P.ps("acc", [128, 512], F32) for _ in range(2)] for _ in range(2)]
        pts_l = [P.sb("pt", [128, 512], BF16) for _ in range(4)]
        sps = RR(sps_l)
        pts = RR(pts_l)
        rz = [P.sb("rz", [128, 2], F32) for _ in range(2)]
        o0 = [P.sb("o0", [128, A_V], F32) for _ in range(2)]
        oo = [P.sb("oo", [128, A_V], F32) for _ in range(2)]
        sq = P.sb("sq", [128, A_V], F32)
        ss = [P.sb("ss", [128, 1], F32) for _ in range(2)]
        yb = [P.sb("yb", [128, A_V], BF16) for _ in range(2)]

        for c in range(2):
            P.dma(lambda e, c=c: e.dma_start(out=qs[:, c, :], in_=qT[c]), writes=[qs])
            P.dma(lambda e, c=c: e.dma_start(out=ks[:, c, :], in_=kT[c]), writes=[ks])
        P.dma(lambda e: e.dma_start(out=vs[:, :, 0:A_V], in_=v.rearrange("(kb p) c -> p kb c", p=128)), writes=[vs])
        P.op("gpsimd", lambda e: e.memset(vs[:, :, A_V:A_V + 1], 1.0), writes=[vs])
        for i in range(4):
            P.dma(lambda e, i=i: e.dma_start(out=lb[:, i, :], in_=lam4[i:i + 1, :].to_broadcast([128, A_QK])), writes=[lb])
        P.dma(lambda e: e.dma_start(out=anb[:], in_=an.to_broadcast([128, A_V])), writes=[anb])
        P.dma(lambda e: e.dma_start(out=lib[:], in_=li.to_broadcast([128, 1])), writes=[lib])
        for i in range(2):
            P.op("vector", lambda e, i=i: e.tensor_tensor(out=lt[:, i, :], in0=lb[:, 2 * i, :], in1=lb[:, 2 * i + 1, :],
                                                          op=ALU.mult), reads=[lb], writes=[lt])
            P.op("vector", lambda e, i=i: e.reduce_sum(out=ls[:, i:i + 1], in_=lt[:, i, :], axis=AX.X), reads=[lt], writes=[ls])
        P.op("scalar", lambda e: e.activation(out=ls[:], in_=ls[:], func=AF.Exp), reads=[ls], writes=[ls])
        P.op("vector", lambda e: e.tensor_tensor(out=lam[:], in0=ls[:, 0:1], in1=ls[:, 1:2], op=ALU.subtract),
             reads=[ls], writes=[lam])
        P.op("vector", lambda e: e.tensor_tensor(out=lam[:], in0=lam[:], in1=lib[:], op=ALU.add), reads=[lam, lib], writes=[lam])
        P.op("vector", lambda e: e.tensor_scalar(out=nlam[:], in0=lam[:], scalar1=-1.0, scalar2=None, op0=ALU.mult),
             reads=[lam], writes=[nlam])
        P.op("vector", lambda e: e.tensor_scalar(out=lib[:], in0=lib[:], scalar1=-1.0, scalar2=1.0, op0=ALU.mult, op1=ALU.add),
             reads=[lib], writes=[lib])
        P.op("vector", lambda e: e.tensor_scalar(out=anb[:], in0=anb[:], scalar1=lib[:, 0:1], scalar2=None, op0=ALU.mult),
             reads=[anb, lib], writes=[anb])

        accs = [[P.sb("accs", [128, A_V + 1], F32) for _ in range(2)] for _ in range(2)]
        iters = [(qt, kb) for qt in range(S // QT) for kb in range(NKB)]
        LOOK = 2

        def emit_scores(it):
            qt, kb = iters[it]
            q0 = qt * QT
            sp = sps_l[it % 4]
            for c in range(2):
                P.op("tensor", lambda e, sp=sp, c=c, kb=kb, q0=q0: e.matmul(
                    sp[:, c * QT:(c + 1) * QT], lhsT=ks[:, c, kb * 128:(kb + 1) * 128], rhs=qs[:, c, q0:q0 + QT],
                    start=True, stop=True), reads=[ks, qs], writes=[sp])
            pt = pts_l[it % 4]
            P.op("scalar", lambda e, sp=sp, pt=pt: e.activation(out=pt[:], in_=sp[:], func=AF.Exp), reads=[sp], writes=[pt])

        for it in range(min(LOOK, len(iters))):
            emit_scores(it)
        for it, (qt, kb) in enumerate(iters):
            q0 = qt * QT
            if it + LOOK < len(iters):
                emit_scores(it + LOOK)
            pt = pts_l[it % 4]
            for c in range(2):
                for j in range(2):
                    a = acc[c][j]
                    P.op("tensor", lambda e, a=a, pt=pt, c=c, j=j, kb=kb: e.matmul(
                        a[:, 0:A_V + 1], lhsT=pt[:, c * QT + j * 128:c * QT + (j + 1) * 128], rhs=vs[:, kb, :],
                        start=(kb == 0), stop=(kb == NKB - 1)), reads=[pt, vs], writes=[a])
            if kb != NKB - 1:
                continue
            for c in range(2):
                for j in range(2):
                    P.op("vector", lambda e, c=c, j=j: e.tensor_copy(out=accs[c][j][:], in_=acc[c][j][:, 0:A_V + 1]),
                         reads=[acc[c][j]], writes=[accs[c][j]])
            for j in range(2):
                a0, a1 = accs[0][j], accs[1][j]
                r, o_0, o_, s_, y_ = rz[j], o0[j], oo[j], ss[j], yb[j]
                P.op("vector", lambda e, a0=a0, r=r: e.reciprocal(out=r[:, 0:1], in_=a0[:, A_V:A_V + 1]), reads=[a0], writes=[r])
                P.op("vector", lambda e, a1=a1, r=r: e.reciprocal(out=r[:, 1:2], in_=a1[:, A_V:A_V + 1]), reads=[a1], writes=[r])
                P.op("gpsimd", lambda e, a0=a0, r=r, o_0=o_0: e.tensor_scalar(out=o_0[:], in0=a0[:, 0:A_V], scalar1=r[:, 0:1],
                                                                              scalar2=None, op0=ALU.mult), reads=[a0, r], writes=[o_0])
                P.op("vector", lambda e, r=r: e.tensor_tensor(out=r[:, 1:2], in0=r[:, 1:2], in1=nlam[:], op=ALU.mult),
                     reads=[r, nlam], writes=[r])
                P.op("vector", lambda e, a1=a1, r=r, o_0=o_0, o_=o_: e.scalar_tensor_tensor(
                    out=o_[:], in0=a1[:, 0:A_V], scalar=r[:, 1:2], in1=o_0[:], op0=ALU.mult, op1=ALU.add),
                    reads=[a1, r, o_0], writes=[o_])
                P.op("gpsimd", lambda e, o_=o_: e.tensor_tensor(out=sq[:], in0=o_[:], in1=o_[:], op=ALU.mult), reads=[o_], writes=[sq])
                P.op("vector", lambda e, s_=s_: e.reduce_sum(out=s_[:], in_=sq[:], axis=AX.X), reads=[sq], writes=[s_])
                P.op("vector", lambda e, s_=s_: e.tensor_scalar(out=s_[:], in0=s_[:], scalar1=1.0 / A_V, scalar2=EPS,
                                                               op0=ALU.mult, op1=ALU.add), reads=[s_], writes=[s_])
                P.op("scalar", lambda e, s_=s_: e.activation(out=s_[:], in_=s_[:], func=AF.Sqrt), reads=[s_], writes=[s_])
                P.op("vector", lambda e, s_=s_: e.reciprocal(out=s_[:], in_=s_[:]), reads=[s_], writes=[s_])
                P.op("vector", lambda e, o_=o_, s_=s_, y_=y_: e.scalar_tensor_tensor(
                    out=y_[:], in0=o_[:], scalar=s_[:, 0:1], in1=anb[:], op0=ALU.mult, op1=ALU.mult),
                    reads=[o_, s_, anb], writes=[y_])
                r0 = q0 + j * 128
                P.dma(lambda e, y_=y_, r0=r0: e.dma_start(out=ya[r0:r0 + 128, :], in_=y_[:]), reads=[y_])
        P.finish()
        P.emit()
    return nc


def run_C1(ncC1, pb, lam4, a_norm_l, lam_init):
    aq = pb[:, O_AQ - 16:O_AQ - 16 + 2048].reshape(S, A_HEADS, 2, A_QK)
    ak = pb[:, O_AK - 16:O_AK - 16 + 2048].reshape(S, A_HEADS, 2, A_QK)
    av = pb[:, O_AV - 16:O_AV - 16 + 2048].reshape(S, A_HEADS, A_V)
    in_maps = []
    for h in range(NCORES):
        in_maps.append({"qT": np.ascontiguousarray(aq[:, h].transpose(1, 2, 0)),
                        "kT": np.ascontiguousarray(ak[:, h].transpose(1, 2, 0)),
                        "v": np.ascontiguousarray(av[:, h]),
                        "lam4": lam4, "an": a_norm_l.reshape(1, A_V),
                        "li": np.full((1, 1), lam_init, np.float32)})
    res = _run(ncC1, in_maps)
    return np.concatenate([r["ya"] for r in res], axis=1)


def View(ap, name="", root=None):
    return Buf(ap, name, root)


def build_C2(nch=None, dbg=False):
    nc = _new_nc()
    qT = _din(nc, "qT", [M_QK, S], BF16)
    kT = _din(nc, "kT", [M_QK, S], BF16)
    kk = _din(nc, "kk", [S, M_QK], BF16)
    vv = _din(nc, "vv", [S, M_V], BF16)
    gi = _din(nc, "gi", [64, 128])
    gf = _din(nc, "gf", [64, 128])
    tri = _din(nc, "tri", [128, 128])
    cst = _din(nc, "cst", [64, 128])
    hd = _dout(nc, "hd", [S, M_V])
    gscr = _dout(nc, "gscr", [64, 128])
    NCH = S // 128
    dbgo = _dout(nc, "dbgo", [128, 4 * 64]) if dbg else None
    with ExitStack() as es:
        P = Prog(nc, es)
        g_i = P.sb("g_i", [64, 128], F32)
        g_f = P.sb("g_f", [64, 128], F32)
        Bw = P.sb("Bw", [64, 128], F32)
        Aw = P.sb("Aw", [64, 128], F32)
        o64 = P.sb("o64", [64, 128], F32)
        cs = P.sb("cs", [64, 128], F32)
        c1 = P.sb("c1", [64, 4], F32)
        rowM = P.sb("rowM", [1, 64], F32)
        r_gl = P.sb("r_gl", [1, NCH], F32)
        r_gp = P.sb("r_gp", [1, NCH], F32)
        r_G = P.sb("r_G", [1, S], F32)
        r_1 = P.sb("r_1", [1, 128], F32)
        one11 = P.sb("one11", [1, 1], F32)
        trim = P.sb("trim", [128, 128], F32)
        onesb = P.sb("onesb", [128, 1], BF16)
        A_col = P.sb("A_col", [128, NCH], F32)
        E_col = P.sb("E_col", [128, NCH], F32)
        GLB = P.sb("GLB", [128, NCH], F32)
        GPB = P.sb("GPB", [128, NCH], F32)
        WS = P.sb("WS", [128, NCH], F32)
        DEC = P.sb("DEC", [128, NCH], F32)
        Cst = [P.sb("C", [128, M_V], F32) for _ in range(2)]
        Cb = [P.sb("Cb", [128, M_V], BF16) for _ in range(2)]
        nst = P.sb("n", [128, 2], F32)
        nb = P.sb("nb", [128, 2], BF16)
        bk = [P.ps("bk", [128, 512], F32) for _ in range(4)]
        sT_l = [View(bk[i][:, 0:128], root=bk[i]) for i in range(2)]
        GmB_l = [View(bk[i][:, 128:256], root=bk[i]) for i in range(2)]
        Dn_l = [View(bk[2 + i][:, 0:1], root=bk[2 + i]) for i in range(2)]
        nS_l = [View(bk[2 + i][:, 2:4], root=bk[2 + i]) for i in range(2)]
        pc1 = View(bk[2][0:64, 4:5], root=bk[2])
        pc2 = View(bk[2][0:64, 5:6], root=bk[2])
        prow = View(bk[2][0:1, 64:128], root=bk[2])
        pcol = [View(bk[i][:, 256:320], root=bk[i]) for i in range(4)]
        N_l = [P.ps("N", [128, M_V], F32) for _ in range(2)]
        KV = [P.ps("KV", [128, M_V], F32) for _ in range(2)]
        kTc_l = [P.sb("kTc", [128, 2, 128], BF16) for _ in range(3)]
        qTc_l = [P.sb("qTc", [128, 2, 128], BF16) for _ in range(3)]
        kc_l = [P.sb("kc", [128, M_QK], BF16) for _ in range(3)]
        vc_l = [P.sb("vc", [128, M_V], BF16) for _ in range(3)]
        Dt_l = [P.sb("Dt", [128, 128], F32) for _ in range(2)]
        W_l = [P.sb("W", [128, 128], F32) for _ in range(2)]
        WI_l = [P.sb("WI", [128, 128], F32) for _ in range(2)]
        sw_l = [P.sb("sw", [128, 128], BF16) for _ in range(2)]
        qtl_l = [P.sb("qtl", [128, 2, 128], BF16) for _ in range(2)]
        ktl_l = [P.sb("ktl", [128, M_QK], BF16) for _ in range(2)]
        den_l = [P.sb("den", [128, 1], F32) for _ in range(2)]
        ho_l = [P.sb("ho", [128, M_V], F32) for _ in range(3)]

        P.dma(lambda e: e.dma_start(out=g_i[:], in_=gi[:, :]), writes=[g_i])
        P.dma(lambda e: e.dma_start(out=g_f[:], in_=gf[:, :]), writes=[g_f])
        P.dma(lambda e: e.dma_start(out=trim[:], in_=tri[:, :]), writes=[trim])
        P.dma(lambda e: e.dma_start(out=cs[:], in_=cst[:, :]), writes=[cs])
        P.op("gpsimd", lambda e: e.memset(r_1[:], 1.0), writes=[r_1])
        P.op("gpsimd", lambda e: e.memset(o64[:], 1.0), writes=[o64])
        P.op("gpsimd", lambda e: e.memset(one11[:], 1.0), writes=[one11])
        P.op("gpsimd", lambda e: e.memset(onesb[:], 1.0), writes=[onesb])
        for j in range(2):
            P.op("gpsimd", lambda e, j=j: e.memset(Cst[j][:], 0.0), writes=[Cst[j]])
            P.op("gpsimd", lambda e, j=j: e.memset(Cb[j][:], 0.0), writes=[Cb[j]])
        P.op("gpsimd", lambda e: e.memset(nst[:], 0.0), writes=[nst])
        P.op("gpsimd", lambda e: e.memset(nb[:], 0.0), writes=[nb])
        P.op("scalar", lambda e: e.activation(out=g_f[:], in_=g_f[:], func=AF.Exp, scale=-1.0), reads=[g_f], writes=[g_f])
        P.op("vector", lambda e: e.tensor_scalar(out=g_f[:], in0=g_f[:], scalar1=1.0, scalar2=None, op0=ALU.add),
             reads=[g_f], writes=[g_f])
        P.op("scalar", lambda e: e.activation(out=g_f[:], in_=g_f[:], func=AF.Ln), reads=[g_f], writes=[g_f])
        P.op("vector", lambda e: e.tensor_scalar(out=g_f[:], in0=g_f[:], scalar1=-1.0, scalar2=None, op0=ALU.mult),
             reads=[g_f], writes=[g_f])
        P.op("vector", lambda e: e.tensor_tensor_scan(out=Bw[:], data0=o64[:], data1=g_f[:], initial=0.0,
                                                      op0=ALU.mult, op1=ALU.add), reads=[o64, g_f], writes=[Bw])
        P.op("vector", lambda e: e.tensor_copy(out=c1[:, 0:1], in_=Bw[:, 127:128]), reads=[Bw], writes=[c1])
        P.op("tensor", lambda e: e.matmul(pc1[:], lhsT=cs[:, 64:128], rhs=c1[:, 0:1], start=True, stop=True),
             reads=[cs, c1], writes=[pc1])
        P.op("vector", lambda e: e.tensor_copy(out=c1[:, 1:2], in_=pc1[:]), reads=[pc1], writes=[c1])
        P.op("vector", lambda e: e.tensor_scalar(out=Bw[:], in0=Bw[:], scalar1=c1[:, 1:2], scalar2=None, op0=ALU.add),
             reads=[Bw, c1], writes=[Bw])
        P.op("vector", lambda e: e.tensor_tensor(out=g_i[:], in0=g_i[:], in1=Bw[:], op=ALU.subtract),
             reads=[g_i, Bw], writes=[g_i])
        P.op("vector", lambda e: e.tensor_tensor_scan(out=Aw[:], data0=g_i[:], data1=g_i[:], initial=-1.0e30,
                                                      op0=ALU.max, op1=ALU.max), reads=[g_i], writes=[Aw])
        P.op("vector", lambda e: e.tensor_copy(out=c1[:, 2:3], in_=Aw[:, 127:128]), reads=[Aw], writes=[c1])
        P.op("tensor", lambda e: e.matmul(prow[:], lhsT=c1[:, 2:3], rhs=cs[:, 0:64], start=True, stop=True),
             reads=[cs, c1], writes=[prow])
        P.op("vector", lambda e: e.tensor_copy(out=rowM[:], in_=prow[:]), reads=[prow], writes=[rowM])
        P.op("vector", lambda e: e.tensor_tensor_scan(out=r_gl[:], data0=rowM[:], data1=rowM[:], initial=0.0,
                                                      op0=ALU.max, op1=ALU.max), reads=[rowM], writes=[r_gl])
        P.op("gpsimd", lambda e: e.memset(r_gp[:, 0:1], 0.0), writes=[r_gp])
        P.op("vector", lambda e: e.tensor_copy(out=r_gp[:, 1:NCH], in_=r_gl[:, 0:NCH - 1]), reads=[r_gl, r_gp], writes=[r_gp])
        P.op("tensor", lambda e: e.matmul(pc2[:], lhsT=r_gp[0:1, :], rhs=one11[0:1, 0:1], start=True, stop=True),
             reads=[r_gp, one11], writes=[pc2])
        P.op("vector", lambda e: e.tensor_copy(out=c1[:, 3:4], in_=pc2[:]), reads=[pc2], writes=[c1])
        P.op("vector", lambda e: e.tensor_scalar(out=Aw[:], in0=Aw[:], scalar1=c1[:, 3:4], scalar2=None, op0=ALU.max),
             reads=[Aw, c1], writes=[Aw])
        P.op("vector", lambda e: e.tensor_tensor(out=Bw[:], in0=Bw[:], in1=Aw[:], op=ALU.add), reads=[Bw, Aw], writes=[Bw])
        P.op("scalar", lambda e: e.activation(out=Bw[:], in_=Bw[:], func=AF.Exp, scale=-1.0), reads=[Bw], writes=[Bw])
        gs = View(gscr)
        P.dma(lambda e: e.dma_start(out=gscr[:, :], in_=Aw[:]), reads=[Aw], writes=[gs])
        P.dma(lambda e: e.dma_start(out=r_G[:], in_=gscr.rearrange("(o c) t -> o (c t)", o=1)), reads=[gs], writes=[r_G])
        for (src, col, pc) in ((g_i, A_col, pcol[0]), (Bw, E_col, pcol[1])):
            P.op("tensor", lambda e, src=src, pc=pc: e.transpose(out=pc[:], in_=src[:], identity=cs[:, 0:64]),
                 reads=[src, cs], writes=[pc])
            P.op("vector", lambda e, col=col, pc=pc: e.tensor_copy(out=col[:], in_=pc[:]), reads=[pc], writes=[col])
        for (row, col, pc) in ((r_gl, GLB, pcol[2]), (r_gp, GPB, pcol[3])):
            P.op("tensor", lambda e, row=row, pc=pc: e.matmul(pc[:], lhsT=r_1[0:1, 0:128], rhs=row[0:1, :], start=True, stop=True),
                 reads=[row, r_1], writes=[pc])
            P.op("vector", lambda e, col=col, pc=pc: e.tensor_copy(out=col[:], in_=pc[:]), reads=[pc], writes=[col])
        P.op("vector", lambda e: e.tensor_tensor(out=WS[:], in0=A_col[:], in1=GLB[:], op=ALU.subtract), reads=[A_col, GLB], writes=[WS])
        P.op("scalar", lambda e: e.activation(out=WS[:], in_=WS[:], func=AF.Exp), reads=[WS], writes=[WS])
        P.op("vector", lambda e: e.tensor_tensor(out=DEC[:], in0=GPB[:], in1=GLB[:], op=ALU.subtract), reads=[GPB, GLB], writes=[DEC])
        P.op("scalar", lambda e: e.activation(out=DEC[:], in_=DEC[:], func=AF.Exp), reads=[DEC], writes=[DEC])

        if dbg:
            for i, col in enumerate((A_col, E_col, GLB, GPB)):
                P.dma(lambda e, i=i, col=col: e.dma_start(out=dbgo[:, i * 64:(i + 1) * 64], in_=col[:]), reads=[col])
        for c in range(NCH if nch is None else nch):
            t0 = c * 128
            kTc, qTc, kc, vc = kTc_l[c % 3], qTc_l[c % 3], kc_l[c % 3], vc_l[c % 3]
            P.dma(lambda e, kTc=kTc, t0=t0: e.dma_start(out=kTc[:], in_=kT[:, t0:t0 + 128].rearrange("(j p) t -> p j t", p=128)), writes=[kTc])
            P.dma(lambda e, qTc=qTc, t0=t0: e.dma_start(out=qTc[:], in_=qT[:, t0:t0 + 128].rearrange("(j p) t -> p j t", p=128)), writes=[qTc])
            P.dma(lambda e, kc=kc, t0=t0: e.dma_start(out=kc[:], in_=kk[t0:t0 + 128, :]), writes=[kc])
            P.dma(lambda e, vc=vc, t0=t0: e.dma_start(out=vc[:], in_=vv[t0:t0 + 128, :]), writes=[vc])
            sT, GmB, Dn, Np, nS = sT_l[c % 2], GmB_l[c % 2], Dn_l[c % 2], N_l[c % 2], nS_l[c % 2]
            Dt, W, WI, sw, qtl, ktl, den, ho = Dt_l[c % 2], W_l[c % 2], WI_l[c % 2], sw_l[c % 2], qtl_l[c % 2], ktl_l[c % 2], den_l[c % 2], ho_l[c % 3]
            for j in range(2):
                P.op("tensor", lambda e, sT=sT, kTc=kTc, qTc=qTc, j=j: e.matmul(sT[:], lhsT=kTc[:, j, :], rhs=qTc[:, j, :],
                                                                               start=(j == 0), stop=(j == 1)),
                     reads=[kTc, qTc], writes=[sT])
            P.op("tensor", lambda e, GmB=GmB, t0=t0: e.matmul(GmB[:], lhsT=r_1[0:1, 0:128], rhs=r_G[0:1, t0:t0 + 128],
                                                              start=True, stop=True), reads=[r_1, r_G], writes=[GmB])
            P.op("vector", lambda e, Dt=Dt, GmB=GmB, c=c: e.tensor_scalar(out=Dt[:], in0=GmB[:], scalar1=A_col[:, c:c + 1], scalar2=0.0,
                                                                          op0=ALU.subtract, op1=ALU.max), reads=[GmB, A_col], writes=[Dt])
            P.op("scalar", lambda e, Dt=Dt, W=W: e.activation(out=W[:], in_=Dt[:], func=AF.Exp, scale=-1.0), reads=[Dt], writes=[W])
            P.op("gpsimd", lambda e, W=W: e.tensor_tensor(out=W[:], in0=W[:], in1=trim[:], op=ALU.mult), reads=[W, trim], writes=[W])
            P.op("vector", lambda e, sw=sw, sT=sT, W=W: e.tensor_tensor(out=sw[:], in0=sT[:], in1=W[:], op=ALU.mult),
                 reads=[sT, W], writes=[sw])
            P.op("scalar", lambda e, WI=WI, GmB=GmB, c=c: e.activation(out=WI[:], in_=GmB[:], func=AF.Exp, scale=-1.0,
                                                                      bias=GPB[:, c:c + 1]), reads=[GmB, GPB], writes=[WI])
            for j in range(2):
                P.op("gpsimd" if j == 0 else "vector", lambda e, qtl=qtl, qTc=qTc, WI=WI, j=j: e.tensor_tensor(
                    out=qtl[:, j, :], in0=qTc[:, j, :], in1=WI[:], op=ALU.mult), reads=[qTc, WI], writes=[qtl])
            P.op("tensor", lambda e, Np=Np, sw=sw, vc=vc: e.matmul(Np[:], lhsT=sw[:], rhs=vc[:], start=True, stop=False),
                 reads=[sw, vc], writes=[Np])
            for j in range(2):
                P.op("tensor", lambda e, Np=Np, qtl=qtl, j=j: e.matmul(Np[:], lhsT=qtl[:, j, :], rhs=Cb[j][:], start=False, stop=(j == 1)),
                     reads=[qtl, Cb[j]], writes=[Np])
            P.op("tensor", lambda e, Dn=Dn, sw=sw: e.matmul(Dn[:], lhsT=sw[:], rhs=onesb[:], start=True, stop=False),
                 reads=[sw, onesb], writes=[Dn])
            for j in range(2):
                P.op("tensor", lambda e, Dn=Dn, qtl=qtl, j=j: e.matmul(Dn[:], lhsT=qtl[:, j, :], rhs=nb[:, j:j + 1], start=False, stop=(j == 1)),
                     reads=[qtl, nb], writes=[Dn])
            P.op("scalar", lambda e, den=den, Dn=Dn: e.activation(out=den[:], in_=Dn[:], func=AF.Abs), reads=[Dn], writes=[den])
            P.op("vector", lambda e, den=den, c=c: e.tensor_scalar(out=den[:], in0=den[:], scalar1=E_col[:, c:c + 1], scalar2=None,
                                                                   op0=ALU.max), reads=[den, E_col], writes=[den])
            P.op("vector", lambda e, den=den: e.reciprocal(out=den[:], in_=den[:]), reads=[den], writes=[den])
            P.op("scalar", lambda e, ho=ho, Np=Np, den=den: e.activation(out=ho[:], in_=Np[:], func=AF.Copy, scale=den[:, 0:1]),
                 reads=[Np, den], writes=[ho])
            P.dma(lambda e, ho=ho, t0=t0: e.dma_start(out=hd[t0:t0 + 128, :], in_=ho[:]), reads=[ho])
            P.op("gpsimd", lambda e, ktl=ktl, kc=kc, c=c: e.tensor_scalar(out=ktl[:], in0=kc[:], scalar1=WS[:, c:c + 1], scalar2=None,
                                                                          op0=ALU.mult), reads=[kc, WS], writes=[ktl])
            for j in range(2):
                P.op("tensor", lambda e, ktl=ktl, vc=vc, j=j: e.matmul(KV[j][:], lhsT=ktl[:, j * 128:(j + 1) * 128], rhs=vc[:],
                                                                      start=True, stop=True), reads=[ktl, vc], writes=[KV[j]])
            for j in range(2):
                P.op("tensor", lambda e, ktl=ktl, j=j, nS=nS: e.matmul(nS[:, j:j + 1], lhsT=ktl[:, j * 128:(j + 1) * 128], rhs=onesb[:],
                                                               start=True, stop=True), reads=[ktl, onesb], writes=[nS])
            for j in range(2):
                P.op("vector", lambda e, j=j, c=c: e.scalar_tensor_tensor(out=Cst[j][:], in0=Cst[j][:], scalar=DEC[:, c:c + 1], in1=KV[j][:],
                                                                          op0=ALU.mult, op1=ALU.add), reads=[Cst[j], DEC, KV[j]], writes=[Cst[j]])
                P.op("scalar", lambda e, j=j: e.activation(out=Cb[j][:], in_=Cst[j][:], func=AF.Copy), reads=[Cst[j]], writes=[Cb[j]])
            P.op("vector", lambda e, c=c, nS=nS: e.scalar_tensor_tensor(out=nst[:], in0=nst[:], scalar=DEC[:, c:c + 1], in1=nS[:],
                                                                 op0=ALU.mult, op1=ALU.add), reads=[nst, DEC, nS], writes=[nst])
            P.op("vector", lambda e: e.tensor_copy(out=nb[:], in_=nst[:]), reads=[nst], writes=[nb])
        P.finish()
        P.emit()
    return nc


def run_C2(ncC2, pb, pg):
    mq = pb[:, O_MQ:O_MQ + 1024].reshape(S, M_HEADS, M_QK)
    mk = pb[:, O_MK:O_MK + 1024].reshape(S, M_HEADS, M_QK)
    mv = pb[:, O_MV:O_MV + 2048].reshape(S, M_HEADS, M_V)
    g = pg.reshape(S, 4, M_HEADS)
    tri = np.triu(np.ones((128, 128), np.float32))
    cst = np.concatenate([np.eye(64, dtype=np.float32), np.triu(np.ones((64, 64), np.float32), 1)], axis=1)
    in_maps = []
    for core in range(NCORES):
        h, d = core // 2, core % 2
        fl = (lambda a: a[::-1]) if d == 1 else (lambda a: a)
        in_maps.append({"qT": np.ascontiguousarray(fl(mq[:, h]).T), "kT": np.ascontiguousarray(fl(mk[:, h]).T),
                        "kk": np.ascontiguousarray(fl(mk[:, h])), "vv": np.ascontiguousarray(fl(mv[:, h])),
                        "gi": np.ascontiguousarray(fl(g[:, 2 * d, h])).reshape(64, 128),
                        "gf": np.ascontiguousarray(fl(g[:, 2 * d + 1, h])).reshape(64, 128), "tri": tri, "cst": cst})
    res = _run(ncC2, in_maps)
    hfw = np.concatenate([res[2 * h]["hd"] for h in range(M_HEADS)], axis=1)
    hbw = np.concatenate([res[2 * h + 1]["hd"][::-1] for h in range(M_HEADS)], axis=1)
    return hfw, np.ascontiguousarray(hbw)


def build_D():
    nc = _new_nc()
    x = _din(nc, "x", [TL, D])
    hfw = _din(nc, "hfw", [TL, D])
    hbw = _din(nc, "hbw", [TL, D])
    ya = _din(nc, "ya", [TL, D], BF16)
    mo = _din(nc, "mo", [TL, D], BF16)
    gmi = _din(nc, "gm", [TL, D], BF16)
    gai = _din(nc, "ga", [TL, D], BF16)
    vecs = _din(nc, "vecs", [5, D])
    wbm = _din(nc, "wbm", [D, D])
    wba = _din(nc, "wba", [D, D])
    wo = _din(nc, "wo", [D, D])
    wr = _din(nc, "wr", [D, NE])
    br = _din(nc, "br", [1, NE])
    idn = _din(nc, "idn", [128, 128], BF16)
    idf = _din(nc, "idf", [128, 128])
    x1o = _dout(nc, "x1", [TL, D])
    h2o = _dout(nc, "h2", [TL, D], BF16)
    Go = _dout(nc, "G", [TL, NE])
    GT = 2
    NG = TL // 128 // GT
    with ExitStack() as es:
        P = Prog(nc, es)
        ident = P.sb("ident", [128, 128], BF16)
        identf = P.sb("identf", [128, 128], F32)
        mnb = P.sb("mnb", [128, D], F32)
        gmb = P.sb("gmb", [128, D], F32)
        wmod = P.sb("wmod", [128, D], F32)
        shb = P.sb("shb", [128, D], F32)
        wrs = P.sb("wrs", [128, 16, NE], F32)
        brb = P.sb("brb", [128, NE], F32)
        tA = [P.sb("tA", [128, D], F32)] * 2
        tB = [P.sb("tB", [128, D], F32)] * 2
        tC = P.sb("tC", [128, D], F32)
        mot = [P.sb("mot", [128, D], BF16)] * 2
        yat = [P.sb("yat", [128, D], BF16)] * 2
        ymb = [P.sb("ymb", [128, D], BF16) for _ in range(2)]
        ss4 = [P.sb("ss4", [128, 4], F32) for _ in range(2)]
        ymT = [P.sb("ymT", [128, 16, 128], BF16) for _ in range(GT)]
        yaT = [P.sb("yaT", [128, 16, 128], BF16) for _ in range(GT)]
        mrg = [P.sb("mrg", [128, D], BF16) for _ in range(GT)]
        xt = [P.sb("xt", [128, D], F32) for _ in range(GT)]
        wts = [P.sb("wt", [128, 16, 512], BF16) for _ in range(3)]
        gch = [P.sb("gch", [128, 512], BF16) for _ in range(4)]
        sgc = [P.sb("sgc", [128, 512], F32) for _ in range(4)]
        t12 = [P.sb("t12", [128, 512], F32) for _ in range(4)]
        ssr = [P.sb("ssr", [128, 1], F32) for _ in range(2)]
        rstd = [P.sb("rstd", [128, 1], F32) for _ in range(2)]
        lg = [P.sb("lg", [128, NE], F32) for _ in range(2)]
        mx8 = [P.sb("mx8", [128, 8], F32) for _ in range(2)]
        msk = [P.sb("msk", [128, NE], F32) for _ in range(2)]
        ex = [P.sb("ex", [128, NE], F32) for _ in range(2)]
        zz = [P.sb("zz", [128, 2], F32) for _ in range(2)]
        ptr_l = [P.ps("ptr", [128, 1024], BF16) for _ in range(2)]
        acc_l = [P.ps("acc", [128, 512], F32) for _ in range(4)]
        ptf_l = [P.ps("ptf", [128, 512], F32) for _ in range(2)]
        ptr, acc, ptf = RR(ptr_l), RR(acc_l), RR(ptf_l)
        wtr, gchr, sgcr, t12r = RR(wts), RR(gch), RR(sgc), RR(t12)
        evac = RR(["vector"])

        P.dma(lambda e: e.dma_start(out=ident[:], in_=idn[:, :]), writes=[ident])
        P.dma(lambda e: e.dma_start(out=identf[:], in_=idf[:, :]), writes=[identf])
        P.dma(lambda e: e.dma_start(out=mnb[:], in_=vecs[0:1, :].to_broadcast([128, D])), writes=[mnb])
        P.dma(lambda e: e.dma_start(out=gmb[:], in_=vecs[1:2, :].to_broadcast([128, D])), writes=[gmb])
        P.dma(lambda e: e.dma_start(out=tC[:], in_=vecs[2:3, :].to_broadcast([128, D])), writes=[tC])
        P.dma(lambda e: e.dma_start(out=wmod[:], in_=vecs[3:4, :].to_broadcast([128, D])), writes=[wmod])
        P.dma(lambda e: e.dma_start(out=shb[:], in_=vecs[4:5, :].to_broadcast([128, D])), writes=[shb])
        P.dma(lambda e: e.dma_start(out=wrs[:], in_=wr.rearrange("(k p) n -> p k n", p=128)), writes=[wrs])
        P.dma(lambda e: e.dma_start(out=brb[:], in_=br.to_broadcast([128, NE])), writes=[brb])
        P.op("vector", lambda e: e.scalar_tensor_tensor(out=wmod[:], in0=wmod[:], scalar=1.0, in1=tC[:],
                                                        op0=ALU.add, op1=ALU.mult), reads=[wmod, tC], writes=[wmod])

        for g in range(NG):
            for i in range(GT):
                r0 = (g * GT + i) * 128
                rows = slice(r0, r0 + 128)
                a, b, m_, y_, yb_, s4 = tA[i % 2], tB[i % 2], mot[i % 2], yat[i % 2], ymb[i % 2], ss4[i % 2]
                P.dma(lambda e, a=a, rows=rows: e.dma_start(out=a[:], in_=hfw[rows, :]), writes=[a])
                P.dma(lambda e, b=b, rows=rows: e.dma_start(out=b[:], in_=hbw[rows, :]), writes=[b])
                P.dma(lambda e, m_=m_, rows=rows: e.dma_start(out=m_[:], in_=mo[rows, :]), writes=[m_])
                P.dma(lambda e, y_=y_, rows=rows: e.dma_start(out=y_[:], in_=ya[rows, :]), writes=[y_])
                P.dma(lambda e, i=i, rows=rows: e.dma_start(out=xt[i][:], in_=x[rows, :]), writes=[xt[i]])
                P.op("gpsimd", lambda e, a=a, b=b: e.tensor_tensor(out=a[:], in0=a[:], in1=b[:], op=ALU.add), reads=[a, b], writes=[a])
                for h in range(M_HEADS):
                    hs = slice(h * M_V, (h + 1) * M_V)
                    P.op("scalar", lambda e, a=a, b=b, s4=s4, h=h, hs=hs: e.activation(out=b[:, hs], in_=a[:, hs], func=AF.Square,
                                                                                     accum_out=s4[:, h:h + 1]), reads=[a], writes=[b, s4])
                P.op("vector", lambda e, s4=s4: e.tensor_scalar(out=s4[:], in0=s4[:], scalar1=1.0 / M_V, scalar2=EPS, op0=ALU.mult, op1=ALU.add),
                     reads=[s4], writes=[s4])
                P.op("scalar", lambda e, s4=s4: e.activation(out=s4[:], in_=s4[:], func=AF.Sqrt), reads=[s4], writes=[s4])
                P.op("vector", lambda e, s4=s4: e.reciprocal(out=s4[:], in_=s4[:]), reads=[s4], writes=[s4])
                for h in range(M_HEADS):
                    hs = slice(h * M_V, (h + 1) * M_V)
                    P.op("vector", lambda e, a=a, s4=s4, h=h, hs=hs: e.scalar_tensor_tensor(
                        out=a[:, hs], in0=a[:, hs], scalar=s4[:, h:h + 1], in1=mnb[:, hs], op0=ALU.mult, op1=ALU.mult),
                        reads=[a, s4, mnb], writes=[a])
                P.op("scalar", lambda e, m_=m_: e.activation(out=tC[:], in_=m_[:], func=AF.Sigmoid), reads=[m_], writes=[tC])
                P.op("gpsimd", lambda e, a=a, yb_=yb_: e.tensor_tensor(out=yb_[:], in0=a[:], in1=tC[:], op=ALU.mult), reads=[a, tC], writes=[yb_])
                transpose_tile(P, yb_, ymT[i], ident, ptr, evac)
                transpose_tile(P, y_, yaT[i], ident, ptr, evac)
            for n in range(4):
                cols = slice(n * 512, (n + 1) * 512)
                w1, w2 = wtr(), wtr()
                P.dma(lambda e, w1=w1, cols=cols: e.dma_start(out=w1[:], in_=wbm[:, cols].rearrange("(k p) n -> p k n", p=128)),
                      writes=[w1], eng="gpsimd")
                P.dma(lambda e, w2=w2, cols=cols: e.dma_start(out=w2[:], in_=wba[:, cols].rearrange("(k p) n -> p k n", p=128)),
                      writes=[w2], eng="gpsimd")
                for i in range(GT):
                    r0 = (g * GT + i) * 128
                    rows = slice(r0, r0 + 128)
                    a1, a2 = acc(), acc()
                    for k in range(16):
                        P.op("tensor", lambda e, a1=a1, w1=w1, i=i, k=k: e.matmul(a1[:], lhsT=ymT[i][:, k, :], rhs=w1[:, k, :],
                                                                                start=(k == 0), stop=(k == 15)), reads=[ymT[i], w1], writes=[a1])
                    for k in range(16):
                        P.op("tensor", lambda e, a2=a2, w2=w2, i=i, k=k: e.matmul(a2[:], lhsT=yaT[i][:, k, :], rhs=w2[:, k, :],
                                                                                start=(k == 0), stop=(k == 15)), reads=[yaT[i], w2], writes=[a2])
                    g1, g2, s1, s2, t1, t2 = gchr(), gchr(), sgcr(), sgcr(), t12r(), t12r()
                    P.dma(lambda e, g1=g1, rows=rows, cols=cols: e.dma_start(out=g1[:], in_=gmi[rows, cols]), writes=[g1])
                    P.dma(lambda e, g2=g2, rows=rows, cols=cols: e.dma_start(out=g2[:], in_=gai[rows, cols]), writes=[g2])
                    P.op("scalar", lambda e, g1=g1, s1=s1: e.activation(out=s1[:], in_=g1[:], func=AF.Sigmoid), reads=[g1], writes=[s1])
                    P.op("scalar", lambda e, g2=g2, s2=s2: e.activation(out=s2[:], in_=g2[:], func=AF.Sigmoid), reads=[g2], writes=[s2])
                    P.op("vector", lambda e, a1=a1, s1=s1, t1=t1: e.tensor_tensor(out=t1[:], in0=a1[:], in1=s1[:], op=ALU.mult), reads=[a1, s1], writes=[t1])
                    P.op("vector", lambda e, a2=a2, s2=s2, t2=t2: e.tensor_tensor(out=t2[:], in0=a2[:], in1=s2[:], op=ALU.mult), reads=[a2, s2], writes=[t2])
                    P.op("gpsimd", lambda e, t1=t1, t2=t2, i=i, cols=cols: e.tensor_tensor(out=mrg[i][:, cols], in0=t1[:], in1=t2[:], op=ALU.add),
                         reads=[t1, t2], writes=[mrg[i]])
            for i in range(GT):
                transpose_tile(P, mrg[i], ymT[i], ident, ptr, evac)
            for n in range(4):
                cols = slice(n * 512, (n + 1) * 512)
                w1 = wtr()
                P.dma(lambda e, w1=w1, cols=cols: e.dma_start(out=w1[:], in_=wo[:, cols].rearrange("(k p) n -> p k n", p=128)),
                      writes=[w1], eng="gpsimd")
                for i in range(GT):
                    a1 = acc()
                    for k in range(16):
                        P.op("tensor", lambda e, a1=a1, w1=w1, i=i, k=k: e.matmul(a1[:], lhsT=ymT[i][:, k, :], rhs=w1[:, k, :],
                                                                                start=(k == 0), stop=(k == 15)), reads=[ymT[i], w1], writes=[a1])
                    t1 = t12r()
                    P.op("vector", lambda e, a1=a1, t1=t1, cols=cols: e.tensor_tensor(out=t1[:], in0=a1[:], in1=gmb[:, cols], op=ALU.mult),
                         reads=[a1, gmb], writes=[t1])
                    P.op("gpsimd", lambda e, t1=t1, i=i, cols=cols: e.tensor_tensor(out=xt[i][:, cols], in0=xt[i][:, cols], in1=t1[:], op=ALU.add),
                         reads=[t1, xt[i]], writes=[xt[i]])
            for i in range(GT):
                r0 = (g * GT + i) * 128
                rows = slice(r0, r0 + 128)
                scr, h2f, h2b = tA[i % 2], tB[i % 2], ymb[i % 2]
                P.dma(lambda e, i=i, rows=rows: e.dma_start(out=x1o[rows, :], in_=xt[i][:]), reads=[xt[i]])
                rmsnorm_mod_tile(P, xt[i], wmod, shb, h2f, scr, ssr[i % 2], rstd[i % 2])
                P.op("scalar", lambda e, h2f=h2f, h2b=h2b: e.activation(out=h2b[:], in_=h2f[:], func=AF.Copy), reads=[h2f], writes=[h2b])
                P.dma(lambda e, h2b=h2b, rows=rows: e.dma_start(out=h2o[rows, :], in_=h2b[:]), reads=[h2b])
                for q4 in range(4):
                    pt = ptf()
                    for j in range(4):
                        k = q4 * 4 + j
                        P.op("tensor", lambda e, pt=pt, j=j, k=k, h2f=h2f: e.transpose(out=pt[:, j * 128:(j + 1) * 128],
                                                                                   in_=h2f[:, k * 128:(k + 1) * 128], identity=identf[:]),
                             reads=[h2f, identf], writes=[pt])
                    P.op("scalar" if q4 % 2 == 0 else "vector",
                         (lambda e, pt=pt, q4=q4: e.activation(out=tC[:, q4 * 512:(q4 + 1) * 512], in_=pt[:], func=AF.Copy)) if q4 % 2 == 0 else
                         (lambda e, pt=pt, q4=q4: e.tensor_copy(out=tC[:, q4 * 512:(q4 + 1) * 512], in_=pt[:])),
                         reads=[pt], writes=[tC])
                a1 = acc()
                for k in range(16):
                    P.op("tensor", lambda e, a1=a1, k=k: e.matmul(a1[:, 0:NE], lhsT=tC[:, k * 128:(k + 1) * 128], rhs=wrs[:, k, :],
                                                               start=(k == 0), stop=(k == 15)), reads=[tC, wrs], writes=[a1])
                l_, m8, mk, e_, z_ = lg[i % 2], mx8[i % 2], msk[i % 2], ex[i % 2], zz[i % 2]
                P.op("vector", lambda e, a1=a1, l_=l_: e.tensor_tensor(out=l_[:], in0=a1[:, 0:NE], in1=brb[:], op=ALU.add), reads=[a1, brb], writes=[l_])
                P.op("vector", lambda e, l_=l_, m8=m8: e.max(out=m8[:], in_=l_[:]), reads=[l_], writes=[m8])
                P.op("vector", lambda e, l_=l_, m8=m8, mk=mk: e.tensor_scalar(out=mk[:], in0=l_[:], scalar1=m8[:, 3:4], scalar2=None, op0=ALU.is_ge),
                     reads=[l_, m8], writes=[mk])
                P.op("vector", lambda e, m8=m8, z_=z_: e.tensor_scalar(out=z_[:, 0:1], in0=m8[:, 0:1], scalar1=-1.0, scalar2=None, op0=ALU.mult),
                     reads=[m8], writes=[z_])
                P.op("scalar", lambda e, l_=l_, e_=e_, z_=z_: e.activation(out=e_[:], in_=l_[:], func=AF.Exp, bias=z_[:, 0:1]), reads=[l_, z_], writes=[e_])
                P.op("vector", lambda e, e_=e_, mk=mk: e.tensor_tensor(out=e_[:], in0=e_[:], in1=mk[:], op=ALU.mult), reads=[e_, mk], writes=[e_])
                P.op("vector", lambda e, e_=e_, z_=z_: e.reduce_sum(out=z_[:, 1:2], in_=e_[:], axis=AX.X), reads=[e_], writes=[z_])
                P.op("vector", lambda e, z_=z_: e.reciprocal(out=z_[:, 1:2], in_=z_[:, 1:2]), reads=[z_], writes=[z_])
                P.op("vector", lambda e, e_=e_, z_=z_: e.tensor_scalar(out=e_[:], in0=e_[:], scalar1=z_[:, 1:2], scalar2=None, op0=ALU.mult),
                     reads=[e_, z_], writes=[e_])
                P.dma(lambda e, e_=e_, rows=rows: e.dma_start(out=Go[rows, :], in_=e_[:]), reads=[e_])
        P.finish()
        P.emit()
    return nc


def run_D(ncD, x2d, hfw, hbw, ya, pb, m_norm_l, g_m, norm_ffn_l, sc_f, sh_f, w_br_m, w_br_a, w_out, w_router, b_router):
    vecs = np.stack([m_norm_l.reshape(D), g_m, norm_ffn_l, sc_f, sh_f]).astype(np.float32)
    idn = np.eye(128, dtype=np.float32).astype(NPBF)
    idf = np.eye(128, dtype=np.float32)
    mo = pb[:, O_MO:O_MO + 2048]
    gm = pb[:, O_GM - 16:O_GM - 16 + 2048]
    ga = pb[:, O_GA - 16:O_GA - 16 + 2048]
    in_maps = []
    for i in range(NCORES):
        rows = slice(i * TL, (i + 1) * TL)
        c = np.ascontiguousarray
        in_maps.append({"x": c(x2d[rows]), "hfw": c(hfw[rows]), "hbw": c(hbw[rows]), "ya": c(ya[rows]), "mo": c(mo[rows]),
                        "gm": c(gm[rows]), "ga": c(ga[rows]), "vecs": vecs, "wbm": w_br_m, "wba": w_br_a, "wo": w_out,
                        "wr": w_router, "br": b_router.reshape(1, NE), "idn": idn, "idf": idf})
    res = _run(ncD, in_maps)
    x1 = np.concatenate([r["x1"] for r in res], axis=0)
    h2 = np.concatenate([r["h2"] for r in res], axis=0)
    G = np.concatenate([r["G"] for r in res], axis=0)
    return x1, h2, G


SPC = BPC * EBLK
SERIAL_SCATTER = False
BIGI = 1000000.0


def build_E():
    nc = _new_nc()
    GTi = _din(nc, "GT", [NE, S])
    h2 = _din(nc, "h2", [S, D], BF16)
    wgu = _din(nc, "wgu", [NE * D * 2, 2048])
    wdn = _din(nc, "wdn", [NE * DFF, D])
    bgu = _din(nc, "bgu", [NE * 2, 2048])
    bdn = _din(nc, "bdn", [NE, D])
    idn = _din(nc, "idn", [128, 128], BF16)
    idf = _din(nc, "idf", [128, 128])
    cst = _din(nc, "cst", [NE, NE + BPC + 32])
    jc = _din(nc, "jc", [128, 48])
    tokid = _din(nc, "tokid", [128, S // 128], I32)
    basei = _din(nc, "base", [128, 2])
    ys = _dout(nc, "ys", [SPC, D], BF16)
    destMo = _dout(nc, "destM", [NE, S])
    stok = _dout(nc, "stok", [SPC + 128, 1], I32)
    NTT = S // 128
    with ExitStack() as es:
        P = Prog(nc, es)
        Wr = [P.sb("W", [128, 16, 2048], BF16) for _ in range(2)]
        Wj = [[View(Wr[r][:, j, :]) for j in range(16)] for r in range(2)]
        raw = Wr[0].t[:].rearrange("p j n -> p (j n)").bitcast(F32)
        mk = View(raw[0:NE, 0:S], root=Wr[0])
        cum = View(raw[0:NE, S:2 * S], root=Wr[0])
        raw1 = Wr[1].t[:].rearrange("p j n -> p (j n)").bitcast(F32)
        dtok = View(raw1[:, 0:NTT * NE].rearrange("p (t e) -> p t e", e=NE), root=Wr[1])
        ident = P.sb("ident", [128, 128], BF16)
        identf = P.sb("identf", [128, 128], F32)
        cs = P.sb("cs", [NE, NE + BPC + 32], F32)
        jct = P.sb("jct", [128, 48], F32)
        tki = P.sb("tki", [128, NTT], I32)
        bas = P.sb("bas", [128, 2], F32)
        c32 = P.sb("c32", [NE, 8], F32)
        cmp_ = P.sb("cmp", [NE, BPC], F32)
        o32 = P.sb("o32", [NE, 128], F32)
        ebf = P.sb("ebf", [128, BPC], F32)
        wif = P.sb("wif", [128, BPC * 48], F32)
        wii = P.sb("wii", [128, BPC * 48], I32)
        bif = P.sb("bif", [128, BPC * 4], F32)
        bii = P.sb("bii", [128, BPC * 4], I32)
        i4 = P.sb("i4", [128, NTT * 4], I32)
        zt = P.sb("zt", [128, SPC // 128 + 1], I32)
        sti = P.sb("sti", [128, SPC // 128], I32)
        ones1 = P.sb("ones1", [1, 128], BF16)
        xb = P.sb("xb", [128, 2, D], BF16)
        xbs = [View(xb[:, sh, :]) for sh in range(2)]
        xbT = P.sb("xbT", [128, 16, 256], BF16)
        bg = P.sb("bg", [128, 2, 2048], BF16)
        bgs = [View(bg[:, c, :]) for c in range(2)]
        bd = P.sb("bd", [128, D], BF16)
        tg = P.sb("tg", [128, 2, 2048], BF16)
        rawt = tg.t[:].rearrange("p a n -> p (a n)").bitcast(F32)
        d8 = View(rawt[:, 0:NTT * 8].rearrange("p (t k) -> p t k", k=8), root=tg)
        d4 = View(rawt[:, 512:512 + NTT * 4], root=tg)
        v1 = View(rawt[:, 768:768 + NTT * 4], root=tg)
        v2 = View(rawt[:, 1024:1024 + NTT * 4], root=tg)
        act = [P.sb("act", [128, DFF], BF16) for _ in range(2)]
        actT = P.sb("actT", [128, 16, 256], BF16)
        tmp_l = [P.sb("tmp", [128, 512], F32) for _ in range(4)]
        ysb_l = [P.sb("ysb", [128, 512], BF16) for _ in range(2)]
        ptr_l = [P.ps("ptr", [128, 1024], BF16) for _ in range(2)]
        acc_l = [P.ps("acc", [128, 512], F32) for _ in range(4)]
        ptr, acc, tmpr, ysbr = RR(ptr_l), RR(acc_l), RR(tmp_l), RR(ysb_l)
        evac = RR(["vector"])
        stv = View(stok)
        scat = [Buf(None, "scat%d" % i) for i in range(NTT * 4)]

        P.dma(lambda e: e.dma_start(out=ident[:], in_=idn[:, :]), writes=[ident])
        P.dma(lambda e: e.dma_start(out=identf[:], in_=idf[:, :]), writes=[identf])
        P.dma(lambda e: e.dma_start(out=cs[:], in_=cst[:, :]), writes=[cs])
        P.dma(lambda e: e.dma_start(out=jct[:], in_=jc[:, :]), writes=[jct])
        tk1 = [P.sb("tk1", [128, 1], I32) for _ in range(NTT)]
        for tt in range(NTT):
            P.dma(lambda e, tt=tt: e.dma_start(out=tk1[tt][:], in_=tokid[:, tt:tt + 1], allow_slow_non_contiguous=True), writes=[tk1[tt]])
        P.dma(lambda e: e.dma_start(out=bas[:], in_=basei[:, :]), writes=[bas])
        P.dma(lambda e: e.dma_start(out=mk[:], in_=GTi[:, :]), writes=[mk])
        P.op("gpsimd", lambda e: e.memset(ones1[:], 1.0), writes=[ones1])
        P.op("gpsimd", lambda e: e.memset(zt[:], 0), writes=[zt])
        P.dma(lambda e: e.dma_start(out=stok.rearrange("(p c) o -> p (c o)", p=128), in_=zt[:]), reads=[zt], writes=[stv])
        P.op("vector", lambda e: e.tensor_scalar(out=mk[:], in0=mk[:], scalar1=0.0, scalar2=None, op0=ALU.is_gt), reads=[mk], writes=[mk])
        P.op("vector", lambda e: e.tensor_tensor_scan(out=cum[:], data0=mk[:], data1=mk[:], initial=0.0, op0=ALU.add, op1=ALU.max),
             reads=[mk], writes=[cum])
        P.op("vector", lambda e: e.tensor_scalar(out=o32[:, 0:NE], in0=cs[:, NE + BPC:NE + BPC + 32], scalar1=cum[:, S - 1:S], scalar2=None,
                                                 op0=ALU.is_lt), reads=[cs, cum], writes=[o32])
        P.op("vector", lambda e: e.reduce_sum(out=c32[:, 1:2], in_=o32[:, 0:NE], axis=AX.X), reads=[o32], writes=[c32])
        P.op("vector", lambda e: e.tensor_scalar(out=c32[:, 1:2], in0=c32[:, 1:2], scalar1=256.0, scalar2=None, op0=ALU.mult), reads=[c32], writes=[c32])
        P.op("gpsimd", lambda e: e.memset(o32[:], 1.0), reads=[o32], writes=[o32])
        a0 = acc()
        P.op("tensor", lambda e: e.matmul(a0[0:NE, 0:1], lhsT=cs[:, 0:NE], rhs=c32[:, 1:2], start=True, stop=True), reads=[cs, c32], writes=[a0])
        P.op("vector", lambda e: e.tensor_copy(out=c32[:, 3:4], in_=a0[0:NE, 0:1]), reads=[a0], writes=[c32])
        P.op("vector", lambda e: e.tensor_tensor(out=c32[:, 4:5], in0=c32[:, 3:4], in1=c32[:, 1:2], op=ALU.subtract), reads=[c32], writes=[c32])
        P.op("vector", lambda e: e.tensor_tensor(out=cum[:], in0=cum[:], in1=mk[:], op=ALU.subtract), reads=[cum, mk], writes=[cum])
        P.op("vector", lambda e: e.tensor_scalar(out=cum[:], in0=cum[:], scalar1=c32[:, 4:5], scalar2=1.0, op0=ALU.add, op1=ALU.add),
             reads=[cum, c32], writes=[cum])
        P.op("vector", lambda e: e.tensor_tensor(out=cum[:], in0=cum[:], in1=mk[:], op=ALU.mult), reads=[cum, mk], writes=[cum])
        P.dma(lambda e: e.dma_start(out=destMo[:, :], in_=cum[:]), reads=[cum])
        P.op("vector", lambda e: e.tensor_scalar(out=cmp_[:], in0=cs[:, NE:NE + BPC], scalar1=c32[:, 3:4], scalar2=None, op0=ALU.is_ge),
             reads=[cs, c32], writes=[cmp_])
        a1 = acc()
        P.op("tensor", lambda e: e.matmul(a1[:, 0:BPC], lhsT=o32[:], rhs=cmp_[:], start=True, stop=True), reads=[o32, cmp_], writes=[a1])
        P.op("vector", lambda e: e.tensor_scalar(out=ebf[:], in0=a1[:, 0:BPC], scalar1=float(NE - 1), scalar2=None, op0=ALU.min), reads=[a1], writes=[ebf])
        for b in range(BPC):
            P.op("vector", lambda e, b=b: e.tensor_scalar(out=bif[:, b * 4 + 3:b * 4 + 4], in0=ebf[:, b:b + 1], scalar1=4096.0, scalar2=None, op0=ALU.mult),
                 reads=[ebf], writes=[bif])
            P.op("vector", lambda e, b=b: e.tensor_scalar(out=wif[:, b * 48:b * 48 + 32], in0=jct[:, 0:32], scalar1=bif[:, b * 4 + 3:b * 4 + 4], scalar2=None, op0=ALU.add),
                 reads=[jct, bif], writes=[wif])
            P.op("vector", lambda e, b=b: e.tensor_scalar(out=bif[:, b * 4 + 3:b * 4 + 4], in0=ebf[:, b:b + 1], scalar1=2048.0, scalar2=None, op0=ALU.mult),
                 reads=[ebf], writes=[bif])
            P.op("vector", lambda e, b=b: e.tensor_scalar(out=wif[:, b * 48 + 32:b * 48 + 48], in0=jct[:, 32:48], scalar1=bif[:, b * 4 + 3:b * 4 + 4], scalar2=None, op0=ALU.add),
                 reads=[jct, bif], writes=[wif])
            P.op("vector", lambda e, b=b: e.tensor_scalar(out=bif[:, b * 4:b * 4 + 1], in0=ebf[:, b:b + 1], scalar1=2.0, scalar2=None, op0=ALU.mult),
                 reads=[ebf], writes=[bif])
            P.op("vector", lambda e, b=b: e.tensor_scalar(out=bif[:, b * 4 + 1:b * 4 + 2], in0=ebf[:, b:b + 1], scalar1=2.0, scalar2=1.0, op0=ALU.mult, op1=ALU.add),
                 reads=[ebf], writes=[bif])
            P.op("vector", lambda e, b=b: e.tensor_copy(out=bif[:, b * 4 + 2:b * 4 + 3], in_=ebf[:, b:b + 1]), reads=[ebf], writes=[bif])
        P.op("vector", lambda e: e.tensor_copy(out=wii[:], in_=wif[:]), reads=[wif], writes=[wii])
        P.op("vector", lambda e: e.tensor_copy(out=bii[:], in_=bif[:]), reads=[bif], writes=[bii])
        for g in range(NTT // 16):
            a = acc()
            for j in range(16):
                tt = g * 16 + j
                P.op("tensor", lambda e, a=a, j=j, tt=tt: e.transpose(out=a[:, j * NE:(j + 1) * NE], in_=cum[:, tt * 128:(tt + 1) * 128],
                                                                    identity=identf[0:NE, 0:NE]), reads=[cum, identf], writes=[a])
            P.op("vector", lambda e, a=a, g=g: e.tensor_copy(out=dtok[:, g * 16:(g + 1) * 16, :], in_=a[:].rearrange("p (t e) -> p t e", e=NE)),
                 reads=[a], writes=[dtok])
        for tt in range(NTT):
            P.op("vector", lambda e, tt=tt: e.max(out=d8[:, tt, :], in_=dtok[:, tt, :]), reads=[dtok], writes=[d8])
        P.op("vector", lambda e: e.tensor_scalar(out=d4[:].rearrange("p (t k) -> p t k", k=4), in0=d8[:, :, 0:4], scalar1=bas[:, 0:1], scalar2=-1.0, op0=ALU.subtract, op1=ALU.add),
             reads=[d8, bas], writes=[d4])
        P.op("vector", lambda e: e.tensor_scalar(out=v1[:], in0=d4[:], scalar1=0.0, scalar2=None, op0=ALU.is_ge), reads=[d4], writes=[v1])
        P.op("vector", lambda e: e.tensor_scalar(out=v2[:], in0=d4[:], scalar1=float(SPC), scalar2=None, op0=ALU.is_lt), reads=[d4], writes=[v2])
        P.op("vector", lambda e: e.tensor_tensor(out=v1[:], in0=v1[:], in1=v2[:], op=ALU.mult), reads=[v1, v2], writes=[v1])
        P.op("vector", lambda e: e.scalar_tensor_tensor(out=d4[:], in0=d4[:], scalar=bas[:, 1:2], in1=v1[:], op0=ALU.subtract, op1=ALU.mult),
             reads=[d4, v1, bas], writes=[d4])
        P.op("vector", lambda e: e.tensor_scalar(out=d4[:], in0=d4[:], scalar1=bas[:, 1:2], scalar2=None, op0=ALU.add), reads=[d4, bas], writes=[d4])
        P.op("vector", lambda e: e.tensor_copy(out=i4[:], in_=d4[:]), reads=[d4], writes=[i4])
        for tt in range(NTT):
            for k in range(4):
                P.dma(lambda e, tt=tt, k=k: e.indirect_dma_start(
                    out=stok[:, :], out_offset=bass.IndirectOffsetOnAxis(ap=i4[:, tt * 4 + k:tt * 4 + k + 1], axis=0), in_=tk1[tt][:, :],
                    in_offset=None), reads=[i4, tk1[tt]] + ([] if SERIAL_SCATTER else [stv]),
                    writes=[scat[tt * 4 + k]] + ([stv] if SERIAL_SCATTER else []), eng="gpsimd")
        P.dma(lambda e: e.dma_start(out=sti[:], in_=stok[0:SPC, :].rearrange("(c p) o -> p (c o)", p=128), allow_slow_non_contiguous=True),
              reads=scat, writes=[sti, stv])

        fz = P.sb("fz", [1, 1], F32)
        P.op("gpsimd", lambda e: e.memset(fz[:], 0.0), writes=[fz, Wr[0], Wr[1], tg] + Wj[0] + Wj[1])
        ring = 0
        for b in range(BPC):
            for sh in range(2):
                P.dma(lambda e, b=b, sh=sh: e.indirect_dma_start(
                    out=xb[:, sh, :], out_offset=None, in_=h2[:, :],
                    in_offset=bass.IndirectOffsetOnAxis(ap=sti[:, b * 2 + sh:b * 2 + sh + 1], axis=0)), reads=[sti], writes=[xbs[sh]], eng="gpsimd")
            for c in range(2):
                P.dma(lambda e, b=b, c=c: e.indirect_dma_start(
                    out=bg[:, c, :], out_offset=None, in_=bgu[:, :],
                    in_offset=bass.IndirectOffsetOnAxis(ap=bii[:, b * 4 + c:b * 4 + c + 1], axis=0)), reads=[bii], writes=[bgs[c]], eng="gpsimd")
            P.dma(lambda e, b=b: e.indirect_dma_start(
                out=bd[:], out_offset=None, in_=bdn[:, :],
                in_offset=bass.IndirectOffsetOnAxis(ap=bii[:, b * 4 + 2:b * 4 + 3], axis=0)), reads=[bii], writes=[bd], eng="gpsimd")
            for sh in range(2):
                transpose_tile(P, xbs[sh], xbT, ident, ptr, evac,
                               dst_sl=lambda g, n, sh=sh: (lambda d, g=g, n=n, sh=sh: d[:, g:g + n, sh * 128:(sh + 1) * 128]))
            for c in range(2):
                r = ring % 2
                ring += 1
                for j in range(16):
                    P.dma(lambda e, b=b, c=c, j=j, r=r: e.indirect_dma_start(
                        out=Wr[r][:, j, :], out_offset=None, in_=wgu[:, :],
                        in_offset=bass.IndirectOffsetOnAxis(ap=wii[:, b * 48 + j * 2 + c:b * 48 + j * 2 + c + 1], axis=0)),
                        reads=[wii], writes=[Wj[r][j]], eng="gpsimd")
                for n in range(4):
                    cols = slice(n * 512, (n + 1) * 512)
                    for sh in range(2):
                        a = acc()
                        for j in range(16):
                            P.op("tensor", lambda e, a=a, r=r, j=j, sh=sh, cols=cols: e.matmul(
                                a[:], lhsT=xbT[:, j, sh * 128:(sh + 1) * 128], rhs=Wr[r][:, j, cols], start=(j == 0), stop=False),
                                reads=[xbT, Wj[r][j]], writes=[a])
                        P.op("tensor", lambda e, a=a, c=c, cols=cols: e.matmul(a[:], lhsT=ones1[0:1, :], rhs=bg[0:1, c, cols], start=False, stop=True),
                             reads=[ones1, bgs[c]], writes=[a])
                        t_ = tmpr()
                        if c == 0:
                            s_ = tmpr()
                            P.op("vector", lambda e, a=a, t_=t_: e.tensor_scalar(out=t_[:], in0=a[:], scalar1=7.0, scalar2=None, op0=ALU.min),
                                 reads=[a], writes=[t_])
                            P.op("scalar", lambda e, t_=t_, s_=s_: e.activation(out=s_[:], in_=t_[:], func=AF.Sigmoid, scale=1.702),
                                 reads=[t_], writes=[s_])
                            P.op("gpsimd", lambda e, t_=t_, s_=s_, sh=sh, cols=cols: e.tensor_tensor(out=tg[:, sh, cols], in0=t_[:], in1=s_[:], op=ALU.mult),
                                 reads=[t_, s_], writes=[tg])
                        else:
                            P.op("vector", lambda e, a=a, t_=t_: e.tensor_scalar(out=t_[:], in0=a[:], scalar1=7.0, scalar2=-7.0, op0=ALU.min, op1=ALU.max),
                                 reads=[a], writes=[t_])
                            P.op("vector", lambda e, t_=t_, sh=sh, cols=cols: e.scalar_tensor_tensor(
                                out=act[sh][:, cols], in0=t_[:], scalar=1.0, in1=tg[:, sh, cols], op0=ALU.add, op1=ALU.mult),
                                reads=[t_, tg], writes=[act[sh]])
            for sh in range(2):
                transpose_tile(P, act[sh], actT, ident, ptr, evac,
                               dst_sl=lambda g, n, sh=sh: (lambda d, g=g, n=n, sh=sh: d[:, g:g + n, sh * 128:(sh + 1) * 128]))
            r = ring % 2
            ring += 1
            for j in range(16):
                P.dma(lambda e, b=b, j=j, r=r: e.indirect_dma_start(
                    out=Wr[r][:, j, :], out_offset=None, in_=wdn[:, :],
                    in_offset=bass.IndirectOffsetOnAxis(ap=wii[:, b * 48 + 32 + j:b * 48 + 33 + j], axis=0)),
                    reads=[wii], writes=[Wj[r][j]], eng="gpsimd")
            for n in range(4):
                cols = slice(n * 512, (n + 1) * 512)
                for sh in range(2):
                    a = acc()
                    for j in range(16):
                        P.op("tensor", lambda e, a=a, r=r, j=j, sh=sh, cols=cols: e.matmul(
                            a[:], lhsT=actT[:, j, sh * 128:(sh + 1) * 128], rhs=Wr[r][:, j, cols], start=(j == 0), stop=False),
                            reads=[actT, Wj[r][j]], writes=[a])
                    P.op("tensor", lambda e, a=a, cols=cols: e.matmul(a[:], lhsT=ones1[0:1, :], rhs=bd[0:1, cols], start=False, stop=True),
                         reads=[ones1, bd], writes=[a])
                    y_ = ysbr()
                    P.op("scalar", lambda e, a=a, y_=y_: e.activation(out=y_[:], in_=a[:], func=AF.Copy), reads=[a], writes=[y_])
                    r0 = b * EBLK + sh * 128
                    P.dma(lambda e, y_=y_, r0=r0, cols=cols: e.dma_start(out=ys[r0:r0 + 128, cols], in_=y_[:]), reads=[y_])
        P.finish()
        P.emit()
    return nc


def run_E(ncE, G, h2, w_gu_l, b_gu_l, w_down_l, b_down_l):
    GT = np.ascontiguousarray(G.T)
    idn = np.eye(128, dtype=np.float32).astype(NPBF)
    idf = np.eye(128, dtype=np.float32)
    U = np.triu(np.ones((NE, NE), np.float32))
    p = np.arange(128, dtype=np.float32)[:, None]
    jcol = np.arange(32)
    jc = np.concatenate([(jcol // 2 * 128)[None, :] * 2.0 + (jcol % 2)[None, :] + 2.0 * p,
                         (np.arange(16) * 128)[None, :] + p], axis=1).astype(np.float32)
    tokid = (np.arange(S // 128, dtype=np.int32)[None, :] * 128 + np.arange(128, dtype=np.int32)[:, None]).astype(np.int32)
    wgu = w_gu_l.reshape(NE * D * 2, 2048)
    wdn = w_down_l.reshape(NE * DFF, D)
    bgu = b_gu_l.reshape(NE * 2, 2048)
    in_maps = []
    for i in range(NCORES):
        thr = ((BPC * i + np.arange(BPC)) * EBLK).astype(np.float32)
        cst = np.concatenate([U, np.tile(thr[None, :], (NE, 1)), np.tile((np.arange(32) * 256.0)[None, :], (NE, 1))], axis=1).astype(np.float32)
        in_maps.append({"GT": GT, "h2": h2, "wgu": wgu, "wdn": wdn, "bgu": bgu, "bdn": b_down_l, "idn": idn, "idf": idf,
                        "cst": cst, "jc": jc, "tokid": tokid, "base": np.stack([np.full(128, SPC * i, np.float32), SPC + np.arange(128, dtype=np.float32)], axis=1)})
    res = _run(ncE, in_maps)
    ys = np.concatenate([r["ys"] for r in res], axis=0)
    destM = res[0]["destM"]
    return ys, destM


def build_F(final):
    nc = _new_nc()
    ysf = _din(nc, "ys", [NSLOT, D], BF16)
    dmi = _din(nc, "dm", [TL, NE])
    gti = _din(nc, "gt", [TL, NE])
    x1 = _din(nc, "x1", [TL, D])
    vecs = _din(nc, "vecs", [2, D])
    out = _dout(nc, "out", [TL, D])
    NT = TL // 128
    with ExitStack() as es:
        P = Prog(nc, es)
        gfb = P.sb("gfb", [128, D], F32)
        nfb = P.sb("nfb", [128, D], F32)
        zb = P.sb("zb", [128, D], F32)
        dm = [P.sb("dm", [128, NE], F32) for _ in range(2)]
        gt = [P.sb("gt", [128, NE], F32) for _ in range(2)]
        d8 = [P.sb("d8", [128, 8], F32) for _ in range(2)]
        d4f = [P.sb("d4f", [128, 4], F32) for _ in range(2)]
        d4i = [P.sb("d4i", [128, 4], I32) for _ in range(2)]
        oh = [P.sb("oh", [128, NE], F32) for _ in range(2)]
        g4 = [P.sb("g4", [128, 4], F32) for _ in range(2)]
        Y = [[P.sb("Y", [128, D], BF16) for _ in range(4)] for _ in range(2)]
        xt = [P.sb("xt", [128, D], F32) for _ in range(2)]
        ac = [P.sb("ac", [128, D], F32) for _ in range(2)]
        scr = P.sb("scr", [128, D], F32)
        ot = [P.sb("ot", [128, D], F32) for _ in range(2)]
        ss = [P.sb("ss", [128, 1], F32) for _ in range(2)]
        rstd = [P.sb("rstd", [128, 1], F32) for _ in range(2)]
        P.dma(lambda e: e.dma_start(out=gfb[:], in_=vecs[0:1, :].to_broadcast([128, D])), writes=[gfb])
        if final:
            P.dma(lambda e: e.dma_start(out=nfb[:], in_=vecs[1:2, :].to_broadcast([128, D])), writes=[nfb])
            P.op("gpsimd", lambda e: e.memset(zb[:], 0.0), writes=[zb])
        for tt in range(NT):
            i = tt % 2
            rows = slice(tt * 128, (tt + 1) * 128)
            P.dma(lambda e, i=i, rows=rows: e.dma_start(out=dm[i][:], in_=dmi[rows, :]), writes=[dm[i]])
            P.dma(lambda e, i=i, rows=rows: e.dma_start(out=gt[i][:], in_=gti[rows, :]), writes=[gt[i]])
            P.dma(lambda e, i=i, rows=rows: e.dma_start(out=xt[i][:], in_=x1[rows, :]), writes=[xt[i]])
            P.op("vector", lambda e, i=i: e.max(out=d8[i][:], in_=dm[i][:]), reads=[dm[i]], writes=[d8[i]])
            P.op("vector", lambda e, i=i: e.tensor_scalar(out=d4f[i][:], in0=d8[i][:, 0:4], scalar1=-1.0, scalar2=None, op0=ALU.add),
                 reads=[d8[i]], writes=[d4f[i]])
            P.op("vector", lambda e, i=i: e.tensor_copy(out=d4i[i][:], in_=d4f[i][:]), reads=[d4f[i]], writes=[d4i[i]])
            for k in range(4):
                P.dma(lambda e, i=i, k=k: e.indirect_dma_start(out=Y[i][k][:], out_offset=None, in_=ysf[:, :],
                                                               in_offset=bass.IndirectOffsetOnAxis(ap=d4i[i][:, k:k + 1], axis=0)),
                      reads=[d4i[i]], writes=[Y[i][k]], eng="gpsimd")
                P.op("vector", lambda e, i=i, k=k: e.tensor_scalar(out=oh[i][:], in0=dm[i][:], scalar1=d8[i][:, k:k + 1], scalar2=None,
                                                                   op0=ALU.is_equal), reads=[dm[i], d8[i]], writes=[oh[i]])
                P.op("vector", lambda e, i=i: e.tensor_tensor(out=oh[i][:], in0=oh[i][:], in1=gt[i][:], op=ALU.mult), reads=[oh[i], gt[i]], writes=[oh[i]])
                P.op("vector", lambda e, i=i, k=k: e.reduce_sum(out=g4[i][:, k:k + 1], in_=oh[i][:], axis=AX.X), reads=[oh[i]], writes=[g4[i]])
            P.op("vector", lambda e, i=i: e.tensor_scalar(out=ac[i][:], in0=Y[i][0][:], scalar1=g4[i][:, 0:1], scalar2=None, op0=ALU.mult),
                 reads=[Y[i][0], g4[i]], writes=[ac[i]])
            for k in range(1, 4):
                P.op("vector", lambda e, i=i, k=k: e.scalar_tensor_tensor(out=ac[i][:], in0=Y[i][k][:], scalar=g4[i][:, k:k + 1], in1=ac[i][:],
                                                                          op0=ALU.mult, op1=ALU.add), reads=[Y[i][k], g4[i], ac[i]], writes=[ac[i]])
            P.op("gpsimd", lambda e, i=i: e.tensor_tensor(out=ac[i][:], in0=ac[i][:], in1=gfb[:], op=ALU.mult), reads=[ac[i], gfb], writes=[ac[i]])
            P.op("gpsimd", lambda e, i=i: e.tensor_tensor(out=xt[i][:], in0=xt[i][:], in1=ac[i][:], op=ALU.add), reads=[ac[i], xt[i]], writes=[xt[i]])
            if final:
                rmsnorm_mod_tile(P, xt[i], nfb, zb, ot[i], scr, ss[i], rstd[i])
                P.dma(lambda e, i=i, rows=rows: e.dma_start(out=out[rows, :], in_=ot[i][:]), reads=[ot[i]])
            else:
                P.dma(lambda e, i=i, rows=rows: e.dma_start(out=out[rows, :], in_=xt[i][:]), reads=[xt[i]])
        P.finish()
        P.emit()
    return nc


def run_F(ncF, ys, destM, G, x1, g_f, norm_final):
    dmT = np.ascontiguousarray(destM.T)
    vecs = np.stack([g_f, norm_final]).astype(np.float32)
    in_maps = []
    for i in range(NCORES):
        rows = slice(i * TL, (i + 1) * TL)
        c = np.ascontiguousarray
        in_maps.append({"ys": ys, "dm": c(dmT[rows]), "gt": c(G[rows]), "x1": c(x1[rows]), "vecs": vecs})
    res = _run(ncF, in_maps)
    return np.concatenate([r["out"] for r in res], axis=0)


def kernel(x, c, norm_mix, norm_ffn, w_ada, b_ada, w_in, b_mgates, m_norm, lam_q1, lam_k1, lam_q2, lam_k2, a_norm,
           w_br_m, w_br_a, w_out, w_router, b_router, w_gu, b_gu, w_down, b_down, norm_final):
    f = lambda a: np.ascontiguousarray(np.asarray(a, dtype=np.float32))
    xs = f(x)[0]
    mod = run_A(f(c), f(w_ada), f(b_ada))
    ncB, ncC1, ncC2, ncD, ncE = build_B(), build_C1(), build_C2(), build_D(), build_E()
    for l in range(DEPTH):
        lam_init = 0.8 - 0.6 * float(np.exp(-0.3 * l))
        sh_m, sc_m, g_m, sh_f, sc_f, g_f = [f(v) for v in np.split(mod[l], 6)]
        pb, pg = run_B(ncB, xs, f(norm_mix[l]), sc_m, sh_m, f(w_in[l]), f(b_mgates[l]))
        lam4 = np.stack([f(lam_q1[l]), f(lam_k1[l]), f(lam_q2[l]), f(lam_k2[l])])
        ya = run_C1(ncC1, pb, lam4, f(a_norm[l]), lam_init)
        hfw, hbw = run_C2(ncC2, pb, pg)
        x1, h2, G = run_D(ncD, xs, hfw, hbw, ya, pb, f(m_norm[l]), g_m, f(norm_ffn[l]), sc_f, sh_f,
                          f(w_br_m[l]), f(w_br_a[l]), f(w_out[l]), f(w_router[l]), f(b_router[l]))
        ys, destM = run_E(ncE, G, h2, f(w_gu[l]), f(b_gu[l]), f(w_down[l]), f(b_down[l]))
        xs = run_F(build_F(l == DEPTH - 1), ys, destM, G, x1, g_f, f(norm_final))
    return xs.reshape(1, S, D).astype(np.float32)
```

```python
import numpy as np
import ml_dtypes
import concourse.bass as bass
import concourse.mybir as mybir
from concourse.bass_utils import run_bass_kernel_spmd
from contextlib import ExitStack

F32 = mybir.dt.float32
BF16 = mybir.dt.bfloat16
I32 = mybir.dt.int32
U32 = mybir.dt.uint32
AF = mybir.ActivationFunctionType
ALU = mybir.AluOpType
AX = mybir.AxisListType
NPBF = ml_dtypes.bfloat16

NCORES = 8
D = 2048
S = 8192
TL = S // NCORES
DEPTH = 2
M_HEADS, M_QK, M_V = 4, 256, 512
A_HEADS, A_QK, A_V = 8, 128, 256
NE, TOPK, DFF, EBLK = 32, 4, 2048, 512
NSLOT = S * TOPK + NE * EBLK
NBLK = NSLOT // EBLK
BPC = NBLK // NCORES
EPS = 1e-6
IN_SIZES = (1024, 1024, 2048, 2048, 16, 2048, 2048, 2048, 2048, 2048)
IN_COLS = sum(IN_SIZES)
O_MQ, O_MK, O_MV, O_MO, O_MG, O_AQ, O_AK, O_AV, O_GM, O_GA = np.cumsum((0,) + IN_SIZES[:-1]).tolist()

ENGS = ["tensor", "vector", "scalar", "gpsimd", "sync"]


class Buf:
    __slots__ = ("t", "lw", "rd", "name", "root")

    def __init__(self, t, name="", root=None):
        self.t = t
        self.lw = None
        self.rd = []
        self.name = name
        self.root = root if root is not None else self

    def __getitem__(self, k):
        return self.t[k]


class Prog:
    def __init__(self, nc, es, ndma=12):
        self.nc = nc
        self.es = es
        self.q = {e: [] for e in ENGS}
        self.cnt = {}
        self.sem = {}
        self.seen = {e: {} for e in ENGS}
        for e in ENGS:
            self.sem[e] = es.enter_context(nc.semaphore("s_" + e))
            self.cnt[e] = 0
        self.dch = {}
        self.dnext = {}
        for q in ("sync", "gpsimd", "scalar"):
            self.dch[q] = []
            for i in range(ndma):
                nm = "d%s%d" % (q[0:2], i)
                self.sem[nm] = es.enter_context(nc.semaphore("s_" + nm))
                self.cnt[nm] = 0
                self.dch[q].append(nm)
            self.dnext[q] = 0
        self.nbuf = 0

    def sb(self, name, shape, dt):
        self.nbuf += 1
        return Buf(self.es.enter_context(self.nc.sbuf_tensor("%s_%d" % (name, self.nbuf), shape, dt)), name)

    def ps(self, name, shape, dt):
        self.nbuf += 1
        return Buf(self.es.enter_context(self.nc.psum_tensor("%s_%d" % (name, self.nbuf), shape, dt)), name)

    def _wait(self, eng, s, i):
        if self.seen[eng].get(s, 0) < i:
            self.seen[eng][s] = i
            sem = self.sem[s]
            self.q[eng].append(lambda e, sem=sem, i=i: e.wait_ge(sem, i))

    def _deps(self, eng, reads, writes, skip_same=False):
        deps = {}
        reads = [b.root for b in reads]
        writes = [b.root for b in writes]
        for b in reads:
            if b.lw is not None:
                s, i = b.lw
                deps[s] = max(deps.get(s, 0), i)
        for b in writes:
            if b.lw is not None:
                s, i = b.lw
                deps[s] = max(deps.get(s, 0), i)
            for s, i in b.rd:
                deps[s] = max(deps.get(s, 0), i)
        for s, i in deps.items():
            if skip_same and s == eng:
                continue
            self._wait(eng, s, i)

    def _mark(self, key, idx, reads, writes):
        reads = [b.root for b in reads]
        writes = [b.root for b in writes]
        for b in reads:
            b.rd = [(s, i) for (s, i) in b.rd if s != key] + [(key, idx)]
        for b in writes:
            b.lw = (key, idx)
            b.rd = []

    def op(self, eng, fn, reads=(), writes=()):
        self._deps(eng, reads, writes, skip_same=(eng == "tensor"))
        self.cnt[eng] += 1
        idx = self.cnt[eng]
        sem = self.sem[eng]
        self.q[eng].append(lambda e, fn=fn, sem=sem: fn(e).then_inc(sem, 1))
        self._mark(eng, idx, reads, writes)

    def dma(self, fn, reads=(), writes=(), eng="sync"):
        chs = self.dch[eng]
        ch = chs[self.dnext[eng]]
        self.dnext[eng] = (self.dnext[eng] + 1) % len(chs)
        if self.cnt[ch] > 0:
            self._wait(eng, ch, self.cnt[ch])
        self._deps(eng, reads, writes)
        self.cnt[ch] += 16
        idx = self.cnt[ch]
        sem = self.sem[ch]
        self.q[eng].append(lambda e, fn=fn, sem=sem: fn(e).then_inc(sem, 16))
        self._mark(ch, idx, reads, writes)

    def finish(self, eng="sync"):
        for s in list(self.sem.keys()):
            if self.cnt[s] > 0 and s != eng:
                self._wait(eng, s, self.cnt[s])

    def emit(self):
        with self.nc.Block() as block:
            for e in ENGS:
                if not self.q[e]:
                    continue
                lst = self.q[e]

                def body(engine, lst=lst):
                    for f in lst:
                        f(engine)
                getattr(block, e)(body)


def _new_nc():
    return bass.Bass("TRN2", target_bir_lowering=False)


def _din(nc, name, shape, dt=F32):
    return nc.dram_tensor(name, list(shape), dt, kind="ExternalInput").ap()


def _dout(nc, name, shape, dt=F32):
    return nc.dram_tensor(name, list(shape), dt, kind="ExternalOutput").ap()


def _run(nc, in_maps):
    res = run_bass_kernel_spmd(nc, in_maps, core_ids=list(range(NCORES)))
    return res.results


class RR:
    def __init__(self, items):
        self.items = list(items)
        self.i = 0

    def __call__(self):
        x = self.items[self.i]
        self.i = (self.i + 1) % len(self.items)
        return x


A_NC = 6 * D // NCORES


def build_A():
    nc = _new_nc()
    cT = _din(nc, "cT", [128, 16])
    wa = _din(nc, "wa", [DEPTH, D, A_NC])
    ba = _din(nc, "ba", [1, DEPTH * A_NC])
    mod = _dout(nc, "mod", [1, DEPTH * A_NC])
    with ExitStack() as es:
        P = Prog(nc, es)
        ct = P.sb("ct", [128, 16], F32)
        ca = P.sb("ca", [128, 16], F32)
        bat = P.sb("bat", [1, DEPTH * A_NC], F32)
        ot = P.sb("ot", [1, DEPTH * A_NC], F32)
        wt = [P.sb("wt", [128, 16, 512], F32) for _ in range(2)]
        pss = [P.ps("ps", [1, 512], F32) for _ in range(2)]
        P.dma(lambda e: e.dma_start(out=ct[:], in_=cT[:, :]), writes=[ct])
        P.dma(lambda e: e.dma_start(out=bat[:], in_=ba[:, :]), writes=[bat])
        P.op("scalar", lambda e: e.activation(out=ca[:], in_=ct[:], func=AF.Silu), reads=[ct], writes=[ca])
        it = 0
        for l in range(DEPTH):
            for n in range(A_NC // 512):
                w = wt[it % 2]
                ps = pss[it % 2]
                src = wa[l, :, n * 512:(n + 1) * 512].rearrange("(k p) n -> p k n", p=128)
                P.dma(lambda e, w=w, src=src: e.dma_start(out=w[:], in_=src), writes=[w])
                for k in range(16):
                    P.op("tensor", lambda e, w=w, ps=ps, k=k: e.matmul(ps[:], lhsT=ca[:, k:k + 1], rhs=w[:, k, :],
                                                                     start=(k == 0), stop=(k == 15)),
                         reads=[ca, w], writes=[ps])
                o0 = l * A_NC + n * 512
                P.op("vector", lambda e, ps=ps, o0=o0: e.tensor_tensor(out=ot[:, o0:o0 + 512], in0=ps[:],
                                                                      in1=bat[:, o0:o0 + 512], op=ALU.add),
                     reads=[ps, bat], writes=[ot])
                it += 1
        P.dma(lambda e: e.dma_start(out=mod[:, :], in_=ot[:]), reads=[ot])
        P.finish()
        P.emit()
    return nc


def run_A(c, w_ada, b_ada):
    nc = build_A()
    cT = np.ascontiguousarray(c.reshape(16, 128).T)
    in_maps = []
    for i in range(NCORES):
        sl = slice(i * A_NC, (i + 1) * A_NC)
        in_maps.append({"cT": cT,
                        "wa": np.ascontiguousarray(w_ada[:, :, sl]),
                        "ba": np.ascontiguousarray(b_ada[:, sl]).reshape(1, DEPTH * A_NC)})
    res = _run(nc, in_maps)
    mod = np.concatenate([r["mod"].reshape(DEPTH, A_NC) for r in res], axis=1)
    return mod


def rmsnorm_mod_tile(P, xt, wmod, shb, hb, scr, ss, rstd):
    P.op("scalar", lambda e: e.activation(out=scr[:], in_=xt[:], func=AF.Square, accum_out=ss[:]),
         reads=[xt], writes=[scr, ss])
    P.op("vector", lambda e: e.tensor_scalar(out=rstd[:], in0=ss[:], scalar1=1.0 / D, scalar2=EPS,
                                             op0=ALU.mult, op1=ALU.add), reads=[ss], writes=[rstd])
    P.op("scalar", lambda e: e.activation(out=rstd[:], in_=rstd[:], func=AF.Sqrt), reads=[rstd], writes=[rstd])
    P.op("vector", lambda e: e.reciprocal(out=rstd[:], in_=rstd[:]), reads=[rstd], writes=[rstd])
    P.op("vector", lambda e: e.scalar_tensor_tensor(out=scr[:], in0=xt[:], scalar=rstd[:, 0:1], in1=wmod[:],
                                                    op0=ALU.mult, op1=ALU.mult),
         reads=[xt, rstd, wmod], writes=[scr])
    P.op("gpsimd", lambda e: e.tensor_tensor(out=hb[:], in0=scr[:], in1=shb[:], op=ALU.add),
         reads=[scr, shb], writes=[hb])


def transpose_tile(P, src, dst, ident, ptr, evac, nchunk=16, src_sl=None, dst_sl=None):
    for g in range(0, nchunk, 8):
        pt = ptr()
        n = min(8, nchunk - g)
        for j in range(n):
            k = g + j
            sl = src_sl(k) if src_sl else slice(k * 128, (k + 1) * 128)
            P.op("tensor", lambda e, pt=pt, j=j, sl=sl: e.transpose(out=pt[:, j * 128:(j + 1) * 128], in_=src[:, sl],
                                                                   identity=ident[:]),
                 reads=[src, ident], writes=[pt])
        eng = "vector"
        dsl = dst_sl(g, n) if dst_sl else (lambda d, g=g, n=n: d[:, g:g + n, :])
        if eng == "scalar":
            P.op("scalar", lambda e, pt=pt, n=n, dsl=dsl: e.activation(out=dsl(dst), in_=pt[:, 0:n * 128].rearrange(
                "p (k t) -> p k t", t=128), func=AF.Copy), reads=[pt], writes=[dst])
        else:
            P.op(eng, lambda e, pt=pt, n=n, dsl=dsl: e.tensor_copy(out=dsl(dst), in_=pt[:, 0:n * 128].rearrange(
                "p (k t) -> p k t", t=128)), reads=[pt], writes=[dst])


def build_B():
    nc = _new_nc()
    x = _din(nc, "x", [TL, D])
    nw = _din(nc, "nw", [1, D])
    sc = _din(nc, "sc", [1, D])
    sh = _din(nc, "sh", [1, D])
    win = _din(nc, "win", [D, IN_COLS])
    bg = _din(nc, "bg", [1, 16])
    cs4 = _din(nc, "cs4", [TL, 128])
    idn = _din(nc, "idn", [128, 128], BF16)
    pb = _dout(nc, "pb", [TL, IN_COLS - 16], BF16)
    pg = _dout(nc, "pg", [TL, 16])
    NT = TL // 128
    with ExitStack() as es:
        P = Prog(nc, es)
        ident = P.sb("ident", [128, 128], BF16)
        nwb = P.sb("nwb", [128, D], F32)
        wmod = P.sb("wmod", [128, D], F32)
        shb = P.sb("shb", [128, D], F32)
        bgb = P.sb("bgb", [128, 16], F32)
        cst = P.sb("cst", [128, NT, 128], F32)
        xts = [P.sb("xt", [128, D], F32) for _ in range(2)]
        scr = P.sb("scr", [128, D], F32)
        hbs = [P.sb("hb", [128, D], BF16) for _ in range(2)]
        ss = [P.sb("ss", [128, 1], F32) for _ in range(2)]
        rstd = [P.sb("rstd", [128, 1], F32) for _ in range(2)]
        hT = [P.sb("hT", [128, 16, 128], BF16) for _ in range(NT)]
        wts = [P.sb("wt", [128, 16, 512], BF16) for _ in range(3)]
        wg = P.sb("wg", [128, 16, 16], BF16)
        ptr_l = [P.ps("ptr", [128, 1024], BF16) for _ in range(2)]
        acc_l = [P.ps("acc", [128, 512], F32) for _ in range(4)]
        stg = [P.sb("stg", [128, 512], BF16) for _ in range(3)]
        r32 = [P.sb("r32", [128, 512], F32) for _ in range(2)]
        rt = [P.sb("rt", [128, 4, 4, 16], F32) for _ in range(2)]
        sg = P.sb("sg", [128, 16], F32)
        ptr = RR(ptr_l)
        acc = RR(acc_l)
        stgr = RR(stg)
        r32r = RR(r32)
        rtr = RR(rt)
        evac = RR(["scalar", "vector"])

        P.dma(lambda e: e.dma_start(out=ident[:], in_=idn[:, :]), writes=[ident])
        P.dma(lambda e: e.dma_start(out=nwb[:], in_=nw.to_broadcast([128, D])), writes=[nwb])
        P.dma(lambda e: e.dma_start(out=wmod[:], in_=sc.to_broadcast([128, D])), writes=[wmod])
        P.dma(lambda e: e.dma_start(out=shb[:], in_=sh.to_broadcast([128, D])), writes=[shb])
        P.dma(lambda e: e.dma_start(out=bgb[:], in_=bg.to_broadcast([128, 16])), writes=[bgb])
        P.dma(lambda e: e.dma_start(out=cst[:], in_=cs4.rearrange("(t p) c -> p t c", p=128)), writes=[cst])
        P.op("vector", lambda e: e.scalar_tensor_tensor(out=wmod[:], in0=wmod[:], scalar=1.0, in1=nwb[:],
                                                        op0=ALU.add, op1=ALU.mult), reads=[wmod, nwb], writes=[wmod])
        for tt in range(NT):
            xt = xts[tt % 2]
            hb = hbs[tt % 2]
            P.dma(lambda e, xt=xt, tt=tt: e.dma_start(out=xt[:], in_=x[tt * 128:(tt + 1) * 128, :]), writes=[xt])
            rmsnorm_mod_tile(P, xt, wmod, shb, hb, scr, ss[tt % 2], rstd[tt % 2])
            transpose_tile(P, hb, hT[tt], ident, ptr, evac)

        chunks = []
        for (o, n) in zip((O_MQ, O_MK, O_MV, O_MO, O_MG, O_AQ, O_AK, O_AV, O_GM, O_GA), IN_SIZES):
            if n == 16:
                chunks.append((o, 16, "mg"))
                continue
            kind = {O_MQ: "mq", O_AQ: "aq", O_AK: "ak"}.get(o, "plain")
            for c0 in range(o, o + n, 512):
                chunks.append((c0, 512, kind))
        wi = 0
        for (c0, ncol, kind) in chunks:
            if kind == "mg":
                w = wg
            else:
                w = wts[wi % 3]
                wi += 1
            src = win[:, c0:c0 + ncol].rearrange("(k p) n -> p k n", p=128)
            P.dma(lambda e, w=w, src=src: e.dma_start(out=w[:], in_=src), writes=[w], eng="gpsimd")
            oc = c0 if c0 < O_MG else c0 - 16
            for tt in range(NT):
                a = acc()
                for k in range(16):
                    P.op("tensor", lambda e, a=a, w=w, tt=tt, k=k, ncol=ncol: e.matmul(
                        a[:, 0:ncol], lhsT=hT[tt][:, k, :], rhs=w[:, k, :], start=(k == 0), stop=(k == 15)),
                        reads=[hT[tt], w], writes=[a])
                rows = slice(tt * 128, (tt + 1) * 128)
                if kind == "mg":
                    P.op("vector", lambda e, a=a: e.tensor_tensor(out=sg[:], in0=a[:, 0:16], in1=bgb[:], op=ALU.add),
                         reads=[a, bgb], writes=[sg])
                    P.dma(lambda e, rows=rows: e.dma_start(out=pg[rows, :], in_=sg[:]), reads=[sg])
                    continue
                st = stgr()
                if kind == "plain" or kind == "mq":
                    scl = 1.0 if kind == "plain" else float(M_QK) ** -0.5
                    eng = evac()
                    if eng == "scalar":
                        P.op("scalar", lambda e, a=a, st=st, scl=scl: e.activation(out=st[:], in_=a[:], func=AF.Copy,
                                                                                 scale=scl), reads=[a], writes=[st])
                    else:
                        P.op("vector", lambda e, a=a, st=st, scl=scl: e.tensor_scalar(
                            out=st[:], in0=a[:], scalar1=scl, scalar2=None, op0=ALU.mult), reads=[a], writes=[st])
                else:
                    scl = float(A_QK) ** -0.5 if kind == "aq" else 1.0
                    r = r32r()
                    t4 = rtr()
                    P.op("scalar", lambda e, a=a, r=r, scl=scl: e.activation(out=r[:], in_=a[:], func=AF.Copy, scale=scl),
                         reads=[a], writes=[r])
                    rv = lambda r: r[:].rearrange("p (g d) -> p g d", d=128)
                    cosv = lambda tt: cst[:, tt, 0:64].rearrange("p (g j) -> p g j", j=16)
                    sinv = lambda tt: cst[:, tt, 64:128].rearrange("p (g j) -> p g j", j=16)
                    P.op("vector", lambda e, r=r, t4=t4, tt=tt: e.tensor_tensor(out=t4[:, 0], in0=rv(r)[:, :, 0:16], in1=cosv(tt), op=ALU.mult),
                         reads=[r, cst], writes=[t4])
                    P.op("vector", lambda e, r=r, t4=t4, tt=tt: e.tensor_tensor(out=t4[:, 1], in0=rv(r)[:, :, 16:32], in1=sinv(tt), op=ALU.mult),
                         reads=[r, cst], writes=[t4])
                    P.op("gpsimd", lambda e, r=r, t4=t4, tt=tt: e.tensor_tensor(out=t4[:, 2], in0=rv(r)[:, :, 0:16], in1=sinv(tt), op=ALU.mult),
                         reads=[r, cst], writes=[t4])
                    P.op("gpsimd", lambda e, r=r, t4=t4, tt=tt: e.tensor_tensor(out=t4[:, 3], in0=rv(r)[:, :, 16:32], in1=cosv(tt), op=ALU.mult),
                         reads=[r, cst], writes=[t4])
                    P.op("vector", lambda e, r=r, t4=t4: e.tensor_tensor(out=rv(r)[:, :, 0:16], in0=t4[:, 0], in1=t4[:, 1], op=ALU.subtract),
                         reads=[t4, r], writes=[r])
                    P.op("vector", lambda e, r=r, t4=t4: e.tensor_tensor(out=rv(r)[:, :, 16:32], in0=t4[:, 2], in1=t4[:, 3], op=ALU.add),
                         reads=[t4, r], writes=[r])
                    P.op("gpsimd", lambda e, r=r, st=st: e.tensor_copy(out=st[:], in_=r[:]), reads=[r], writes=[st])
                P.dma(lambda e, rows=rows, oc=oc, st=st: e.dma_start(out=pb[rows, oc:oc + 512], in_=st[:]), reads=[st])
        P.finish()
        P.emit()
    return nc


def rope_tables():
    half = A_QK // 4 // 2
    inv = (500000.0 ** (-np.arange(0, 2 * half, 2, dtype=np.float32) / np.float32(2 * half))).astype(np.float32)
    ang = np.arange(S, dtype=np.float32)[:, None] * inv[None, :]
    cos = np.cos(ang).astype(np.float32)
    sin = np.sin(ang).astype(np.float32)
    return np.concatenate([np.tile(cos, (1, 4)), np.tile(sin, (1, 4))], axis=1)


def run_B(ncB, x2d, nw, sc, sh, w_in_l, bg):
    cs4 = rope_tables()
    idn = np.eye(128, dtype=np.float32).astype(NPBF)
    in_maps = []
    for i in range(NCORES):
        rows = slice(i * TL, (i + 1) * TL)
        in_maps.append({"x": np.ascontiguousarray(x2d[rows]), "nw": nw.reshape(1, D), "sc": sc.reshape(1, D),
                        "sh": sh.reshape(1, D), "win": w_in_l, "bg": bg.reshape(1, 16),
                        "cs4": np.ascontiguousarray(cs4[rows]), "idn": idn})
    res = _run(ncB, in_maps)
    pb = np.concatenate([r["pb"] for r in res], axis=0)
    pg = np.concatenate([r["pg"] for r in res], axis=0)
    return pb, pg


def build_C1():
    nc = _new_nc()
    qT = _din(nc, "qT", [2, 128, S], BF16)
    kT = _din(nc, "kT", [2, 128, S], BF16)
    v = _din(nc, "v", [S, A_V], BF16)
    lam4 = _din(nc, "lam4", [4, A_QK])
    an = _din(nc, "an", [1, A_V])
    li = _din(nc, "li", [1, 1])
    ya = _dout(nc, "ya", [S, A_V], BF16)
    NKB = S // 128
    QT = 256
    with ExitStack() as es:
        P = Prog(nc, es)
        qs = P.sb("qs", [128, 2, S], BF16)
        ks = P.sb("ks", [128, 2, S], BF16)
        vs = P.sb("vs", [128, NKB, A_V + 1], BF16)
        lb = P.sb("lb", [128, 4, A_QK], F32)
        lt = P.sb("lt", [128, 2, A_QK], F32)
        ls = P.sb("ls", [128, 2], F32)
        lib = P.sb("lib", [128, 1], F32)
        lam = P.sb("lam", [128, 1], F32)
        nlam = P.sb("nlam", [128, 1], F32)
        anb = P.sb("anb", [128, A_V], F32)
        sps_l = [P.ps("sps", [128, 512], F32) for _ in range(4)]
        acc = [[P.ps("acc", [128, 512], F32) for _ in range(2)] for _ in range(2)]
        pts_l = [P.sb("pt", [128, 512], BF16) for _ in range(4)]
        sps = RR(sps_l)
        pts = RR(pts_l)
        rz = [P.sb("rz", [128, 2], F32) for _ in range(2)]
        o0 = [P.sb("o0", [128, A_V], F32) for _ in range(2)]
        oo = [P.sb("oo", [128, A_V], F32) for _ in range(2)]
        sq = P.sb("sq", [128, A_V], F32)
        ss = [P.sb("ss", [128, 1], F32) for _ in range(2)]
        yb = [P.sb("yb", [128, A_V], BF16) for _ in range(2)]

        for c in range(2):
            P.dma(lambda e, c=c: e.dma_start(out=qs[:, c, :], in_=qT[c]), writes=[qs])
            P.dma(lambda e, c=c: e.dma_start(out=ks[:, c, :], in_=kT[c]), writes=[ks])
        P.dma(lambda e: e.dma_start(out=vs[:, :, 0:A_V], in_=v.rearrange("(kb p) c -> p kb c", p=128)), writes=[vs])
        P.op("gpsimd", lambda e: e.memset(vs[:, :, A_V:A_V + 1], 1.0), writes=[vs])
        for i in range(4):
            P.dma(lambda e, i=i: e.dma_start(out=lb[:, i, :], in_=lam4[i:i + 1, :].to_broadcast([128, A_QK])), writes=[lb])
        P.dma(lambda e: e.dma_start(out=anb[:], in_=an.to_broadcast([128, A_V])), writes=[anb])
        P.dma(lambda e: e.dma_start(out=lib[:], in_=li.to_broadcast([128, 1])), writes=[lib])
        for i in range(2):
            P.op("vector", lambda e, i=i: e.tensor_tensor(out=lt[:, i, :], in0=lb[:, 2 * i, :], in1=lb[:, 2 * i + 1, :],
                                                          op=ALU.mult), reads=[lb], writes=[lt])
            P.op("vector", lambda e, i=i: e.reduce_sum(out=ls[:, i:i + 1], in_=lt[:, i, :], axis=AX.X), reads=[lt], writes=[ls])
        P.op("scalar", lambda e: e.activation(out=ls[:], in_=ls[:], func=AF.Exp), reads=[ls], writes=[ls])
        P.op("vector", lambda e: e.tensor_tensor(out=lam[:], in0=ls[:, 0:1], in1=ls[:, 1:2], op=ALU.subtract),
             reads=[ls], writes=[lam])
        P.op("vector", lambda e: e.tensor_tensor(out=lam[:], in0=lam[:], in1=lib[:], op=ALU.add), reads=[lam, lib], writes=[lam])
        P.op("vector", lambda e: e.tensor_scalar(out=nlam[:], in0=lam[:], scalar1=-1.0, scalar2=None, op0=ALU.mult),
             reads=[lam], writes=[nlam])
        P.op("vector", lambda e: e.tensor_scalar(out=lib[:], in0=lib[:], scalar1=-1.0, scalar2=1.0, op0=ALU.mult, op1=ALU.add),
             reads=[lib], writes=[lib])
        P.op("vector", lambda e: e.tensor_scalar(out=anb[:], in0=anb[:], scalar1=lib[:, 0:1], scalar2=None, op0=ALU.mult),
             reads=[anb, lib], writes=[anb])

        accs = [[P.sb("accs", [128, A_V + 1], F32) for _ in range(2)] for _ in range(2)]
        iters = [(qt, kb) for qt in range(S // QT) for kb in range(NKB)]
        LOOK = 2

        def emit_scores(it):
            qt, kb = iters[it]
            q0 = qt * QT
            sp = sps_l[it % 4]
            for c in range(2):
                P.op("tensor", lambda e, sp=sp, c=c, kb=kb, q0=q0: e.matmul(
                    sp[:, c * QT:(c + 1) * QT], lhsT=ks[:, c, kb * 128:(kb + 1) * 128], rhs=qs[:, c, q0:q0 + QT],
                    start=True, stop=True), reads=[ks, qs], writes=[sp])
            pt = pts_l[it % 4]
            P.op("scalar", lambda e, sp=sp, pt=pt: e.activation(out=pt[:], in_=sp[:], func=AF.Exp), reads=[sp], writes=[pt])

        for it in range(min(LOOK, len(iters))):
            emit_scores(it)
        for it, (qt, kb) in enumerate(iters):
            q0 = qt * QT
            if it + LOOK < len(iters):
                emit_scores(it + LOOK)
            pt = pts_l[it % 4]
            for c in range(2):
                for j in range(2):
                    a = acc[c][j]
                    P.op("tensor", lambda e, a=a, pt=pt, c=c, j=j, kb=kb: e.matmul(
                        a[:, 0:A_V + 1], lhsT=pt[:, c * QT + j * 128:c * QT + (j + 1) * 128], rhs=vs[:, kb, :],
                        start=(kb == 0), stop=(kb == NKB - 1)), reads=[pt, vs], writes=[a])
            if kb != NKB - 1:
                continue
            for c in range(2):
                for j in range(2):
                    P.op("vector", lambda e, c=c, j=j: e.tensor_copy(out=accs[c][j][:], in_=acc[c][j][:, 0:A_V + 1]),
                         reads=[acc[c][j]], writes=[accs[c][j]])
            for j in range(2):
                a0, a1 = accs[0][j], accs[1][j]
                r, o_0, o_, s_, y_ = rz[j], o0[j], oo[j], ss[j], yb[j]
                P.op("vector", lambda e, a0=a0, r=r: e.reciprocal(out=r[:, 0:1], in_=a0[:, A_V:A_V + 1]), reads=[a0], writes=[r])
                P.op("vector", lambda e, a1=a1, r=r: e.reciprocal(out=r[:, 1:2], in_=a1[:, A_V:A_V + 1]), reads=[a1], writes=[r])
                P.op("gpsimd", lambda e, a0=a0, r=r, o_0=o_0: e.tensor_scalar(out=o_0[:], in0=a0[:, 0:A_V], scalar1=r[:, 0:1],
                                                                              scalar2=None, op0=ALU.mult), reads=[a0, r], writes=[o_0])
                P.op("vector", lambda e, r=r: e.tensor_tensor(out=r[:, 1:2], in0=r[:, 1:2], in1=nlam[:], op=ALU.mult),
                     reads=[r, nlam], writes=[r])
                P.op("vector", lambda e, a1=a1, r=r, o_0=o_0, o_=o_: e.scalar_tensor_tensor(
                    out=o_[:], in0=a1[:, 0:A_V], scalar=r[:, 1:2], in1=o_0[:], op0=ALU.mult, op1=ALU.add),
                    reads=[a1, r, o_0], writes=[o_])
                P.op("gpsimd", lambda e, o_=o_: e.tensor_tensor(out=sq[:], in0=o_[:], in1=o_[:], op=ALU.mult), reads=[o_], writes=[sq])
                P.op("vector", lambda e, s_=s_: e.reduce_sum(out=s_[:], in_=sq[:], axis=AX.X), reads=[sq], writes=[s_])
                P.op("vector", lambda e, s_=s_: e.tensor_scalar(out=s_[:], in0=s_[:], scalar1=1.0 / A_V, scalar2=EPS,
                                                               op0=ALU.mult, op1=ALU.add), reads=[s_], writes=[s_])
                P.op("scalar", lambda e, s_=s_: e.activation(out=s_[:], in_=s_[:], func=AF.Sqrt), reads=[s_], writes=[s_])
                P.op("vector", lambda e, s_=s_: e.reciprocal(out=s_[:], in_=s_[:]), reads=[s_], writes=[s_])
                P.op("vector", lambda e, o_=o_, s_=s_, y_=y_: e.scalar_tensor_tensor(
                    out=y_[:], in0=o_[:], scalar=s_[:, 0:1], in1=anb[:], op0=ALU.mult, op1=ALU.mult),
                    reads=[o_, s_, anb], writes=[y_])
                r0 = q0 + j * 128
                P.dma(lambda e, y_=y_, r0=r0: e.dma_start(out=ya[r0:r0 + 128, :], in_=y_[:]), reads=[y_])
        P.finish()
        P.emit()
    return nc


def run_C1(ncC1, pb, lam4, a_norm_l, lam_init):
    aq = pb[:, O_AQ - 16:O_AQ - 16 + 2048].reshape(S, A_HEADS, 2, A_QK)
    ak = pb[:, O_AK - 16:O_AK - 16 + 2048].reshape(S, A_HEADS, 2, A_QK)
    av = pb[:, O_AV - 16:O_AV - 16 + 2048].reshape(S, A_HEADS, A_V)
    in_maps = []
    for h in range(NCORES):
        in_maps.append({"qT": np.ascontiguousarray(aq[:, h].transpose(1, 2, 0)),
                        "kT": np.ascontiguousarray(ak[:, h].transpose(1, 2, 0)),
                        "v": np.ascontiguousarray(av[:, h]),
                        "lam4": lam4, "an": a_norm_l.reshape(1, A_V),
                        "li": np.full((1, 1), lam_init, np.float32)})
    res = _run(ncC1, in_maps)
    return np.concatenate([r["ya"] for r in res], axis=1)


def View(ap, name="", root=None):
    return Buf(ap, name, root)


def build_C2(nch=None, dbg=False):
    nc = _new_nc()
    qT = _din(nc, "qT", [M_QK, S], BF16)
    kT = _din(nc, "kT", [M_QK, S], BF16)
    kk = _din(nc, "kk", [S, M_QK], BF16)
    vv = _din(nc, "vv", [S, M_V], BF16)
    gi = _din(nc, "gi", [64, 128])
    gf = _din(nc, "gf", [64, 128])
    tri = _din(nc, "tri", [128, 128])
    cst = _din(nc, "cst", [64, 128])
    hd = _dout(nc, "hd", [S, M_V])
    gscr = _dout(nc, "gscr", [64, 128])
    NCH = S // 128
    dbgo = _dout(nc, "dbgo", [128, 4 * 64]) if dbg else None
    with ExitStack() as es:
        P = Prog(nc, es)
        g_i = P.sb("g_i", [64, 128], F32)
        g_f = P.sb("g_f", [64, 128], F32)
        Bw = P.sb("Bw", [64, 128], F32)
        Aw = P.sb("Aw", [64, 128], F32)
        o64 = P.sb("o64", [64, 128], F32)
        cs = P.sb("cs", [64, 128], F32)
        c1 = P.sb("c1", [64, 4], F32)
        rowM = P.sb("rowM", [1, 64], F32)
        r_gl = P.sb("r_gl", [1, NCH], F32)
        r_gp = P.sb("r_gp", [1, NCH], F32)
        r_G = P.sb("r_G", [1, S], F32)
        r_1 = P.sb("r_1", [1, 128], F32)
        one11 = P.sb("one11", [1, 1], F32)
        trim = P.sb("trim", [128, 128], F32)
        onesb = P.sb("onesb", [128, 1], BF16)
        A_col = P.sb("A_col", [128, NCH], F32)
        E_col = P.sb("E_col", [128, NCH], F32)
        GLB = P.sb("GLB", [128, NCH], F32)
        GPB = P.sb("GPB", [128, NCH], F32)
        WS = P.sb("WS", [128, NCH], F32)
        DEC = P.sb("DEC", [128, NCH], F32)
        Cst = [P.sb("C", [128, M_V], F32) for _ in range(2)]
        Cb = [P.sb("Cb", [128, M_V], BF16) for _ in range(2)]
        nst = P.sb("n", [128, 2], F32)
        nb = P.sb("nb", [128, 2], BF16)
        bk = [P.ps("bk", [128, 512], F32) for _ in range(4)]
        sT_l = [View(bk[i][:, 0:128], root=bk[i]) for i in range(2)]
        GmB_l = [View(bk[i][:, 128:256], root=bk[i]) for i in range(2)]
        Dn_l = [View(bk[2 + i][:, 0:1], root=bk[2 + i]) for i in range(2)]
        nS_l = [View(bk[2 + i][:, 2:4], root=bk[2 + i]) for i in range(2)]
        pc1 = View(bk[2][0:64, 4:5], root=bk[2])
        pc2 = View(bk[2][0:64, 5:6], root=bk[2])
        prow = View(bk[2][0:1, 64:128], root=bk[2])
        pcol = [View(bk[i][:, 256:320], root=bk[i]) for i in range(4)]
        N_l = [P.ps("N", [128, M_V], F32) for _ in range(2)]
        KV = [P.ps("KV", [128, M_V], F32) for _ in range(2)]
        kTc_l = [P.sb("kTc", [128, 2, 128], BF16) for _ in range(3)]
        qTc_l = [P.sb("qTc", [128, 2, 128], BF16) for _ in range(3)]
        kc_l = [P.sb("kc", [128, M_QK], BF16) for _ in range(3)]
        vc_l = [P.sb("vc", [128, M_V], BF16) for _ in range(3)]
        Dt_l = [P.sb("Dt", [128, 128], F32) for _ in range(2)]
        W_l = [P.sb("W", [128, 128], F32) for _ in range(2)]
        WI_l = [P.sb("WI", [128, 128], F32) for _ in range(2)]
        sw_l = [P.sb("sw", [128, 128], BF16) for _ in range(2)]
        qtl_l = [P.sb("qtl", [128, 2, 128], BF16) for _ in range(2)]
        ktl_l = [P.sb("ktl", [128, M_QK], BF16) for _ in range(2)]
        den_l = [P.sb("den", [128, 1], F32) for _ in range(2)]
        ho_l = [P.sb("ho", [128, M_V], F32) for _ in range(3)]

        P.dma(lambda e: e.dma_start(out=g_i[:], in_=gi[:, :]), writes=[g_i])
        P.dma(lambda e: e.dma_start(out=g_f[:], in_=gf[:, :]), writes=[g_f])
        P.dma(lambda e: e.dma_start(out=trim[:], in_=tri[:, :]), writes=[trim])
        P.dma(lambda e: e.dma_start(out=cs[:], in_=cst[:, :]), writes=[cs])
        P.op("gpsimd", lambda e: e.memset(r_1[:], 1.0), writes=[r_1])
        P.op("gpsimd", lambda e: e.memset(o64[:], 1.0), writes=[o64])
        P.op("gpsimd", lambda e: e.memset(one11[:], 1.0), writes=[one11])
        P.op("gpsimd", lambda e: e.memset(onesb[:], 1.0), writes=[onesb])
        for j in range(2):
            P.op("gpsimd", lambda e, j=j: e.memset(Cst[j][:], 0.0), writes=[Cst[j]])
            P.op("gpsimd", lambda e, j=j: e.memset(Cb[j][:], 0.0), writes=[Cb[j]])
        P.op("gpsimd", lambda e: e.memset(nst[:], 0.0), writes=[nst])
        P.op("gpsimd", lambda e: e.memset(nb[:], 0.0), writes=[nb])
        P.op("scalar", lambda e: e.activation(out=g_f[:], in_=g_f[:], func=AF.Exp, scale=-1.0), reads=[g_f], writes=[g_f])
        P.op("vector", lambda e: e.tensor_scalar(out=g_f[:], in0=g_f[:], scalar1=1.0, scalar2=None, op0=ALU.add),
             reads=[g_f], writes=[g_f])
        P.op("scalar", lambda e: e.activation(out=g_f[:], in_=g_f[:], func=AF.Ln), reads=[g_f], writes=[g_f])
        P.op("vector", lambda e: e.tensor_scalar(out=g_f[:], in0=g_f[:], scalar1=-1.0, scalar2=None, op0=ALU.mult),
             reads=[g_f], writes=[g_f])
        P.op("vector", lambda e: e.tensor_tensor_scan(out=Bw[:], data0=o64[:], data1=g_f[:], initial=0.0,
                                                      op0=ALU.mult, op1=ALU.add), reads=[o64, g_f], writes=[Bw])
        P.op("vector", lambda e: e.tensor_copy(out=c1[:, 0:1], in_=Bw[:, 127:128]), reads=[Bw], writes=[c1])
        P.op("tensor", lambda e: e.matmul(pc1[:], lhsT=cs[:, 64:128], rhs=c1[:, 0:1], start=True, stop=True),
             reads=[cs, c1], writes=[pc1])
        P.op("vector", lambda e: e.tensor_copy(out=c1[:, 1:2], in_=pc1[:]), reads=[pc1], writes=[c1])
        P.op("vector", lambda e: e.tensor_scalar(out=Bw[:], in0=Bw[:], scalar1=c1[:, 1:2], scalar2=None, op0=ALU.add),
             reads=[Bw, c1], writes=[Bw])
        P.op("vector", lambda e: e.tensor_tensor(out=g_i[:], in0=g_i[:], in1=Bw[:], op=ALU.subtract),
             reads=[g_i, Bw], writes=[g_i])
        P.op("vector", lambda e: e.tensor_tensor_scan(out=Aw[:], data0=g_i[:], data1=g_i[:], initial=-1.0e30,
                                                      op0=ALU.max, op1=ALU.max), reads=[g_i], writes=[Aw])
        P.op("vector", lambda e: e.tensor_copy(out=c1[:, 2:3], in_=Aw[:, 127:128]), reads=[Aw], writes=[c1])
        P.op("tensor", lambda e: e.matmul(prow[:], lhsT=c1[:, 2:3], rhs=cs[:, 0:64], start=True, stop=True),
             reads=[cs, c1], writes=[prow])
        P.op("vector", lambda e: e.tensor_copy(out=rowM[:], in_=prow[:]), reads=[prow], writes=[rowM])
        P.op("vector", lambda e: e.tensor_tensor_scan(out=r_gl[:], data0=rowM[:], data1=rowM[:], initial=0.0,
                                                      op0=ALU.max, op1=ALU.max), reads=[rowM], writes=[r_gl])
        P.op("gpsimd", lambda e: e.memset(r_gp[:, 0:1], 0.0), writes=[r_gp])
        P.op("vector", lambda e: e.tensor_copy(out=r_gp[:, 1:NCH], in_=r_gl[:, 0:NCH - 1]), reads=[r_gl, r_gp], writes=[r_gp])
        P.op("tensor", lambda e: e.matmul(pc2[:], lhsT=r_gp[0:1, :], rhs=one11[0:1, 0:1], start=True, stop=True),
             reads=[r_gp, one11], writes=[pc2])
        P.op("vector", lambda e: e.tensor_copy(out=c1[:, 3:4], in_=pc2[:]), reads=[pc2], writes=[c1])
        P.op("vector", lambda e: e.tensor_scalar(out=Aw[:], in0=Aw[:], scalar1=c1[:, 3:4], scalar2=None, op0=ALU.max),
             reads=[Aw, c1], writes=[Aw])
        P.op("vector", lambda e: e.tensor_tensor(out=Bw[:], in0=Bw[:], in1=Aw[:], op=ALU.add), reads=[Bw, Aw], writes=[Bw])
        P.op("scalar", lambda e: e.activation(out=Bw[:], in_=Bw[:], func=AF.Exp, scale=-1.0), reads=[Bw], writes=[Bw])
        gs = View(gscr)
        P.dma(lambda e: e.dma_start(out=gscr[:, :], in_=Aw[:]), reads=[Aw], writes=[gs])
        P.dma(lambda e: e.dma_start(out=r_G[:], in_=gscr.rearrange("(o c) t -> o (c t)", o=1)), reads=[gs], writes=[r_G])
        for (src, col, pc) in ((g_i, A_col, pcol[0]), (Bw, E_col, pcol[1])):
            P.op("tensor", lambda e, src=src, pc=pc: e.transpose(out=pc[:], in_=src[:], identity=cs[:, 0:64]),
                 reads=[src, cs], writes=[pc])
            P.op("vector", lambda e, col=col, pc=pc: e.tensor_copy(out=col[:], in_=pc[:]), reads=[pc], writes=[col])
        for (row, col, pc) in ((r_gl, GLB, pcol[2]), (r_gp, GPB, pcol[3])):
            P.op("tensor", lambda e, row=row, pc=pc: e.matmul(pc[:], lhsT=r_1[0:1, 0:128], rhs=row[0:1, :], start=True, stop=True),
                 reads=[row, r_1], writes=[pc])
            P.op("vector", lambda e, col=col, pc=pc: e.tensor_copy(out=col[:], in_=pc[:]), reads=[pc], writes=[col])
        P.op("vector", lambda e: e.tensor_tensor(out=WS[:], in0=A_col[:], in1=GLB[:], op=ALU.subtract), reads=[A_col, GLB], writes=[WS])
        P.op("scalar", lambda e: e.activation(out=WS[:], in_=WS[:], func=AF.Exp), reads=[WS], writes=[WS])
        P.op("vector", lambda e: e.tensor_tensor(out=DEC[:], in0=GPB[:], in1=GLB[:], op=ALU.subtract), reads=[GPB, GLB], writes=[DEC])
        P.op("scalar", lambda e: e.activation(out=DEC[:], in_=DEC[:], func=AF.Exp), reads=[DEC], writes=[DEC])

        if dbg:
            for i, col in enumerate((A_col, E_col, GLB, GPB)):
                P.dma(lambda e, i=i, col=col: e.dma_start(out=dbgo[:, i * 64:(i + 1) * 64], in_=col[:]), reads=[col])
        for c in range(NCH if nch is None else nch):
            t0 = c * 128
            kTc, qTc, kc, vc = kTc_l[c % 3], qTc_l[c % 3], kc_l[c % 3], vc_l[c % 3]
            P.dma(lambda e, kTc=kTc, t0=t0: e.dma_start(out=kTc[:], in_=kT[:, t0:t0 + 128].rearrange("(j p) t -> p j t", p=128)), writes=[kTc])
            P.dma(lambda e, qTc=qTc, t0=t0: e.dma_start(out=qTc[:], in_=qT[:, t0:t0 + 128].rearrange("(j p) t -> p j t", p=128)), writes=[qTc])
            P.dma(lambda e, kc=kc, t0=t0: e.dma_start(out=kc[:], in_=kk[t0:t0 + 128, :]), writes=[kc])
            P.dma(lambda e, vc=vc, t0=t0: e.dma_start(out=vc[:], in_=vv[t0:t0 + 128, :]), writes=[vc])
            sT, GmB, Dn, Np, nS = sT_l[c % 2], GmB_l[c % 2], Dn_l[c % 2], N_l[c % 2], nS_l[c % 2]
            Dt, W, WI, sw, qtl, ktl, den, ho = Dt_l[c % 2], W_l[c % 2], WI_l[c % 2], sw_l[c % 2], qtl_l[c % 2], ktl_l[c % 2], den_l[c % 2], ho_l[c % 3]
            for j in range(2):
                P.op("tensor", lambda e, sT=sT, kTc=kTc, qTc=qTc, j=j: e.matmul(sT[:], lhsT=kTc[:, j, :], rhs=qTc[:, j, :],
                                                                               start=(j == 0), stop=(j == 1)),
                     reads=[kTc, qTc], writes=[sT])
            P.op("tensor", lambda e, GmB=GmB, t0=t0: e.matmul(GmB[:], lhsT=r_1[0:1, 0:128], rhs=r_G[0:1, t0:t0 + 128],
                                                              start=True, stop=True), reads=[r_1, r_G], writes=[GmB])
            P.op("vector", lambda e, Dt=Dt, GmB=GmB, c=c: e.tensor_scalar(out=Dt[:], in0=GmB[:], scalar1=A_col[:, c:c + 1], scalar2=0.0,
                                                                          op0=ALU.subtract, op1=ALU.max), reads=[GmB, A_col], writes=[Dt])
            P.op("scalar", lambda e, Dt=Dt, W=W: e.activation(out=W[:], in_=Dt[:], func=AF.Exp, scale=-1.0), reads=[Dt], writes=[W])
            P.op("gpsimd", lambda e, W=W: e.tensor_tensor(out=W[:], in0=W[:], in1=trim[:], op=ALU.mult), reads=[W, trim], writes=[W])
            P.op("vector", lambda e, sw=sw, sT=sT, W=W: e.tensor_tensor(out=sw[:], in0=sT[:], in1=W[:], op=ALU.mult),
                 reads=[sT, W], writes=[sw])
            P.op("scalar", lambda e, WI=WI, GmB=GmB, c=c: e.activation(out=WI[:], in_=GmB[:], func=AF.Exp, scale=-1.0,
                                                                      bias=GPB[:, c:c + 1]), reads=[GmB, GPB], writes=[WI])
            for j in range(2):
                P.op("gpsimd" if j == 0 else "vector", lambda e, qtl=qtl, qTc=qTc, WI=WI, j=j: e.tensor_tensor(
                    out=qtl[:, j, :], in0=qTc[:, j, :], in1=WI[:], op=ALU.mult), reads=[qTc, WI], writes=[qtl])
            P.op("tensor", lambda e, Np=Np, sw=sw, vc=vc: e.matmul(Np[:], lhsT=sw[:], rhs=vc[:], start=True, stop=False),
                 reads=[sw, vc], writes=[Np])
            for j in range(2):
                P.op("tensor", lambda e, Np=Np, qtl=qtl, j=j: e.matmul(Np[:], lhsT=qtl[:, j, :], rhs=Cb[j][:], start=False, stop=(j == 1)),
                     reads=[qtl, Cb[j]], writes=[Np])
            P.op("tensor", lambda e, Dn=Dn, sw=sw: e.matmul(Dn[:], lhsT=sw[:], rhs=onesb[:], start=True, stop=False),
                 reads=[sw, onesb], writes=[Dn])
            for j in range(2):
                P.op("tensor", lambda e, Dn=Dn, qtl=qtl, j=j: e.matmul(Dn[:], lhsT=qtl[:, j, :], rhs=nb[:, j:j + 1], start=False, stop=(j == 1)),
                     reads=[qtl, nb], writes=[Dn])
            P.op("scalar", lambda e, den=den, Dn=Dn: e.activation(out=den[:], in_=Dn[:], func=AF.Abs), reads=[Dn], writes=[den])
            P.op("vector", lambda e, den=den, c=c: e.tensor_scalar(out=den[:], in0=den[:], scalar1=E_col[:, c:c + 1], scalar2=None,
                                                                   op0=ALU.max), reads=[den, E_col], writes=[den])
            P.op("vector", lambda e, den=den: e.reciprocal(out=den[:], in_=den[:]), reads=[den], writes=[den])
            P.op("scalar", lambda e, ho=ho, Np=Np, den=den: e.activation(out=ho[:], in_=Np[:], func=AF.Copy, scale=den[:, 0:1]),
                 reads=[Np, den], writes=[ho])
            P.dma(lambda e, ho=ho, t0=t0: e.dma_start(out=hd[t0:t0 + 128, :], in_=ho[:]), reads=[ho])
            P.op("gpsimd", lambda e, ktl=ktl, kc=kc, c=c: e.tensor_scalar(out=ktl[:], in0=kc[:], scalar1=WS[:, c:c + 1], scalar2=None,
                                                                          op0=ALU.mult), reads=[kc, WS], writes=[ktl])
            for j in range(2):
                P.op("tensor", lambda e, ktl=ktl, vc=vc, j=j: e.matmul(KV[j][:], lhsT=ktl[:, j * 128:(j + 1) * 128], rhs=vc[:],
                                                                      start=True, stop=True), reads=[ktl, vc], writes=[KV[j]])
            for j in range(2):
                P.op("tensor", lambda e, ktl=ktl, j=j, nS=nS: e.matmul(nS[:, j:j + 1], lhsT=ktl[:, j * 128:(j + 1) * 128], rhs=onesb[:],
                                                               start=True, stop=True), reads=[ktl, onesb], writes=[nS])
            for j in range(2):
                P.op("vector", lambda e, j=j, c=c: e.scalar_tensor_tensor(out=Cst[j][:], in0=Cst[j][:], scalar=DEC[:, c:c + 1], in1=KV[j][:],
                                                                          op0=ALU.mult, op1=ALU.add), reads=[Cst[j], DEC, KV[j]], writes=[Cst[j]])
                P.op("scalar", lambda e, j=j: e.activation(out=Cb[j][:], in_=Cst[j][:], func=AF.Copy), reads=[Cst[j]], writes=[Cb[j]])
            P.op("vector", lambda e, c=c, nS=nS: e.scalar_tensor_tensor(out=nst[:], in0=nst[:], scalar=DEC[:, c:c + 1], in1=nS[:],
                                                                 op0=ALU.mult, op1=ALU.add), reads=[nst, DEC, nS], writes=[nst])
            P.op("vector", lambda e: e.tensor_copy(out=nb[:], in_=nst[:]), reads=[nst], writes=[nb])
        P.finish()
        P.emit()
    return nc


def run_C2(ncC2, pb, pg):
    mq = pb[:, O_MQ:O_MQ + 1024].reshape(S, M_HEADS, M_QK)
    mk = pb[:, O_MK:O_MK + 1024].reshape(S, M_HEADS, M_QK)
    mv = pb[:, O_MV:O_MV + 2048].reshape(S, M_HEADS, M_V)
    g = pg.reshape(S, 4, M_HEADS)
    tri = np.triu(np.ones((128, 128), np.float32))
    cst = np.concatenate([np.eye(64, dtype=np.float32), np.triu(np.ones((64, 64), np.float32), 1)], axis=1)
    in_maps = []
    for core in range(NCORES):
        h, d = core // 2, core % 2
        fl = (lambda a: a[::-1]) if d == 1 else (lambda a: a)
        in_maps.append({"qT": np.ascontiguousarray(fl(mq[:, h]).T), "kT": np.ascontiguousarray(fl(mk[:, h]).T),
                        "kk": np.ascontiguousarray(fl(mk[:, h])), "vv": np.ascontiguousarray(fl(mv[:, h])),
                        "gi": np.ascontiguousarray(fl(g[:, 2 * d, h])).reshape(64, 128),
                        "gf": np.ascontiguousarray(fl(g[:, 2 * d + 1, h])).reshape(64, 128), "tri": tri, "cst": cst})
    res = _run(ncC2, in_maps)
    hfw = np.concatenate([res[2 * h]["hd"] for h in range(M_HEADS)], axis=1)
    hbw = np.concatenate([res[2 * h + 1]["hd"][::-1] for h in range(M_HEADS)], axis=1)
    return hfw, np.ascontiguousarray(hbw)


def build_D():
    nc = _new_nc()
    x = _din(nc, "x", [TL, D])
    hfw = _din(nc, "hfw", [TL, D])
    hbw = _din(nc, "hbw", [TL, D])
    ya = _din(nc, "ya", [TL, D], BF16)
    mo = _din(nc, "mo", [TL, D], BF16)
    gmi = _din(nc, "gm", [TL, D], BF16)
    gai = _din(nc, "ga", [TL, D], BF16)
    vecs = _din(nc, "vecs", [5, D])
    wbm = _din(nc, "wbm", [D, D])
    wba = _din(nc, "wba", [D, D])
    wo = _din(nc, "wo", [D, D])
    wr = _din(nc, "wr", [D, NE])
    br = _din(nc, "br", [1, NE])
    idn = _din(nc, "idn", [128, 128], BF16)
    idf = _din(nc, "idf", [128, 128])
    x1o = _dout(nc, "x1", [TL, D])
    h2o = _dout(nc, "h2", [TL, D], BF16)
    Go = _dout(nc, "G", [TL, NE])
    GT = 2
    NG = TL // 128 // GT
    with ExitStack() as es:
        P = Prog(nc, es)
        ident = P.sb("ident", [128, 128], BF16)
        identf = P.sb("identf", [128, 128], F32)
        mnb = P.sb("mnb", [128, D], F32)
        gmb = P.sb("gmb", [128, D], F32)
        wmod = P.sb("wmod", [128, D], F32)
        shb = P.sb("shb", [128, D], F32)
        wrs = P.sb("wrs", [128, 16, NE], F32)
        brb = P.sb("brb", [128, NE], F32)
        tA = [P.sb("tA", [128, D], F32)] * 2
        tB = [P.sb("tB", [128, D], F32)] * 2
        tC = P.sb("tC", [128, D], F32)
        mot = [P.sb("mot", [128, D], BF16)] * 2
        yat = [P.sb("yat", [128, D], BF16)] * 2
        ymb = [P.sb("ymb", [128, D], BF16) for _ in range(2)]
        ss4 = [P.sb("ss4", [128, 4], F32) for _ in range(2)]
        ymT = [P.sb("ymT", [128, 16, 128], BF16) for _ in range(GT)]
        yaT = [P.sb("yaT", [128, 16, 128], BF16) for _ in range(GT)]
        mrg = [P.sb("mrg", [128, D], BF16) for _ in range(GT)]
        xt = [P.sb("xt", [128, D], F32) for _ in range(GT)]
        wts = [P.sb("wt", [128, 16, 512], BF16) for _ in range(3)]
        gch = [P.sb("gch", [128, 512], BF16) for _ in range(4)]
        sgc = [P.sb("sgc", [128, 512], F32) for _ in range(4)]
        t12 = [P.sb("t12", [128, 512], F32) for _ in range(4)]
        ssr = [P.sb("ssr", [128, 1], F32) for _ in range(2)]
        rstd = [P.sb("rstd", [128, 1], F32) for _ in range(2)]
        lg = [P.sb("lg", [128, NE], F32) for _ in range(2)]
        mx8 = [P.sb("mx8", [128, 8], F32) for _ in range(2)]
        msk = [P.sb("msk", [128, NE], F32) for _ in range(2)]
        ex = [P.sb("ex", [128, NE], F32) for _ in range(2)]
        zz = [P.sb("zz", [128, 2], F32) for _ in range(2)]
        ptr_l = [P.ps("ptr", [128, 1024], BF16) for _ in range(2)]
        acc_l = [P.ps("acc", [128, 512], F32) for _ in range(4)]
        ptf_l = [P.ps("ptf", [128, 512], F32) for _ in range(2)]
        ptr, acc, ptf = RR(ptr_l), RR(acc_l), RR(ptf_l)
        wtr, gchr, sgcr, t12r = RR(wts), RR(gch), RR(sgc), RR(t12)
        evac = RR(["vector"])

        P.dma(lambda e: e.dma_start(out=ident[:], in_=idn[:, :]), writes=[ident])
        P.dma(lambda e: e.dma_start(out=identf[:], in_=idf[:, :]), writes=[identf])
        P.dma(lambda e: e.dma_start(out=mnb[:], in_=vecs[0:1, :].to_broadcast([128, D])), writes=[mnb])
        P.dma(lambda e: e.dma_start(out=gmb[:], in_=vecs[1:2, :].to_broadcast([128, D])), writes=[gmb])
        P.dma(lambda e: e.dma_start(out=tC[:], in_=vecs[2:3, :].to_broadcast([128, D])), writes=[tC])
        P.dma(lambda e: e.dma_start(out=wmod[:], in_=vecs[3:4, :].to_broadcast([128, D])), writes=[wmod])
        P.dma(lambda e: e.dma_start(out=shb[:], in_=vecs[4:5, :].to_broadcast([128, D])), writes=[shb])
        P.dma(lambda e: e.dma_start(out=wrs[:], in_=wr.rearrange("(k p) n -> p k n", p=128)), writes=[wrs])
        P.dma(lambda e: e.dma_start(out=brb[:], in_=br.to_broadcast([128, NE])), writes=[brb])
        P.op("vector", lambda e: e.scalar_tensor_tensor(out=wmod[:], in0=wmod[:], scalar=1.0, in1=tC[:],
                                                        op0=ALU.add, op1=ALU.mult), reads=[wmod, tC], writes=[wmod])

        for g in range(NG):
            for i in range(GT):
                r0 = (g * GT + i) * 128
                rows = slice(r0, r0 + 128)
                a, b, m_, y_, yb_, s4 = tA[i % 2], tB[i % 2], mot[i % 2], yat[i % 2], ymb[i % 2], ss4[i % 2]
                P.dma(lambda e, a=a, rows=rows: e.dma_start(out=a[:], in_=hfw[rows, :]), writes=[a])
                P.dma(lambda e, b=b, rows=rows: e.dma_start(out=b[:], in_=hbw[rows, :]), writes=[b])
                P.dma(lambda e, m_=m_, rows=rows: e.dma_start(out=m_[:], in_=mo[rows, :]), writes=[m_])
                P.dma(lambda e, y_=y_, rows=rows: e.dma_start(out=y_[:], in_=ya[rows, :]), writes=[y_])
                P.dma(lambda e, i=i, rows=rows: e.dma_start(out=xt[i][:], in_=x[rows, :]), writes=[xt[i]])
                P.op("gpsimd", lambda e, a=a, b=b: e.tensor_tensor(out=a[:], in0=a[:], in1=b[:], op=ALU.add), reads=[a, b], writes=[a])
                for h in range(M_HEADS):
                    hs = slice(h * M_V, (h + 1) * M_V)
                    P.op("scalar", lambda e, a=a, b=b, s4=s4, h=h, hs=hs: e.activation(out=b[:, hs], in_=a[:, hs], func=AF.Square,
                                                                                     accum_out=s4[:, h:h + 1]), reads=[a], writes=[b, s4])
                P.op("vector", lambda e, s4=s4: e.tensor_scalar(out=s4[:], in0=s4[:], scalar1=1.0 / M_V, scalar2=EPS, op0=ALU.mult, op1=ALU.add),
                     reads=[s4], writes=[s4])
                P.op("scalar", lambda e, s4=s4: e.activation(out=s4[:], in_=s4[:], func=AF.Sqrt), reads=[s4], writes=[s4])
                P.op("vector", lambda e, s4=s4: e.reciprocal(out=s4[:], in_=s4[:]), reads=[s4], writes=[s4])
                for h in range(M_HEADS):
                    hs = slice(h * M_V, (h + 1) * M_V)
                    P.op("vector", lambda e, a=a, s4=s4, h=h, hs=hs: e.scalar_tensor_tensor(
                        out=a[:, hs], in0=a[:, hs], scalar=s4[:, h:h + 1], in1=mnb[:, hs], op0=ALU.mult, op1=ALU.mult),
                        reads=[a, s4, mnb], writes=[a])
                P.op("scalar", lambda e, m_=m_: e.activation(out=tC[:], in_=m_[:], func=AF.Sigmoid), reads=[m_], writes=[tC])
                P.op("gpsimd", lambda e, a=a, yb_=yb_: e.tensor_tensor(out=yb_[:], in0=a[:], in1=tC[:], op=ALU.mult), reads=[a, tC], writes=[yb_])
                transpose_tile(P, yb_, ymT[i], ident, ptr, evac)
                transpose_tile(P, y_, yaT[i], ident, ptr, evac)
            for n in range(4):
                cols = slice(n * 512, (n + 1) * 512)
                w1, w2 = wtr(), wtr()
                P.dma(lambda e, w1=w1, cols=cols: e.dma_start(out=w1[:], in_=wbm[:, cols].rearrange("(k p) n -> p k n", p=128)),
                      writes=[w1], eng="gpsimd")
                P.dma(lambda e, w2=w2, cols=cols: e.dma_start(out=w2[:], in_=wba[:, cols].rearrange("(k p) n -> p k n", p=128)),
                      writes=[w2], eng="gpsimd")
                for i in range(GT):
                    r0 = (g * GT + i) * 128
                    rows = slice(r0, r0 + 128)
                    a1, a2 = acc(), acc()
                    for k in range(16):
                        P.op("tensor", lambda e, a1=a1, w1=w1, i=i, k=k: e.matmul(a1[:], lhsT=ymT[i][:, k, :], rhs=w1[:, k, :],
                                                                                start=(k == 0), stop=(k == 15)), reads=[ymT[i], w1], writes=[a1])
                    for k in range(16):
                        P.op("tensor", lambda e, a2=a2, w2=w2, i=i, k=k: e.matmul(a2[:], lhsT=yaT[i][:, k, :], rhs=w2[:, k, :],
                                                                                start=(k == 0), stop=(k == 15)), reads=[yaT[i], w2], writes=[a2])
                    g1, g2, s1, s2, t1, t2 = gchr(), gchr(), sgcr(), sgcr(), t12r(), t12r()
                    P.dma(lambda e, g1=g1, rows=rows, cols=cols: e.dma_start(out=g1[:], in_=gmi[rows, cols]), writes=[g1])
                    P.dma(lambda e, g2=g2, rows=rows, cols=cols: e.dma_start(out=g2[:], in_=gai[rows, cols]), writes=[g2])
                    P.op("scalar", lambda e, g1=g1, s1=s1: e.activation(out=s1[:], in_=g1[:], func=AF.Sigmoid), reads=[g1], writes=[s1])
                    P.op("scalar", lambda e, g2=g2, s2=s2: e.activation(out=s2[:], in_=g2[:], func=AF.Sigmoid), reads=[g2], writes=[s2])
                    P.op("vector", lambda e, a1=a1, s1=s1, t1=t1: e.tensor_tensor(out=t1[:], in0=a1[:], in1=s1[:], op=ALU.mult), reads=[a1, s1], writes=[t1])
                    P.op("vector", lambda e, a2=a2, s2=s2, t2=t2: e.tensor_tensor(out=t2[:], in0=a2[:], in1=s2[:], op=ALU.mult), reads=[a2, s2], writes=[t2])
                    P.op("gpsimd", lambda e, t1=t1, t2=t2, i=i, cols=cols: e.tensor_tensor(out=mrg[i][:, cols], in0=t1[:], in1=t2[:], op=ALU.add),
                         reads=[t1, t2], writes=[mrg[i]])
            for i in range(GT):
                transpose_tile(P, mrg[i], ymT[i], ident, ptr, evac)
            for n in range(4):
                cols = slice(n * 512, (n + 1) * 512)
                w1 = wtr()
                P.dma(lambda e, w1=w1, cols=cols: e.dma_start(out=w1[:], in_=wo[:, cols].rearrange("(k p) n -> p k n", p=128)),
                      writes=[w1], eng="gpsimd")
                for i in range(GT):
                    a1 = acc()
                    for k in range(16):
                        P.op("tensor", lambda e, a1=a1, w1=w1, i=i, k=k: e.matmul(a1[:], lhsT=ymT[i][:, k, :], rhs=w1[:, k, :],
                                                                                start=(k == 0), stop=(k == 15)), reads=[ymT[i], w1], writes=[a1])
                    t1 = t12r()
                    P.op("vector", lambda e, a1=a1, t1=t1, cols=cols: e.tensor_tensor(out=t1[:], in0=a1[:], in1=gmb[:, cols], op=ALU.mult),
                         reads=[a1, gmb], writes=[t1])
                    P.op("gpsimd", lambda e, t1=t1, i=i, cols=cols: e.tensor_tensor(out=xt[i][:, cols], in0=xt[i][:, cols], in1=t1[:], op=ALU.add),
                         reads=[t1, xt[i]], writes=[xt[i]])
            for i in range(GT):
                r0 = (g * GT + i) * 128
                rows = slice(r0, r0 + 128)
                scr, h2f, h2b = tA[i % 2], tB[i % 2], ymb[i % 2]
                P.dma(lambda e, i=i, rows=rows: e.dma_start(out=x1o[rows, :], in_=xt[i][:]), reads=[xt[i]])
                rmsnorm_mod_tile(P, xt[i], wmod, shb, h2f, scr, ssr[i % 2], rstd[i % 2])
                P.op("scalar", lambda e, h2f=h2f, h2b=h2b: e.activation(out=h2b[:], in_=h2f[:], func=AF.Copy), reads=[h2f], writes=[h2b])
                P.dma(lambda e, h2b=h2b, rows=rows: e.dma_start(out=h2o[rows, :], in_=h2b[:]), reads=[h2b])
                for q4 in range(4):
                    pt = ptf()
                    for j in range(4):
                        k = q4 * 4 + j
                        P.op("tensor", lambda e, pt=pt, j=j, k=k, h2f=h2f: e.transpose(out=pt[:, j * 128:(j + 1) * 128],
                                                                                   in_=h2f[:, k * 128:(k + 1) * 128], identity=identf[:]),
                             reads=[h2f, identf], writes=[pt])
                    P.op("scalar" if q4 % 2 == 0 else "vector",
                         (lambda e, pt=pt, q4=q4: e.activation(out=tC[:, q4 * 512:(q4 + 1) * 512], in_=pt[:], func=AF.Copy)) if q4 % 2 == 0 else
                         (lambda e, pt=pt, q4=q4: e.tensor_copy(out=tC[:, q4 * 512:(q4 + 1) * 512], in_=pt[:])),
                         reads=[pt], writes=[tC])
                a1 = acc()
                for k in range(16):
                    P.op("tensor", lambda e, a1=a1, k=k: e.matmul(a1[:, 0:NE], lhsT=tC[:, k * 128:(k + 1) * 128], rhs=wrs[:, k, :],
                                                               start=(k == 0), stop=(k == 15)), reads=[tC, wrs], writes=[a1])
                l_, m8, mk, e_, z_ = lg[i % 2], mx8[i % 2], msk[i % 2], ex[i % 2], zz[i % 2]
                P.op("vector", lambda e, a1=a1, l_=l_: e.tensor_tensor(out=l_[:], in0=a1[:, 0:NE], in1=brb[:], op=ALU.add), reads=[a1, brb], writes=[l_])
                P.op("vector", lambda e, l_=l_, m8=m8: e.max(out=m8[:], in_=l_[:]), reads=[l_], writes=[m8])
                P.op("vector", lambda e, l_=l_, m8=m8, mk=mk: e.tensor_scalar(out=mk[:], in0=l_[:], scalar1=m8[:, 3:4], scalar2=None, op0=ALU.is_ge),
                     reads=[l_, m8], writes=[mk])
                P.op("vector", lambda e, m8=m8, z_=z_: e.tensor_scalar(out=z_[:, 0:1], in0=m8[:, 0:1], scalar1=-1.0, scalar2=None, op0=ALU.mult),
                     reads=[m8], writes=[z_])
                P.op("scalar", lambda e, l_=l_, e_=e_, z_=z_: e.activation(out=e_[:], in_=l_[:], func=AF.Exp, bias=z_[:, 0:1]), reads=[l_, z_], writes=[e_])
                P.op("vector", lambda e, e_=e_, mk=mk: e.tensor_tensor(out=e_[:], in0=e_[:], in1=mk[:], op=ALU.mult), reads=[e_, mk], writes=[e_])
                P.op("vector", lambda e, e_=e_, z_=z_: e.reduce_sum(out=z_[:, 1:2], in_=e_[:], axis=AX.X), reads=[e_], writes=[z_])
                P.op("vector", lambda e, z_=z_: e.reciprocal(out=z_[:, 1:2], in_=z_[:, 1:2]), reads=[z_], writes=[z_])
                P.op("vector", lambda e, e_=e_, z_=z_: e.tensor_scalar(out=e_[:], in0=e_[:], scalar1=z_[:, 1:2], scalar2=None, op0=ALU.mult),
                     reads=[e_, z_], writes=[e_])
                P.dma(lambda e, e_=e_, rows=rows: e.dma_start(out=Go[rows, :], in_=e_[:]), reads=[e_])
        P.finish()
        P.emit()
    return nc


def run_D(ncD, x2d, hfw, hbw, ya, pb, m_norm_l, g_m, norm_ffn_l, sc_f, sh_f, w_br_m, w_br_a, w_out, w_router, b_router):
    vecs = np.stack([m_norm_l.reshape(D), g_m, norm_ffn_l, sc_f, sh_f]).astype(np.float32)
    idn = np.eye(128, dtype=np.float32).astype(NPBF)
    idf = np.eye(128, dtype=np.float32)
    mo = pb[:, O_MO:O_MO + 2048]
    gm = pb[:, O_GM - 16:O_GM - 16 + 2048]
    ga = pb[:, O_GA - 16:O_GA - 16 + 2048]
    in_maps = []
    for i in range(NCORES):
        rows = slice(i * TL, (i + 1) * TL)
        c = np.ascontiguousarray
        in_maps.append({"x": c(x2d[rows]), "hfw": c(hfw[rows]), "hbw": c(hbw[rows]), "ya": c(ya[rows]), "mo": c(mo[rows]),
                        "gm": c(gm[rows]), "ga": c(ga[rows]), "vecs": vecs, "wbm": w_br_m, "wba": w_br_a, "wo": w_out,
                        "wr": w_router, "br": b_router.reshape(1, NE), "idn": idn, "idf": idf})
    res = _run(ncD, in_maps)
    x1 = np.concatenate([r["x1"] for r in res], axis=0)
    h2 = np.concatenate([r["h2"] for r in res], axis=0)
    G = np.concatenate([r["G"] for r in res], axis=0)
    return x1, h2, G


SPC = BPC * EBLK
NSH = EBLK // 128
WC = 1024
SERIAL_SCATTER = False
BIGI = 1000000.0


def build_E(nblk=None):
    nc = _new_nc()
    GTi = _din(nc, "GT", [NE, S])
    h2 = _din(nc, "h2", [S, D], BF16)
    wgu = _din(nc, "wgu", [NE * D * 4, WC])
    wdn = _din(nc, "wdn", [NE * DFF * 2, WC])
    bgu = _din(nc, "bgu", [NE * 2, 2048])
    bdn = _din(nc, "bdn", [NE, D])
    idn = _din(nc, "idn", [128, 128], BF16)
    idf = _din(nc, "idf", [128, 128])
    cst = _din(nc, "cst", [NE, NE + BPC + 32])
    jc = _din(nc, "jc", [128, 96])
    tokid = _din(nc, "tokid", [128, S // 128], I32)
    basei = _din(nc, "base", [128, 2])
    ys = _dout(nc, "ys", [SPC, D], BF16)
    destMo = _dout(nc, "destM", [NE, S])
    stok = _dout(nc, "stok", [SPC + 128, 1], I32)
    NTT = S // 128
    with ExitStack() as es:
        P = Prog(nc, es)
        Wr = [P.sb("W", [128, 16, WC], BF16) for _ in range(2)]
        Wj = [[View(Wr[r][:, j, :]) for j in range(16)] for r in range(2)]
        raw = Wr[0].t[:].rearrange("p j n -> p (j n)").bitcast(F32)
        mk = View(raw[0:NE, 0:S], root=Wr[0])
        raw1 = Wr[1].t[:].rearrange("p j n -> p (j n)").bitcast(F32)
        cum = View(raw1[0:NE, 0:S], root=Wr[1])
        ident = P.sb("ident", [128, 128], BF16)
        identf = P.sb("identf", [128, 128], F32)
        cs = P.sb("cs", [NE, NE + BPC + 32], F32)
        jct = P.sb("jct", [128, 96], F32)
        tki = P.sb("tki", [128, NTT], I32)
        bas = P.sb("bas", [128, 2], F32)
        c32 = P.sb("c32", [NE, 8], F32)
        cmp_ = P.sb("cmp", [NE, BPC], F32)
        o32 = P.sb("o32", [NE, 128], F32)
        ebf = P.sb("ebf", [128, BPC], F32)
        wif = P.sb("wif", [128, BPC * 96], F32)
        wii = P.sb("wii", [128, BPC * 96], I32)
        bif = P.sb("bif", [128, BPC * 4], F32)
        bii = P.sb("bii", [128, BPC * 4], I32)
        i4 = P.sb("i4", [128, NTT * 4], I32)
        zt = P.sb("zt", [128, SPC // 128 + 1], I32)
        sti = P.sb("sti", [128, SPC // 128], I32)
        ones1 = P.sb("ones1", [1, 128], BF16)
        xb = P.sb("xb", [128, NSH, D], BF16)
        xbs = [View(xb[:, sh, :]) for sh in range(NSH)]
        xbT = P.sb("xbT", [128, 16, EBLK], BF16)
        rawx = xbT.t[:].rearrange("p j n -> p (j n)").bitcast(F32)
        dtok = View(rawx[:, 0:NTT * NE].rearrange("p (t e) -> p t e", e=NE), root=xbT)
        bg = P.sb("bg", [128, 2, 2048], BF16)
        bgs = [View(bg[:, c, :]) for c in range(2)]
        bd = P.sb("bd", [128, D], BF16)
        tg = P.sb("tg", [128, NSH, 2048], BF16)
        rawt = tg.t[:].rearrange("p a n -> p (a n)").bitcast(F32)
        d8 = View(rawt[:, 0:NTT * 8].rearrange("p (t k) -> p t k", k=8), root=tg)
        d4 = View(rawt[:, 512:512 + NTT * 4], root=tg)
        v1 = View(rawt[:, 768:768 + NTT * 4], root=tg)
        v2 = View(rawt[:, 1024:1024 + NTT * 4], root=tg)
        act = [P.sb("act", [128, DFF], BF16) for _ in range(NSH)]
        actT = P.sb("actT", [128, 16, EBLK], BF16)
        tmp_l = [P.sb("tmp", [128, 512], F32) for _ in range(4)]
        ysb_l = [P.sb("ysb", [128, 512], BF16) for _ in range(2)]
        ptr_l = [P.ps("ptr", [128, 1024], BF16) for _ in range(2)]
        acc_l = [P.ps("acc", [128, 512], F32) for _ in range(4)]
        ptr, acc, tmpr, ysbr = RR(ptr_l), RR(acc_l), RR(tmp_l), RR(ysb_l)
        evac = RR(["vector"])
        stv = View(stok)
        scat = [Buf(None, "scat%d" % i) for i in range(NTT * 4)]

        P.dma(lambda e: e.dma_start(out=ident[:], in_=idn[:, :]), writes=[ident])
        P.dma(lambda e: e.dma_start(out=identf[:], in_=idf[:, :]), writes=[identf])
        P.dma(lambda e: e.dma_start(out=cs[:], in_=cst[:, :]), writes=[cs])
        P.dma(lambda e: e.dma_start(out=jct[:], in_=jc[:, :]), writes=[jct])
        tk1 = [P.sb("tk1", [128, 1], I32) for _ in range(NTT)]
        for tt in range(NTT):
            P.dma(lambda e, tt=tt: e.dma_start(out=tk1[tt][:], in_=tokid[:, tt:tt + 1], allow_slow_non_contiguous=True), writes=[tk1[tt]])
        P.dma(lambda e: e.dma_start(out=bas[:], in_=basei[:, :]), writes=[bas])
        P.dma(lambda e: e.dma_start(out=mk[:], in_=GTi[:, :]), writes=[mk])
        P.op("gpsimd", lambda e: e.memset(ones1[:], 1.0), writes=[ones1])
        P.op("gpsimd", lambda e: e.memset(zt[:], 0), writes=[zt])
        P.dma(lambda e: e.dma_start(out=stok.rearrange("(p c) o -> p (c o)", p=128), in_=zt[:]), reads=[zt], writes=[stv])
        P.op("vector", lambda e: e.tensor_scalar(out=mk[:], in0=mk[:], scalar1=0.0, scalar2=None, op0=ALU.is_gt), reads=[mk], writes=[mk])
        P.op("vector", lambda e: e.tensor_tensor_scan(out=cum[:], data0=mk[:], data1=mk[:], initial=0.0, op0=ALU.add, op1=ALU.max),
             reads=[mk], writes=[cum])
        P.op("vector", lambda e: e.tensor_scalar(out=o32[:, 0:NE], in0=cs[:, NE + BPC:NE + BPC + 32], scalar1=cum[:, S - 1:S], scalar2=None,
                                                 op0=ALU.is_lt), reads=[cs, cum], writes=[o32])
        P.op("vector", lambda e: e.reduce_sum(out=c32[:, 1:2], in_=o32[:, 0:NE], axis=AX.X), reads=[o32], writes=[c32])
        P.op("vector", lambda e: e.tensor_scalar(out=c32[:, 1:2], in0=c32[:, 1:2], scalar1=float(EBLK), scalar2=None, op0=ALU.mult), reads=[c32], writes=[c32])
        P.op("gpsimd", lambda e: e.memset(o32[:], 1.0), reads=[o32], writes=[o32])
        a0 = acc()
        P.op("tensor", lambda e: e.matmul(a0[0:NE, 0:1], lhsT=cs[:, 0:NE], rhs=c32[:, 1:2], start=True, stop=True), reads=[cs, c32], writes=[a0])
        P.op("vector", lambda e: e.tensor_copy(out=c32[:, 3:4], in_=a0[0:NE, 0:1]), reads=[a0], writes=[c32])
        P.op("vector", lambda e: e.tensor_tensor(out=c32[:, 4:5], in0=c32[:, 3:4], in1=c32[:, 1:2], op=ALU.subtract), reads=[c32], writes=[c32])
        P.op("vector", lambda e: e.tensor_tensor(out=cum[:], in0=cum[:], in1=mk[:], op=ALU.subtract), reads=[cum, mk], writes=[cum])
        P.op("vector", lambda e: e.tensor_scalar(out=cum[:], in0=cum[:], scalar1=c32[:, 4:5], scalar2=1.0, op0=ALU.add, op1=ALU.add),
             reads=[cum, c32], writes=[cum])
        P.op("vector", lambda e: e.tensor_tensor(out=cum[:], in0=cum[:], in1=mk[:], op=ALU.mult), reads=[cum, mk], writes=[cum])
        P.dma(lambda e: e.dma_start(out=destMo[:, :], in_=cum[:]), reads=[cum])
        P.op("vector", lambda e: e.tensor_scalar(out=cmp_[:], in0=cs[:, NE:NE + BPC], scalar1=c32[:, 3:4], scalar2=None, op0=ALU.is_ge),
             reads=[cs, c32], writes=[cmp_])
        a1 = acc()
        P.op("tensor", lambda e: e.matmul(a1[:, 0:BPC], lhsT=o32[:], rhs=cmp_[:], start=True, stop=True), reads=[o32, cmp_], writes=[a1])
        P.op("vector", lambda e: e.tensor_scalar(out=ebf[:], in0=a1[:, 0:BPC], scalar1=float(NE - 1), scalar2=None, op0=ALU.min), reads=[a1], writes=[ebf])
        for b in range(BPC):
            P.op("vector", lambda e, b=b: e.tensor_scalar(out=bif[:, b * 4 + 3:b * 4 + 4], in0=ebf[:, b:b + 1], scalar1=8192.0, scalar2=None, op0=ALU.mult),
                 reads=[ebf], writes=[bif])
            P.op("vector", lambda e, b=b: e.tensor_scalar(out=wif[:, b * 96:b * 96 + 64], in0=jct[:, 0:64], scalar1=bif[:, b * 4 + 3:b * 4 + 4], scalar2=None, op0=ALU.add),
                 reads=[jct, bif], writes=[wif])
            P.op("vector", lambda e, b=b: e.tensor_scalar(out=bif[:, b * 4 + 3:b * 4 + 4], in0=ebf[:, b:b + 1], scalar1=4096.0, scalar2=None, op0=ALU.mult),
                 reads=[ebf], writes=[bif])
            P.op("vector", lambda e, b=b: e.tensor_scalar(out=wif[:, b * 96 + 64:b * 96 + 96], in0=jct[:, 64:96], scalar1=bif[:, b * 4 + 3:b * 4 + 4], scalar2=None, op0=ALU.add),
                 reads=[jct, bif], writes=[wif])
            P.op("vector", lambda e, b=b: e.tensor_scalar(out=bif[:, b * 4:b * 4 + 1], in0=ebf[:, b:b + 1], scalar1=2.0, scalar2=None, op0=ALU.mult),
                 reads=[ebf], writes=[bif])
            P.op("vector", lambda e, b=b: e.tensor_scalar(out=bif[:, b * 4 + 1:b * 4 + 2], in0=ebf[:, b:b + 1], scalar1=2.0, scalar2=1.0, op0=ALU.mult, op1=ALU.add),
                 reads=[ebf], writes=[bif])
            P.op("vector", lambda e, b=b: e.tensor_copy(out=bif[:, b * 4 + 2:b * 4 + 3], in_=ebf[:, b:b + 1]), reads=[ebf], writes=[bif])
        P.op("vector", lambda e: e.tensor_copy(out=wii[:], in_=wif[:]), reads=[wif], writes=[wii])
        P.op("vector", lambda e: e.tensor_copy(out=bii[:], in_=bif[:]), reads=[bif], writes=[bii])
        for g in range(NTT // 16):
            a = acc()
            for j in range(16):
                tt = g * 16 + j
                P.op("tensor", lambda e, a=a, j=j, tt=tt: e.transpose(out=a[:, j * NE:(j + 1) * NE], in_=cum[:, tt * 128:(tt + 1) * 128],
                                                                    identity=identf[0:NE, 0:NE]), reads=[cum, identf], writes=[a])
            P.op("vector", lambda e, a=a, g=g: e.tensor_copy(out=dtok[:, g * 16:(g + 1) * 16, :], in_=a[:].rearrange("p (t e) -> p t e", e=NE)),
                 reads=[a], writes=[dtok])
        for tt in range(NTT):
            P.op("vector", lambda e, tt=tt: e.max(out=d8[:, tt, :], in_=dtok[:, tt, :]), reads=[dtok], writes=[d8])
        P.op("vector", lambda e: e.tensor_scalar(out=d4[:].rearrange("p (t k) -> p t k", k=4), in0=d8[:, :, 0:4], scalar1=bas[:, 0:1], scalar2=-1.0, op0=ALU.subtract, op1=ALU.add),
             reads=[d8, bas], writes=[d4])
        P.op("vector", lambda e: e.tensor_scalar(out=v1[:], in0=d4[:], scalar1=0.0, scalar2=None, op0=ALU.is_ge), reads=[d4], writes=[v1])
        P.op("vector", lambda e: e.tensor_scalar(out=v2[:], in0=d4[:], scalar1=float(SPC), scalar2=None, op0=ALU.is_lt), reads=[d4], writes=[v2])
        P.op("vector", lambda e: e.tensor_tensor(out=v1[:], in0=v1[:], in1=v2[:], op=ALU.mult), reads=[v1, v2], writes=[v1])
        P.op("vector", lambda e: e.scalar_tensor_tensor(out=d4[:], in0=d4[:], scalar=bas[:, 1:2], in1=v1[:], op0=ALU.subtract, op1=ALU.mult),
             reads=[d4, v1, bas], writes=[d4])
        P.op("vector", lambda e: e.tensor_scalar(out=d4[:], in0=d4[:], scalar1=bas[:, 1:2], scalar2=None, op0=ALU.add), reads=[d4, bas], writes=[d4])
        P.op("vector", lambda e: e.tensor_copy(out=i4[:], in_=d4[:]), reads=[d4], writes=[i4])
        for tt in range(NTT):
            for k in range(4):
                P.dma(lambda e, tt=tt, k=k: e.indirect_dma_start(
                    out=stok[:, :], out_offset=bass.IndirectOffsetOnAxis(ap=i4[:, tt * 4 + k:tt * 4 + k + 1], axis=0), in_=tk1[tt][:, :],
                    in_offset=None), reads=[i4, tk1[tt]] + ([] if SERIAL_SCATTER else [stv]),
                    writes=[scat[tt * 4 + k]] + ([stv] if SERIAL_SCATTER else []), eng="gpsimd")
        P.dma(lambda e: e.dma_start(out=sti[:], in_=stok[0:SPC, :].rearrange("(c p) o -> p (c o)", p=128), allow_slow_non_contiguous=True),
              reads=scat, writes=[sti, stv])

        fz = P.sb("fz", [1, 1], F32)
        P.op("gpsimd", lambda e: e.memset(fz[:], 0.0), writes=[fz, Wr[0], Wr[1], tg, xbT] + Wj[0] + Wj[1])
        ring = 0
        for b in range(BPC if nblk is None else nblk):
            for sh in range(NSH):
                P.dma(lambda e, b=b, sh=sh: e.indirect_dma_start(
                    out=xb[:, sh, :], out_offset=None, in_=h2[:, :],
                    in_offset=bass.IndirectOffsetOnAxis(ap=sti[:, b * NSH + sh:b * NSH + sh + 1], axis=0)), reads=[sti], writes=[xbs[sh]], eng="gpsimd")
            for c in range(2):
                P.dma(lambda e, b=b, c=c: e.indirect_dma_start(
                    out=bg[:, c, :], out_offset=None, in_=bgu[:, :],
                    in_offset=bass.IndirectOffsetOnAxis(ap=bii[:, b * 4 + c:b * 4 + c + 1], axis=0)), reads=[bii], writes=[bgs[c]], eng="gpsimd")
            P.dma(lambda e, b=b: e.indirect_dma_start(
                out=bd[:], out_offset=None, in_=bdn[:, :],
                in_offset=bass.IndirectOffsetOnAxis(ap=bii[:, b * 4 + 2:b * 4 + 3], axis=0)), reads=[bii], writes=[bd], eng="gpsimd")
            for sh in range(NSH):
                transpose_tile(P, xbs[sh], xbT, ident, ptr, evac,
                               dst_sl=lambda g, n, sh=sh: (lambda d, g=g, n=n, sh=sh: d[:, g:g + n, sh * 128:(sh + 1) * 128]))
            for c4 in (0, 2, 1, 3):
                r = ring % 2
                ring += 1
                for j in range(16):
                    P.dma(lambda e, b=b, c4=c4, j=j, r=r: e.indirect_dma_start(
                        out=Wr[r][:, j, :], out_offset=None, in_=wgu[:, :],
                        in_offset=bass.IndirectOffsetOnAxis(ap=wii[:, b * 96 + j * 4 + c4:b * 96 + j * 4 + c4 + 1], axis=0)),
                        reads=[wii], writes=[Wj[r][j]], eng="gpsimd")
                c = c4 // 2
                for n in range(WC // 512):
                    colsW = slice(n * 512, (n + 1) * 512)
                    g0 = (c4 % 2) * WC + n * 512
                    cols = slice(g0, g0 + 512)
                    for sh in range(NSH):
                        a = acc()
                        for j in range(16):
                            P.op("tensor", lambda e, a=a, r=r, j=j, sh=sh, colsW=colsW: e.matmul(
                                a[:], lhsT=xbT[:, j, sh * 128:(sh + 1) * 128], rhs=Wr[r][:, j, colsW], start=(j == 0), stop=False),
                                reads=[xbT, Wj[r][j]], writes=[a])
                        P.op("tensor", lambda e, a=a, c=c, cols=cols: e.matmul(a[:], lhsT=ones1[0:1, :], rhs=bg[0:1, c, cols], start=False, stop=True),
                             reads=[ones1, bgs[c]], writes=[a])
                        t_ = tmpr()
                        if c == 0:
                            s_ = tmpr()
                            P.op("vector", lambda e, a=a, t_=t_: e.tensor_scalar(out=t_[:], in0=a[:], scalar1=7.0, scalar2=None, op0=ALU.min),
                                 reads=[a], writes=[t_])
                            P.op("scalar", lambda e, t_=t_, s_=s_: e.activation(out=s_[:], in_=t_[:], func=AF.Sigmoid, scale=1.702),
                                 reads=[t_], writes=[s_])
                            P.op("gpsimd", lambda e, t_=t_, s_=s_, sh=sh, cols=cols: e.tensor_tensor(out=tg[:, sh, cols], in0=t_[:], in1=s_[:], op=ALU.mult),
                                 reads=[t_, s_], writes=[tg])
                        else:
                            P.op("vector", lambda e, a=a, t_=t_: e.tensor_scalar(out=t_[:], in0=a[:], scalar1=7.0, scalar2=-7.0, op0=ALU.min, op1=ALU.max),
                                 reads=[a], writes=[t_])
                            P.op("vector", lambda e, t_=t_, sh=sh, cols=cols: e.scalar_tensor_tensor(
                                out=act[sh][:, cols], in0=t_[:], scalar=1.0, in1=tg[:, sh, cols], op0=ALU.add, op1=ALU.mult),
                                reads=[t_, tg], writes=[act[sh]])
            for sh in range(NSH):
                transpose_tile(P, act[sh], actT, ident, ptr, evac,
                               dst_sl=lambda g, n, sh=sh: (lambda d, g=g, n=n, sh=sh: d[:, g:g + n, sh * 128:(sh + 1) * 128]))
            for c2 in range(2):
                r = ring % 2
                ring += 1
                for j in range(16):
                    P.dma(lambda e, b=b, j=j, r=r, c2=c2: e.indirect_dma_start(
                        out=Wr[r][:, j, :], out_offset=None, in_=wdn[:, :],
                        in_offset=bass.IndirectOffsetOnAxis(ap=wii[:, b * 96 + 64 + j * 2 + c2:b * 96 + 64 + j * 2 + c2 + 1], axis=0)),
                        reads=[wii], writes=[Wj[r][j]], eng="gpsimd")
                for n in range(WC // 512):
                    colsW = slice(n * 512, (n + 1) * 512)
                    g0 = c2 * WC + n * 512
                    cols = slice(g0, g0 + 512)
                    for sh in range(NSH):
                        a = acc()
                        for j in range(16):
                            P.op("tensor", lambda e, a=a, r=r, j=j, sh=sh, colsW=colsW: e.matmul(
                                a[:], lhsT=actT[:, j, sh * 128:(sh + 1) * 128], rhs=Wr[r][:, j, colsW], start=(j == 0), stop=False),
                                reads=[actT, Wj[r][j]], writes=[a])
                        P.op("tensor", lambda e, a=a, cols=cols: e.matmul(a[:], lhsT=ones1[0:1, :], rhs=bd[0:1, cols], start=False, stop=True),
                             reads=[ones1, bd], writes=[a])
                        y_ = ysbr()
                        P.op("scalar", lambda e, a=a, y_=y_: e.activation(out=y_[:], in_=a[:], func=AF.Copy), reads=[a], writes=[y_])
                        r0 = b * EBLK + sh * 128
                        P.dma(lambda e, y_=y_, r0=r0, cols=cols: e.dma_start(out=ys[r0:r0 + 128, cols], in_=y_[:]), reads=[y_])
        P.finish()
        P.emit()
    return nc


def run_E(ncE, G, h2, w_gu_l, b_gu_l, w_down_l, b_down_l):
    GT = np.ascontiguousarray(G.T)
    idn = np.eye(128, dtype=np.float32).astype(NPBF)
    idf = np.eye(128, dtype=np.float32)
    U = np.triu(np.ones((NE, NE), np.float32))
    p = np.arange(128, dtype=np.float32)[:, None]
    jg = np.arange(64)
    jd = np.arange(32)
    jc = np.concatenate([(jg // 4 * 128)[None, :] * 4.0 + (jg % 4)[None, :] + 4.0 * p,
                         (jd // 2 * 128)[None, :] * 2.0 + (jd % 2)[None, :] + 2.0 * p], axis=1).astype(np.float32)
    tokid = (np.arange(S // 128, dtype=np.int32)[None, :] * 128 + np.arange(128, dtype=np.int32)[:, None]).astype(np.int32)
    wgu = w_gu_l.reshape(NE * D * 4, WC)
    wdn = w_down_l.reshape(NE * DFF * 2, WC)
    bgu = b_gu_l.reshape(NE * 2, 2048)
    in_maps = []
    for i in range(NCORES):
        thr = ((BPC * i + np.arange(BPC)) * EBLK).astype(np.float32)
        cst = np.concatenate([U, np.tile(thr[None, :], (NE, 1)), np.tile((np.arange(32) * float(EBLK))[None, :], (NE, 1))], axis=1).astype(np.float32)
        in_maps.append({"GT": GT, "h2": h2, "wgu": wgu, "wdn": wdn, "bgu": bgu, "bdn": b_down_l, "idn": idn, "idf": idf,
                        "cst": cst, "jc": jc, "tokid": tokid, "base": np.stack([np.full(128, SPC * i, np.float32), SPC + np.arange(128, dtype=np.float32)], axis=1)})
    res = _run(ncE, in_maps)
    ys = np.concatenate([r["ys"] for r in res], axis=0)
    destM = res[0]["destM"]
    return ys, destM


def build_F(final):
    nc = _new_nc()
    ysf = _din(nc, "ys", [NSLOT, D], BF16)
    dmi = _din(nc, "dm", [TL, NE])
    gti = _din(nc, "gt", [TL, NE])
    x1 = _din(nc, "x1", [TL, D])
    vecs = _din(nc, "vecs", [2, D])
    out = _dout(nc, "out", [TL, D])
    NT = TL // 128
    with ExitStack() as es:
        P = Prog(nc, es)
        gfb = P.sb("gfb", [128, D], F32)
        nfb = P.sb("nfb", [128, D], F32)
        zb = P.sb("zb", [128, D], F32)
        dm = [P.sb("dm", [128, NE], F32) for _ in range(2)]
        gt = [P.sb("gt", [128, NE], F32) for _ in range(2)]
        d8 = [P.sb("d8", [128, 8], F32) for _ in range(2)]
        d4f = [P.sb("d4f", [128, 4], F32) for _ in range(2)]
        d4i = [P.sb("d4i", [128, 4], I32) for _ in range(2)]
        oh = [P.sb("oh", [128, NE], F32) for _ in range(2)]
        g4 = [P.sb("g4", [128, 4], F32) for _ in range(2)]
        Y = [[P.sb("Y", [128, D], BF16) for _ in range(4)] for _ in range(2)]
        xt = [P.sb("xt", [128, D], F32) for _ in range(2)]
        ac = [P.sb("ac", [128, D], F32) for _ in range(2)]
        scr = P.sb("scr", [128, D], F32)
        ot = [P.sb("ot", [128, D], F32) for _ in range(2)]
        ss = [P.sb("ss", [128, 1], F32) for _ in range(2)]
        rstd = [P.sb("rstd", [128, 1], F32) for _ in range(2)]
        P.dma(lambda e: e.dma_start(out=gfb[:], in_=vecs[0:1, :].to_broadcast([128, D])), writes=[gfb])
        if final:
            P.dma(lambda e: e.dma_start(out=nfb[:], in_=vecs[1:2, :].to_broadcast([128, D])), writes=[nfb])
            P.op("gpsimd", lambda e: e.memset(zb[:], 0.0), writes=[zb])
        for tt in range(NT):
            i = tt % 2
            rows = slice(tt * 128, (tt + 1) * 128)
            P.dma(lambda e, i=i, rows=rows: e.dma_start(out=dm[i][:], in_=dmi[rows, :]), writes=[dm[i]])
            P.dma(lambda e, i=i, rows=rows: e.dma_start(out=gt[i][:], in_=gti[rows, :]), writes=[gt[i]])
            P.dma(lambda e, i=i, rows=rows: e.dma_start(out=xt[i][:], in_=x1[rows, :]), writes=[xt[i]])
            P.op("vector", lambda e, i=i: e.max(out=d8[i][:], in_=dm[i][:]), reads=[dm[i]], writes=[d8[i]])
            P.op("vector", lambda e, i=i: e.tensor_scalar(out=d4f[i][:], in0=d8[i][:, 0:4], scalar1=-1.0, scalar2=None, op0=ALU.add),
                 reads=[d8[i]], writes=[d4f[i]])
            P.op("vector", lambda e, i=i: e.tensor_copy(out=d4i[i][:], in_=d4f[i][:]), reads=[d4f[i]], writes=[d4i[i]])
            for k in range(4):
                P.dma(lambda e, i=i, k=k: e.indirect_dma_start(out=Y[i][k][:], out_offset=None, in_=ysf[:, :],
                                                               in_offset=bass.IndirectOffsetOnAxis(ap=d4i[i][:, k:k + 1], axis=0)),
                      reads=[d4i[i]], writes=[Y[i][k]], eng="gpsimd")
                P.op("vector", lambda e, i=i, k=k: e.tensor_scalar(out=oh[i][:], in0=dm[i][:], scalar1=d8[i][:, k:k + 1], scalar2=None,
                                                                   op0=ALU.is_equal), reads=[dm[i], d8[i]], writes=[oh[i]])
                P.op("vector", lambda e, i=i: e.tensor_tensor(out=oh[i][:], in0=oh[i][:], in1=gt[i][:], op=ALU.mult), reads=[oh[i], gt[i]], writes=[oh[i]])
                P.op("vector", lambda e, i=i, k=k: e.reduce_sum(out=g4[i][:, k:k + 1], in_=oh[i][:], axis=AX.X), reads=[oh[i]], writes=[g4[i]])
            P.op("vector", lambda e, i=i: e.tensor_scalar(out=ac[i][:], in0=Y[i][0][:], scalar1=g4[i][:, 0:1], scalar2=None, op0=ALU.mult),
                 reads=[Y[i][0], g4[i]], writes=[ac[i]])
            for k in range(1, 4):
                P.op("vector", lambda e, i=i, k=k: e.scalar_tensor_tensor(out=ac[i][:], in0=Y[i][k][:], scalar=g4[i][:, k:k + 1], in1=ac[i][:],
                                                                          op0=ALU.mult, op1=ALU.add), reads=[Y[i][k], g4[i], ac[i]], writes=[ac[i]])
            P.op("gpsimd", lambda e, i=i: e.tensor_tensor(out=ac[i][:], in0=ac[i][:], in1=gfb[:], op=ALU.mult), reads=[ac[i], gfb], writes=[ac[i]])
            P.op("gpsimd", lambda e, i=i: e.tensor_tensor(out=xt[i][:], in0=xt[i][:], in1=ac[i][:], op=ALU.add), reads=[ac[i], xt[i]], writes=[xt[i]])
            if final:
                rmsnorm_mod_tile(P, xt[i], nfb, zb, ot[i], scr, ss[i], rstd[i])
                P.dma(lambda e, i=i, rows=rows: e.dma_start(out=out[rows, :], in_=ot[i][:]), reads=[ot[i]])
            else:
                P.dma(lambda e, i=i, rows=rows: e.dma_start(out=out[rows, :], in_=xt[i][:]), reads=[xt[i]])
        P.finish()
        P.emit()
    return nc


def run_F(ncF, ys, destM, G, x1, g_f, norm_final):
    dmT = np.ascontiguousarray(destM.T)
    vecs = np.stack([g_f, norm_final]).astype(np.float32)
    in_maps = []
    for i in range(NCORES):
        rows = slice(i * TL, (i + 1) * TL)
        c = np.ascontiguousarray
        in_maps.append({"ys": ys, "dm": c(dmT[rows]), "gt": c(G[rows]), "x1": c(x1[rows]), "vecs": vecs})
    res = _run(ncF, in_maps)
    return np.concatenate([r["out"] for r in res], axis=0)


def kernel(x, c, norm_mix, norm_ffn, w_ada, b_ada, w_in, b_mgates, m_norm, lam_q1, lam_k1, lam_q2, lam_k2, a_norm,
           w_br_m, w_br_a, w_out, w_router, b_router, w_gu, b_gu, w_down, b_down, norm_final):
    f = lambda a: np.ascontiguousarray(np.asarray(a, dtype=np.float32))
    xs = f(x)[0]
    mod = run_A(f(c), f(w_ada), f(b_ada))
    ncB, ncC1, ncC2, ncD, ncE = build_B(), build_C1(), build_C2(), build_D(), build_E()
    for l in range(DEPTH):
        lam_init = 0.8 - 0.6 * float(np.exp(-0.3 * l))
        sh_m, sc_m, g_m, sh_f, sc_f, g_f = [f(v) for v in np.split(mod[l], 6)]
        pb, pg = run_B(ncB, xs, f(norm_mix[l]), sc_m, sh_m, f(w_in[l]), f(b_mgates[l]))
        lam4 = np.stack([f(lam_q1[l]), f(lam_k1[l]), f(lam_q2[l]), f(lam_k2[l])])
        ya = run_C1(ncC1, pb, lam4, f(a_norm[l]), lam_init)
        hfw, hbw = run_C2(ncC2, pb, pg)
        x1, h2, G = run_D(ncD, xs, hfw, hbw, ya, pb, f(m_norm[l]), g_m, f(norm_ffn[l]), sc_f, sh_f,
                          f(w_br_m[l]), f(w_br_a[l]), f(w_out[l]), f(w_router[l]), f(b_router[l]))
        ys, destM = run_E(ncE, G, h2, f(w_gu[l]), f(b_gu[l]), f(w_down[l]), f(b_down[l]))
        xs = run_F(build_F(l == DEPTH - 1), ys, destM, G, x1, g_f, f(norm_final))
    return xs.reshape(1, S, D).astype(np.float32)
```
